# Optimizing a Trainium2 kernel written in Bass

```python
import jax, jax.numpy as jnp
from jax import lax
import numpy as np

D_MODEL = 2048
BATCH = 4
SEQ = 8192
DEPTH = 4

N_MIXERS = 2
EPS = 1e-6

A_HEAD_DIM = 128
A_HEADS = D_MODEL // A_HEAD_DIM
A_WIDTH = A_HEADS * A_HEAD_DIM
A_PATTERNS = ((128, 1), (512, 4), (2048, 16))
A_N_GROUPS = len(A_PATTERNS)
A_PROJ = A_N_GROUPS * 3 * A_WIDTH
A_BLOCK = 128
ROPE_THETA = 10000.0

R_QK_DIM = 256
R_V_DIM = 512
R_HEADS = D_MODEL // R_QK_DIM
R_QK_WIDTH = R_HEADS * R_QK_DIM
R_V_WIDTH = R_HEADS * R_V_DIM
R_PROJ = 2 * R_QK_WIDTH + 2 * R_V_WIDTH
R_CHUNK = 128

D_FF = 5632
CONV_WIDTH = 3

N_ATTN_LAYERS = (DEPTH + N_MIXERS - 1) // N_MIXERS
N_RET_LAYERS = DEPTH // N_MIXERS

kernel_name = 'hybrid_dilated_swa_retention_convffn'


def rmsnorm(x, g):
    xf = x.astype(jnp.float32)
    y = xf * lax.rsqrt(jnp.mean(xf * xf, axis=-1, keepdims=True) + EPS)
    return (y * g.astype(jnp.float32)).astype(x.dtype)


def rotary_tables(seq_len, inv_freq):
    ang = jnp.arange(seq_len, dtype=jnp.float32)[:, None] * inv_freq[None, :]
    return jnp.cos(ang), jnp.sin(ang)


def rotate(x, cos, sin):
    x1, x2 = jnp.split(x.astype(jnp.float32), 2, axis=-1)
    c, s = cos[None, :, None, :], sin[None, :, None, :]
    return jnp.concatenate([x1 * c - x2 * s, x1 * s + x2 * c], axis=-1).astype(x.dtype)


def dilated_window_group(q, k, v, window, dilation):
    B, S, H, Dh = q.shape
    n_back = window // dilation
    assert n_back <= A_BLOCK
    span = dilation * A_BLOCK
    s_pad = -(-S // span) * span
    L = s_pad // dilation
    nb = L // A_BLOCK
    pad = ((0, 0), (0, s_pad - S), (0, 0), (0, 0))

    def to_blocks(t):
        t = jnp.pad(t, pad).reshape(B, L, dilation, H, Dh).transpose(0, 2, 1, 3, 4)
        return t.reshape(B, dilation, nb, A_BLOCK, H, Dh)

    def with_prev(t):
        prev = jnp.pad(t, ((0, 0), (0, 0), (1, 0), (0, 0), (0, 0), (0, 0)))[:, :, :-1]
        return jnp.concatenate([prev, t], axis=3)

    qb = to_blocks(q)
    kb = with_prev(to_blocks(k))
    vb = with_prev(to_blocks(v))
    s = jnp.einsum('brnihd,brnjhd->brnhij', qb, kb,
                   preferred_element_type=jnp.float32) * (Dh ** -0.5)
    i = jnp.arange(A_BLOCK)[:, None]
    j = jnp.arange(2 * A_BLOCK)[None, :]
    dist = i + A_BLOCK - j
    band = (dist >= 0) & (dist <= n_back)
    has_prev = (jnp.arange(nb) > 0)[:, None, None]
    valid = band[None] & (has_prev | (j >= A_BLOCK)[None])
    s = jnp.where(valid[None, None, :, None], s, -jnp.inf)
    m = jnp.max(s, axis=-1, keepdims=True)
    p = jnp.exp(s - m)
    l = jnp.sum(p, axis=-1, keepdims=True)
    o = jnp.einsum('brnhij,brnjhd->brnihd', p / l, vb.astype(jnp.float32))
    lse = (m + jnp.log(l))[..., 0].transpose(0, 1, 2, 4, 3)
    o = o.reshape(B, dilation, L, H, Dh).transpose(0, 2, 1, 3, 4).reshape(B, s_pad, H, Dh)[:, :S]
    lse = lse.reshape(B, dilation, L, H).transpose(0, 2, 1, 3).reshape(B, s_pad, H)[:, :S]
    return o, lse


def dilated_attention_mixer(h, w_in, w_out, cos, sin):
    B, S, _ = h.shape
    proj = (h @ w_in).reshape(B, S, A_N_GROUPS, 3, A_HEADS, A_HEAD_DIM)
    outs, lses = [], []
    for g, (window, dilation) in enumerate(A_PATTERNS):
        q = rotate(proj[:, :, g, 0], cos, sin)
        k = rotate(proj[:, :, g, 1], cos, sin)
        o, lse = dilated_window_group(q, k, proj[:, :, g, 2], window, dilation)
        outs.append(o)
        lses.append(lse)
    wts = jax.nn.softmax(jnp.stack(lses), axis=0)
    o = jnp.einsum('gbsh,gbshd->bshd', wts, jnp.stack(outs))
    return o.reshape(B, S, A_WIDTH).astype(h.dtype) @ w_out


def chunkwise_retention(q, k, v):
    B, S, H, dk = q.shape
    dv = v.shape[-1]
    C = R_CHUNK
    N = S // C
    log_gamma = jnp.log1p(-jnp.exp2(-5.0 - jnp.arange(H, dtype=jnp.float32)))
    idx = jnp.arange(C, dtype=jnp.float32)
    rel = idx[:, None] - idx[None, :]
    decay = jnp.where(rel >= 0, jnp.exp(log_gamma[:, None, None] * jnp.maximum(rel, 0.0)), 0.0)
    q_decay = jnp.exp(log_gamma[None, :] * (idx[:, None] + 1.0))
    k_decay = jnp.exp(log_gamma[None, :] * (C - 1.0 - idx[:, None]))
    chunk_decay = jnp.exp(log_gamma * C)
    qc = q.astype(jnp.float32).reshape(B, N, C, H, dk)
    kc = k.astype(jnp.float32).reshape(B, N, C, H, dk)
    vc = v.astype(jnp.float32).reshape(B, N, C, H, dv)
    scores = jnp.einsum('bnihd,bnjhd->bnhij', qc, kc) * decay[None, None]
    y_intra = jnp.einsum('bnhij,bnjhe->bnihe', scores, vc)

    def step(state, inp):
        q_n, k_n, v_n = inp
        cross = jnp.einsum('bihd,bhde->bihe', q_n * q_decay[None, :, :, None], state)
        state = state * chunk_decay[None, :, None, None] + jnp.einsum(
            'bjhd,bjhe->bhde', k_n * k_decay[None, :, :, None], v_n)
        return state, cross

    state0 = jnp.zeros((B, H, dk, dv), jnp.float32)
    _, y_cross = lax.scan(step, state0, (qc.swapaxes(0, 1), kc.swapaxes(0, 1), vc.swapaxes(0, 1)))
    return (y_intra + y_cross.swapaxes(0, 1)).reshape(B, S, H, dv)


def retention_mixer(h, w_in, w_out, cos, sin):
    B, S, _ = h.shape
    proj = h @ w_in
    q, k, v, g = jnp.split(proj, [R_QK_WIDTH, 2 * R_QK_WIDTH, 2 * R_QK_WIDTH + R_V_WIDTH], axis=-1)
    q = rotate(q.reshape(B, S, R_HEADS, R_QK_DIM), cos, sin)
    k = rotate(k.reshape(B, S, R_HEADS, R_QK_DIM), cos, sin) * (R_QK_DIM ** -0.5)
    y = chunkwise_retention(q, k, v.reshape(B, S, R_HEADS, R_V_DIM))
    mu = jnp.mean(y, axis=-1, keepdims=True)
    var = jnp.mean(jnp.square(y - mu), axis=-1, keepdims=True)
    y = (y - mu) * lax.rsqrt(var + EPS)
    out = jax.nn.silu(g.astype(jnp.float32)) * y.reshape(B, S, R_V_WIDTH)
    return out.astype(h.dtype) @ w_out


def conv_ffn(h, w_up, conv_w, conv_b, w_down):
    u = h @ w_up
    c = u.shape[-1]
    u = lax.conv_general_dilated(
        u, conv_w[:, None, :].astype(u.dtype), window_strides=(1,),
        padding=[(CONV_WIDTH - 1, 0)], dimension_numbers=('NWC', 'WIO', 'NWC'),
        feature_group_count=c) + conv_b
    gate, up = jnp.split(u, 2, axis=-1)
    return (jax.nn.silu(gate) * up) @ w_down


def setup_inputs(seed: int = 0) -> dict:
    key = jax.random.key(seed)
    ks = jax.random.split(key, 12)
    f32 = jnp.float32
    out_scale = (2.0 * DEPTH) ** -0.5

    def w(k, shape, fan_in, scale=1.0):
        return jax.random.normal(k, shape, f32) * (fan_in ** -0.5 * scale)

    return {
        'x': jax.random.normal(ks[0], (BATCH, SEQ, D_MODEL), f32),
        'norm_mix': 1.0 + 0.02 * jax.random.normal(ks[1], (DEPTH, D_MODEL), f32),
        'norm_ffn': 1.0 + 0.02 * jax.random.normal(ks[2], (DEPTH, D_MODEL), f32),
        'norm_final': 1.0 + 0.02 * jax.random.normal(ks[3], (D_MODEL,), f32),
        'w_in_attn': w(ks[4], (N_ATTN_LAYERS, D_MODEL, A_PROJ), D_MODEL),
        'w_out_attn': w(ks[5], (N_ATTN_LAYERS, A_WIDTH, D_MODEL), A_WIDTH, out_scale),
        'w_in_ret': w(ks[6], (N_RET_LAYERS, D_MODEL, R_PROJ), D_MODEL),
        'w_out_ret': w(ks[7], (N_RET_LAYERS, R_V_WIDTH, D_MODEL), R_V_WIDTH, out_scale),
        'w_up': w(ks[8], (DEPTH, D_MODEL, 2 * D_FF), D_MODEL),
        'conv_w': w(ks[9], (DEPTH, CONV_WIDTH, 2 * D_FF), CONV_WIDTH),
        'conv_b': 0.01 * jax.random.normal(ks[10], (DEPTH, 2 * D_FF), f32),
        'w_down': w(ks[11], (DEPTH, D_FF, D_MODEL), D_FF, out_scale),
    }


def reference(x, norm_mix, norm_ffn, norm_final, w_in_attn, w_out_attn, w_in_ret, w_out_ret,
              w_up, conv_w, conv_b, w_down):
    S = x.shape[1]
    inv_freq_a = ROPE_THETA ** (-jnp.arange(0, A_HEAD_DIM, 2, dtype=jnp.float32) / A_HEAD_DIM)
    cos_a, sin_a = rotary_tables(S, inv_freq_a)
    inv_freq_r = ROPE_THETA ** (-jnp.linspace(0.0, 1.0, R_QK_DIM // 2, dtype=jnp.float32))
    cos_r, sin_r = rotary_tables(S, inv_freq_r)
    h = x
    for i in range(DEPTH):
        hn = rmsnorm(h, norm_mix[i])
        li = i // N_MIXERS
        if i % N_MIXERS == 0:
            h = h + dilated_attention_mixer(hn, w_in_attn[li], w_out_attn[li], cos_a, sin_a)
        else:
            h = h + retention_mixer(hn, w_in_ret[li], w_out_ret[li], cos_r, sin_r)
        h = h + conv_ffn(rmsnorm(h, norm_ffn[i]), w_up[i], conv_w[i], conv_b[i], w_down[i])
    return rmsnorm(h, norm_final)
```

```python
import contextlib
import numpy as np
import ml_dtypes
import concourse.bass as bass
import concourse.mybir as mybir
from concourse.bass_utils import run_bass_kernel_spmd

F32 = mybir.dt.float32
BF16 = mybir.dt.bfloat16
AF = mybir.ActivationFunctionType
ALU = mybir.AluOpType

D = 2048
DFF = 5632
NFF = DFF // 128
EPS = 1e-6
A_PAT = ((128, 1), (512, 4), (2048, 16))
A_PROJ = 18432
R_PROJ = 12288
QB = 2048


class Eng:
    def __init__(self, name, h, sem):
        self.name, self.h, self.sem = name, h, sem
        self.count = 0
        self.seen = {}


class Slot:
    def __init__(self, sem):
        self.sem = sem
        self.count = 0


class Sched:
    def __init__(self, nc, es):
        self.nc = nc
        sem = lambda n: es.enter_context(nc.semaphore(n))
        self.E = {
            "pe": Eng("pe", nc.tensor, sem("s_pe")),
            "act": Eng("act", nc.scalar, sem("s_act")),
            "dve": Eng("dve", nc.vector, sem("s_dve")),
            "pool": Eng("pool", nc.gpsimd, sem("s_pool")),
            "sp": Eng("sp", nc.sync, sem("s_sp")),
        }
        self.rings = {
            "sp": [Slot(sem(f"d_sp{i}")) for i in range(40)],
            "pool": [Slot(sem(f"d_pl{i}")) for i in range(32)],
            "act": [Slot(sem(f"d_ac{i}")) for i in range(12)],
        }
        self.rpos = {"sp": 0, "pool": 0, "act": 0}
        self.lw = {}
        self.rd = {}

    def _wait(self, E, dep):
        sem, v, owner = dep
        if v <= 0:
            return
        if owner == E.name:
            if E.name == "pe":
                return
            if v <= E.count - 3:
                return
        if E.seen.get(sem.name, 0) >= v:
            return
        E.h.wait_ge(sem, v)
        E.seen[sem.name] = v

    def _deps(self, E, reads, writes):
        for k in reads:
            if k in self.lw:
                self._wait(E, self.lw[k])
        for k in writes:
            if k in self.lw:
                self._wait(E, self.lw[k])
            for d in self.rd.get(k, {}).values():
                self._wait(E, d)

    def _stamp(self, stamp, reads, writes):
        for k in writes:
            self.lw[k] = stamp
            self.rd[k] = {}
        for k in reads:
            self.rd.setdefault(k, {})[stamp[0].name] = stamp

    def op(self, eng, emit, reads=(), writes=(), inc=True):
        E = self.E[eng]
        self._deps(E, reads, writes)
        ins = emit()
        if inc:
            E.count += 1
            ins.then_inc(E.sem, 1)
            stamp = (E.sem, E.count, eng)
        else:
            stamp = (E.sem, E.count + 1, eng)
        self._stamp(stamp, reads, writes)
        return ins

    def dma(self, q, out, in_, reads=(), writes=()):
        Q = self.E[q]
        ring = self.rings[q]
        slot = ring[self.rpos[q] % len(ring)]
        self.rpos[q] += 1
        self._wait(Q, (slot.sem, slot.count, None))
        self._deps(Q, reads, writes)
        ins = Q.h.dma_start(out=out, in_=in_)
        ins.then_inc(slot.sem, 16)
        slot.count += 16
        self._stamp((slot.sem, slot.count, None), reads, writes)
        return ins

    def barrier(self):
        for E in self.E.values():
            for F in self.E.values():
                if F is not E:
                    self._wait(E, (F.sem, F.count, F.name))
            for ring in self.rings.values():
                for s in ring:
                    self._wait(E, (s.sem, s.count, None))
        self.lw = {}
        self.rd = {}


def build(T, depth):
    nc = bass.Bass("TRN2", target_bir_lowering=False)
    NTB = T // 512
    n_attn = (depth + 1) // 2
    n_ret = depth // 2

    def din(name, shape, dt=F32):
        return nc.dram_tensor(name, list(shape), dt, kind="ExternalInput").ap()

    def dscr(name, shape, dt):
        return nc.dram_tensor(name, list(shape), dt).ap()

    x = din("x", [T, D])
    gains = din("gains", [128, 9 * 16])
    ident_in = din("ident", [128, 128])
    ca_in, sa_in = din("ca", [128, T]), din("sa", [128, T])
    cr_in, sr_in = din("cr", [128, T]), din("sr", [128, T])
    amask_in = din("amask", [128, 256])
    rmask_in = din("rmask", [128, 128])
    rconst_in = din("rconst", [128, 24])
    cw_in = din("cw", [128, 4 * 88 * 3])
    cb_in = din("cb", [128, 4 * 88])
    w_in_attn = din("w_in_attn", [2, D, A_PROJ])
    w_out_attn = din("w_out_attn", [2, D, D])
    w_in_ret = din("w_in_ret", [2, D, R_PROJ])
    w_out_ret = din("w_out_ret", [2, 4096, D])
    w_up = din("w_up", [4, D, 2 * DFF])
    w_down = din("w_down", [4, DFF, D])
    out = nc.dram_tensor("out", [T, D], F32, kind="ExternalOutput").ap()

    hT = dscr("hT", [D, T], F32)
    oT = dscr("oT", [4096, T], BF16)
    wqk_a = dscr("wqk_a", [max(n_attn, 1), 48, 128, 16, 256], BF16)
    wv_a = dscr("wv_a", [max(n_attn, 1), 12, 128, 16, 512], BF16)
    wo_a = dscr("wo_a", [max(n_attn, 1), 16, 128, 16, 128], BF16)
    wqk_r = dscr("wqk_r", [max(n_ret, 1), 16, 128, 16, 256], BF16)
    wvg_r = dscr("wvg_r", [max(n_ret, 1), 16, 128, 16, 512], BF16)
    wo_r = dscr("wo_r", [max(n_ret, 1), 16, 128, 32, 128], BF16)
    wup_b = dscr("wup_b", [max(depth, 1), 88, 128, 16, 128], BF16)
    wdn_b = dscr("wdn_b", [max(depth, 1), 16, 128, NFF, 128], BF16)
    qk_a = dscr("qk_a", [3, 2, 8, 2, 128, T], BF16)
    v_a = dscr("v_a", [3, T, D], BF16)
    qk_r = dscr("qk_r", [2, 8, 2, 128, T], BF16)
    v_r = dscr("v_r", [T, 4096], BF16)
    sg_r = dscr("sg_r", [T, 4096], BF16)

    es = contextlib.ExitStack()
    with es:
        S = Sched(nc, es)

        uid = [0]

        def sb(st, name, shape, dt):
            uid[0] += 1
            return st.enter_context(nc.sbuf_tensor(f"sb{uid[0]}_{name}", list(shape), dt))

        ps = [es.enter_context(nc.psum_tensor(f"ps{i}", [128, 512], F32)) for i in range(8)]
        pk = [("ps", i) for i in range(8)]

        ident = sb(es, "ident", [128, 128], F32)
        identb = sb(es, "identb", [128, 128], BF16)
        onesb = sb(es, "onesb", [128, 128], BF16)
        gsb = sb(es, "gsb", [128, 9 * 16], F32)
        amask = sb(es, "amask", [128, 256], BF16)
        rmask = sb(es, "rmask", [128, 128], F32)
        rconst = sb(es, "rconst", [128, 24], F32)
        cw = sb(es, "cw", [128, 4 * 88 * 3], F32)
        cb = sb(es, "cb", [128, 4 * 88], F32)
        with contextlib.ExitStack() as st:
            am32 = sb(st, "am32", [128, 256], F32)
            S.dma("sp", ident[:], ident_in, writes=["ident"])
            S.dma("sp", gsb[:], gains, writes=["gsb"])
            S.dma("sp", am32[:], amask_in, writes=["am32"])
            S.dma("sp", rmask[:], rmask_in, writes=["rmask"])
            S.dma("sp", rconst[:], rconst_in, writes=["rconst"])
            S.dma("sp", cw[:], cw_in, writes=["cw"])
            S.dma("sp", cb[:], cb_in, writes=["cb"])
            S.op("dve", lambda: nc.vector.tensor_copy(identb[:], ident[:]), reads=["ident"], writes=["identb"])
            S.op("dve", lambda: nc.vector.tensor_copy(amask[:], am32[:]), reads=["am32"], writes=["amask"])
            S.op("dve", lambda: nc.vector.memset(onesb[:], 1.0), writes=["onesb"])
            S.barrier()

        def cast(dst, src):
            S.dma("pool", dst, src)

        def kview(w, c0, n):
            return w[:, c0:c0 + n].rearrange("(kc p) c -> p kc c", p=128)

        for l in range(depth):
            li = l // 2
            if l % 2 == 0:
                w = w_in_attn[li]
                for g in range(3):
                    for ty in range(2):
                        for hp in range(8):
                            base = g * 6144 + ty * 2048 + hp * 256
                            blk = (g * 2 + ty) * 8 + hp
                            src = w[:, base:base + 256].rearrange("(kc p) (e x d) -> p kc x e d", p=128, e=2, x=2)
                            dstv = wqk_a[li, blk].rearrange("p kc (x e d) -> p kc x e d", x=2, e=2)
                            for xx in range(2):
                                for ee in range(2):
                                    cast(dstv[:, :, xx, ee], src[:, :, xx, ee])
                    for cbk in range(4):
                        cast(wv_a[li, g * 4 + cbk], kview(w, g * 6144 + 4096 + cbk * 512, 512))
                for m in range(16):
                    cast(wo_a[li, m], kview(w_out_attn[li], m * 128, 128))
            else:
                w = w_in_ret[li]
                for b in range(16):
                    cast(wqk_r[li, b], kview(w, b * 256, 256))
                for b in range(16):
                    cast(wvg_r[li, b], kview(w, 4096 + b * 512, 512))
                for m in range(16):
                    cast(wo_r[li, m], kview(w_out_ret[li], m * 128, 128))
            for j in range(88):
                cast(wup_b[l, j], kview(w_up[l], j * 128, 128))
            for m in range(16):
                cast(wdn_b[l, m], kview(w_down[l], m * 128, 128))

        with contextlib.ExitStack() as st:
            xs = [sb(st, f"xs{i}", [128, D], F32) for i in range(2)]
            stg = [sb(st, f"xstg{i}", [128, 16, 512], F32) for i in range(2)]
            for tb in range(NTB):
                sg = stg[tb % 2]
                sgk = ("xstg", tb % 2)
                for j in range(4):
                    ti = tb * 4 + j
                    xt = xs[ti % 2]
                    xk = ("xs", ti % 2)
                    S.dma("sp", xt[:], x[ti * 128:(ti + 1) * 128, :], writes=[xk])
                    for c4 in range(4):
                        p = (ti * 4 + c4) % 8
                        for cc in range(4):
                            c = c4 * 4 + cc
                            S.op("pe", lambda c=c, cc=cc, p=p, xt=xt: nc.tensor.transpose(
                                ps[p][:, cc * 128:(cc + 1) * 128], xt[:, c * 128:(c + 1) * 128], ident[:]),
                                reads=[xk], writes=[pk[p]], inc=(cc == 3))
                        eng = "act" if c4 % 2 == 0 else "dve"
                        src = ps[p][:].rearrange("p (c t) -> p c t", c=4)
                        dst = sg[:, c4 * 4:(c4 + 1) * 4, j * 128:(j + 1) * 128]
                        if eng == "act":
                            S.op("act", lambda dst=dst, src=src: nc.scalar.copy(dst, src), reads=[pk[p]], writes=[sgk])
                        else:
                            S.op("dve", lambda dst=dst, src=src: nc.vector.tensor_copy(dst, src), reads=[pk[p]], writes=[sgk])
                S.dma("sp", hT[:, tb * 512:(tb + 1) * 512].rearrange("(c p) t -> p c t", p=128), sg[:],
                      reads=[sgk], writes=[("hT", tb)])
            S.barrier()

        def rmsnorm(ht, htk, sq, sqk, rstd, rstdk, dst_fn, dstk, gidx, pbank):
            S.op("act", lambda: nc.scalar.activation(sq[:], ht[:], AF.Square), reads=[htk], writes=[sqk])
            for c in range(16):
                S.op("pe", lambda c=c: nc.tensor.matmul(ps[pbank][:], onesb[:], sq[:, c, :], start=(c == 0), stop=(c == 15)),
                     reads=[sqk], writes=[pk[pbank]], inc=(c == 15))
            S.op("act", lambda: nc.scalar.activation(rstd[:], ps[pbank][:], AF.Sqrt, bias=EPS, scale=1.0 / D),
                 reads=[pk[pbank]], writes=[rstdk])
            S.op("dve", lambda: nc.vector.reciprocal(rstd[:], rstd[:]), reads=[rstdk], writes=[rstdk])
            for c in range(16):
                S.op("dve", lambda c=c: nc.vector.scalar_tensor_tensor(
                    dst_fn(c), ht[:, c, :], gsb[:, gidx * 16 + c:gidx * 16 + c + 1], rstd[:], ALU.mult, ALU.mult),
                    reads=[htk, rstdk], writes=[dstk])

        def proj_rot(wb, wbk, hn, hnk, t0, ctab, stab, tabk, rot, rotk, pb):
            for xx in range(2):
                for kc in range(16):
                    S.op("pe", lambda xx=xx, kc=kc: nc.tensor.matmul(
                        ps[pb + xx][:], wb[:, kc, xx * 128:(xx + 1) * 128], hn[:, kc, t0:t0 + 512],
                        start=(kc == 0), stop=(kc == 15)),
                        reads=[wbk, hnk], writes=[pk[pb + xx]], inc=(kc == 15))
            return

        for l in range(depth):
            li = l // 2
            is_attn = (l % 2 == 0)
            with contextlib.ExitStack() as st:
                TBA = 1024
                ht = sb(st, "p_ht", [128, 16, 512], F32)
                sq = sb(st, "p_sq", [128, 16, 512], BF16)
                rstd = sb(st, "p_rstd", [128, 512], F32)
                hn = sb(st, "p_hn", [128, 16, TBA], BF16)
                ctab = sb(st, "p_ct", [128, TBA], F32)
                stab = sb(st, "p_st", [128, TBA], F32)
                wbs = [sb(st, f"p_wb{i}", [128, 16, 256], BF16) for i in range(3)]
                wvs = [sb(st, f"p_wv{i}", [128, 16, 512], BF16) for i in range(2)]
                rots = [sb(st, f"p_rot{i}", [128, 2, TBA], BF16) for i in range(2)]
                tmp = [sb(st, f"p_tmp{i}", [128, 4, 512], BF16) for i in range(2)]
                vst = [sb(st, f"p_vst{i}", [128, 512], BF16) for i in range(3)]
                c_in, s_in = (ca_in, sa_in) if is_attn else (cr_in, sr_in)
                wctr = 0
                vctr = 0
                rctr = 0
                tctr = 0
                sctr = 0
                for tb in range(T // TBA):
                    tok0 = tb * TBA
                    S.dma("sp", ctab[:], c_in[:, tok0:tok0 + TBA], writes=["ctab"])
                    S.dma("sp", stab[:], s_in[:, tok0:tok0 + TBA], writes=["stab"])
                    for hf in range(TBA // 512):
                        t0 = tok0 + hf * 512
                        S.dma("sp", ht[:], hT[:, t0:t0 + 512].rearrange("(c p) t -> p c t", p=128),
                              reads=[("hT", t0 // 512)], writes=["ht"])
                        rmsnorm(ht, "ht", sq, "sq", rstd, "rstd",
                                lambda c, hf=hf: hn[:, c, hf * 512:(hf + 1) * 512], "hn", l, 7)
                    if is_attn:
                        tiles = [(wqk_a[li, (g * 2 + ty) * 8 + hp], qk_a[g, ty, hp]) for g in range(3) for ty in range(2) for hp in range(8)]
                    else:
                        tiles = [(wqk_r[li, ty * 8 + h], qk_r[ty, h]) for ty in range(2) for h in range(8)]
                    for (wsrc, qdst) in tiles:
                        wb = wbs[wctr % 3]
                        wbk = ("wb", wctr % 3)
                        wctr += 1
                        S.dma("sp", wb[:], wsrc, writes=[wbk])
                        rot = rots[rctr % 2]
                        rotk = ("rot", rctr % 2)
                        rctr += 1
                        for hf in range(TBA // 512):
                            pb = (tctr % 3) * 2
                            tm = tmp[tctr % 2]
                            tmk = ("tmp", tctr % 2)
                            tctr += 1
                            for xx in range(2):
                                for kc in range(16):
                                    S.op("pe", lambda xx=xx, kc=kc, wb=wb, pb=pb, hf=hf: nc.tensor.matmul(
                                        ps[pb + xx][:], wb[:, kc, xx * 128:(xx + 1) * 128], hn[:, kc, hf * 512:(hf + 1) * 512],
                                        start=(kc == 0), stop=(kc == 15)),
                                        reads=[wbk, "hn"], writes=[pk[pb + xx]], inc=(kc == 15))
                            cs = ctab[:, hf * 512:(hf + 1) * 512]
                            ss = stab[:, hf * 512:(hf + 1) * 512]
                            X1, X2 = ps[pb][:], ps[pb + 1][:]
                            for i, (a, b) in enumerate(((X1, cs), (X2, ss), (X1, ss), (X2, cs))):
                                S.op("dve", lambda i=i, a=a, b=b, tm=tm: nc.vector.tensor_tensor(tm[:, i, :], a, b, ALU.mult),
                                     reads=[pk[pb], pk[pb + 1], "ctab", "stab"], writes=[tmk])
                            S.op("pool", lambda tm=tm, rot=rot, hf=hf: nc.gpsimd.tensor_tensor(
                                rot[:, 0, hf * 512:(hf + 1) * 512], tm[:, 0, :], tm[:, 1, :], ALU.subtract),
                                reads=[tmk], writes=[rotk])
                            S.op("pool", lambda tm=tm, rot=rot, hf=hf: nc.gpsimd.tensor_tensor(
                                rot[:, 1, hf * 512:(hf + 1) * 512], tm[:, 2, :], tm[:, 3, :], ALU.add),
                                reads=[tmk], writes=[rotk])
                        S.dma("sp", qdst[:, :, tok0:tok0 + TBA].rearrange("r p t -> p r t"), rot[:],
                              reads=[rotk], writes=[("qk", tb)])
                    if is_attn:
                        vt = [(wv_a[li, g * 4 + cbk], v_a[g], cbk * 512, False) for g in range(3) for cbk in range(4)]
                    else:
                        vt = [(wvg_r[li, b], v_r, b * 512, False) for b in range(8)] + \
                             [(wvg_r[li, 8 + b], sg_r, b * 512, True) for b in range(8)]
                    for (wsrc, vdst, c0, silu) in vt:
                        wv = wvs[vctr % 2]
                        wvk = ("wv", vctr % 2)
                        vctr += 1
                        S.dma("sp", wv[:], wsrc, writes=[wvk])
                        for tt in range(TBA // 128):
                            pb = 6 + (sctr % 2)
                            vs = vst[sctr % 3]
                            vsk = ("vst", sctr % 3)
                            sctr += 1
                            for kc in range(16):
                                S.op("pe", lambda kc=kc, wv=wv, pb=pb, tt=tt: nc.tensor.matmul(
                                    ps[pb][:], hn[:, kc, tt * 128:(tt + 1) * 128], wv[:, kc, :],
                                    start=(kc == 0), stop=(kc == 15)),
                                    reads=[wvk, "hn"], writes=[pk[pb]], inc=(kc == 15))
                            fn = AF.Silu if silu else AF.Copy
                            S.op("act", lambda vs=vs, pb=pb, fn=fn: nc.scalar.activation(vs[:], ps[pb][:], fn),
                                 reads=[pk[pb]], writes=[vsk])
                            S.dma("sp", vdst[tok0 + tt * 128:tok0 + (tt + 1) * 128, c0:c0 + 512], vs[:],
                                  reads=[vsk], writes=[("v", tb)])
                S.barrier()

            if is_attn:
                with contextlib.ExitStack() as st:
                    acc = [sb(st, f"a_acc{e}", [128, 2, QB], F32) for e in range(2)]
                    qt = [sb(st, f"a_q{i}", [128, 2, QB], BF16) for i in range(2)]
                    kt = [sb(st, f"a_k{i}", [128, 2, 2 * QB], BF16) for i in range(2)]
                    vt_ = [sb(st, f"a_v{i}", [128, 32, 256], BF16) for i in range(2)]
                    pts = [sb(st, f"a_pt{i}", [128, 2, 128], BF16) for i in range(4)]
                    rec = sb(st, "a_rec", [128, QB], F32)
                    osb = [sb(st, f"a_o{i}", [128, QB], BF16) for i in range(2)]
                    lctr = 0
                    bctr = 0
                    octr = 0
                    scale = 128.0 ** -0.5
                    for hp in range(8):
                        for sbk in range(T // QB):
                            q0 = sbk * QB
                            for g, (win, d) in enumerate(A_PAT):
                                span = 128 * d
                                halo = span if sbk > 0 else 0
                                bi = lctr % 2
                                lctr += 1
                                qk_, kk_, vk_ = ("aq", bi), ("ak", bi), ("av", bi)
                                S.dma("sp", qt[bi][:], qk_a[g, 0, hp][:, :, q0:q0 + QB].rearrange("r p t -> p r t"),
                                      writes=[qk_])
                                S.dma("sp", kt[bi][:, :, 0:halo + QB],
                                      qk_a[g, 1, hp][:, :, q0 - halo:q0 + QB].rearrange("r p t -> p r t"), writes=[kk_])
                                nrow = (halo + QB) // span
                                vsrc = v_a[g][q0 - halo:q0 + QB, hp * 256:(hp + 1) * 256].rearrange(
                                    "(n j r) c -> j n r c", j=128, r=d)
                                vdst = vt_[bi][:, 0:nrow * d, :].rearrange("j (n r) c -> j n r c", r=d)
                                for n_ in range(nrow):
                                    S.dma("sp", vdst[:, n_], vsrc[:, n_], writes=[vk_])
                                hrow = halo // span
                                for nl in range(QB // span):
                                    for r in range(d):
                                        for e in range(2):
                                            has_prev = (sbk > 0) or (nl > 0)
                                            pt = pts[bctr % 4]
                                            ptk = ("pt", bctr % 4)
                                            sp_ = bctr % 4
                                            np_ = 4 + bctr % 4
                                            bctr += 1
                                            qcols = slice(nl * span + r, nl * span + r + 127 * d + 1, d)
                                            kbs = ([0] if has_prev else []) + [1]
                                            stv = ps[sp_][:, 0:256].rearrange("p (k i) -> p k i", k=2)
                                            for kb in kbs:
                                                koff = (hrow + nl - 1 + kb) * span + r
                                                kcols = slice(koff, koff + 127 * d + 1, d)
                                                for R in range(2):
                                                    S.op("pe", lambda kb=kb, R=R, kcols=kcols, qcols=qcols, e=e, bi=bi, stv=stv: nc.tensor.matmul(
                                                        stv[:, kb, :], kt[bi][e * 64:(e + 1) * 64, R, kcols], qt[bi][e * 64:(e + 1) * 64, R, qcols],
                                                        start=(R == 0), stop=(R == 1)),
                                                        reads=[qk_, kk_], writes=[pk[sp_]], inc=(R == 1))
                                            k0 = kbs[0]
                                            S.op("act", lambda pt=pt, stv=stv, k0=k0: nc.scalar.activation(
                                                pt[:, k0:2, :], stv[:, k0:2, :], AF.Exp, scale=scale),
                                                reads=[pk[sp_]], writes=[ptk])
                                            S.op("pool", lambda pt=pt, k0=k0: nc.gpsimd.tensor_tensor(
                                                pt[:, k0:2, :], pt[:, k0:2, :],
                                                amask[:].rearrange("p (k i) -> p k i", k=2)[:, k0:2, :], ALU.mult),
                                                reads=[ptk, "amask"], writes=[ptk])
                                            ndv = ps[np_][:, 0:256].rearrange("p (k i) -> p k i", k=2)
                                            for idx, kb in enumerate(kbs):
                                                vrow = (hrow + nl - 1 + kb) * d + r
                                                S.op("pe", lambda kb=kb, vrow=vrow, e=e, bi=bi, pt=pt, ndv=ndv, idx=idx, kbs=kbs: nc.tensor.matmul(
                                                    ndv[:, 0, :], vt_[bi][:, vrow, e * 128:(e + 1) * 128], pt[:, kb, :],
                                                    start=(idx == 0), stop=(idx == len(kbs) - 1)),
                                                    reads=[vk_, ptk], writes=[pk[np_]], inc=False)
                                            for idx, kb in enumerate(kbs):
                                                S.op("pe", lambda kb=kb, pt=pt, ndv=ndv, idx=idx, kbs=kbs: nc.tensor.matmul(
                                                    ndv[:, 1, :], onesb[:], pt[:, kb, :],
                                                    start=(idx == 0), stop=(idx == len(kbs) - 1)),
                                                    reads=[ptk], writes=[pk[np_]], inc=(idx == len(kbs) - 1))
                                            av = acc[e][:, :, qcols]
                                            ak = ("acc", e)
                                            if g == 0:
                                                S.op("dve", lambda av=av, ndv=ndv: nc.vector.tensor_copy(av, ndv),
                                                     reads=[pk[np_]], writes=[ak])
                                            else:
                                                S.op("dve", lambda av=av, ndv=ndv: nc.vector.tensor_tensor(av, ndv, av, ALU.add),
                                                     reads=[pk[np_], ak], writes=[ak])
                            for e in range(2):
                                ak = ("acc", e)
                                ob = osb[octr % 2]
                                obk = ("osb", octr % 2)
                                octr += 1
                                S.op("dve", lambda e=e: nc.vector.reciprocal(rec[:], acc[e][:, 1, :]), reads=[ak], writes=["rec"])
                                S.op("dve", lambda e=e, ob=ob: nc.vector.tensor_tensor(ob[:], acc[e][:, 0, :], rec[:], ALU.mult),
                                     reads=[ak, "rec"], writes=[obk])
                                h = hp * 2 + e
                                S.dma("sp", oT[h * 128:(h + 1) * 128, q0:q0 + QB], ob[:], reads=[obk], writes=[("oT", 0)])
                    S.barrier()
            else:
                with contextlib.ExitStack() as st:
                    GT = 256
                    qs = [sb(st, f"r_q{i}", [128, 8, 2, GT], BF16) for i in range(2)]
                    ks = [sb(st, f"r_k{i}", [128, 8, 2, GT], BF16) for i in range(2)]
                    vs_ = [sb(st, f"r_v{i}", [128, 4096], BF16) for i in range(2)]
                    gs_ = [sb(st, f"r_g{i}", [128, 4096], BF16) for i in range(2)]
                    Sf = sb(st, "r_S", [128, 8, 2, 512], F32)
                    Sb_ = sb(st, "r_Sb", [128, 8, 2, 512], BF16)
                    pts = [sb(st, f"r_pt{i}", [128, 128], BF16) for i in range(3)]
                    kds = [sb(st, f"r_kd{i}", [128, 2, 128], BF16) for i in range(3)]
                    stt = [sb(st, f"r_st{i}", [128, 6], F32) for i in range(3)]
                    mv = [sb(st, f"r_mv{i}", [128, 2], F32) for i in range(3)]
                    rs = [sb(st, f"r_rs{i}", [128, 2], F32) for i in range(3)]
                    yn = [sb(st, f"r_yn{i}", [128, 512], BF16) for i in range(3)]
                    yg = [sb(st, f"r_yg{i}", [128, 4096], BF16) for i in range(2)]
                    ots = [sb(st, f"r_ot{i}", [128, 32, GT], BF16) for i in range(2)]
                    log_g = [float(np.log1p(-np.exp2(-5.0 - h))) for h in range(8)]
                    cdec = [float(np.exp(lg * 128.0)) for lg in log_g]
                    NCH = T // 128
                    c3 = 0
                    for grp in range(T // GT):
                        bi = grp % 2
                        t0 = grp * GT
                        S.dma("sp", qs[bi][:], qk_r[0][:, :, :, t0:t0 + GT].rearrange("h r p t -> p h r t"), writes=[("rq", bi)])
                        S.dma("sp", ks[bi][:], qk_r[1][:, :, :, t0:t0 + GT].rearrange("h r p t -> p h r t"), writes=[("rk", bi)])
                        for cl in range(GT // 128):
                            n = grp * (GT // 128) + cl
                            cols = slice(cl * 128, (cl + 1) * 128)
                            ygb = yg[n % 2]
                            ygk = ("yg", n % 2)
                            vi = n % 2
                            S.dma("sp", vs_[vi][:], v_r[n * 128:(n + 1) * 128, :], writes=[("rv", vi)])
                            S.dma("sp", gs_[vi][:], sg_r[n * 128:(n + 1) * 128, :], writes=[("rg", vi)])
                            for h in range(8):
                                i3 = c3 % 3
                                c3 += 1
                                p_st, p_y, p_su = i3, 3 + (c3 % 2), 5 + (c3 % 2)
                                stv = ps[p_st][:, 0:128]
                                for R in range(2):
                                    S.op("pe", lambda R=R, h=h, bi=bi, cols=cols, stv=stv: nc.tensor.matmul(
                                        stv, ks[bi][:, h, R, cols], qs[bi][:, h, R, cols], start=(R == 0), stop=(R == 1)),
                                        reads=[("rq", bi), ("rk", bi)], writes=[pk[p_st]], inc=(R == 1))
                                pt = pts[i3]
                                S.op("dve", lambda pt=pt, stv=stv, h=h: nc.vector.scalar_tensor_tensor(
                                    pt[:], stv, rconst[:, h:h + 1], rmask[:], ALU.mult, ALU.mult),
                                    reads=[pk[p_st], "rconst", "rmask"], writes=[("rpt", i3)])
                                vh = vs_[vi][:, h * 512:(h + 1) * 512]
                                yv = ps[p_y][:]
                                S.op("pe", lambda pt=pt, vh=vh, yv=yv, n=n: nc.tensor.matmul(yv, pt[:], vh, start=True, stop=(n == 0)),
                                     reads=[("rpt", i3), ("rv", vi)], writes=[pk[p_y]], inc=(n == 0))
                                if n > 0:
                                    for R in range(2):
                                        S.op("pe", lambda R=R, h=h, bi=bi, cols=cols, yv=yv: nc.tensor.matmul(
                                            yv, qs[bi][:, h, R, cols], Sb_[:, h, R, :], start=False, stop=(R == 1)),
                                            reads=[("rq", bi), ("Sb", h)], writes=[pk[p_y]], inc=(R == 1))
                                S.op("dve", lambda yv=yv, i3=i3: nc.vector.bn_stats(stt[i3][:], yv), reads=[pk[p_y]], writes=[("stt", i3)])
                                S.op("dve", lambda i3=i3: nc.vector.bn_aggr(mv[i3][:], stt[i3][:]), reads=[("stt", i3)], writes=[("mv", i3)])
                                S.op("act", lambda i3=i3, h=h: nc.scalar.activation(
                                    rs[i3][:, 0:1], mv[i3][:, 1:2], AF.Sqrt, bias=rconst[:, 16 + h:17 + h], scale=1.0),
                                    reads=[("mv", i3), "rconst"], writes=[("rs", i3)])
                                S.op("dve", lambda i3=i3: nc.vector.reciprocal(rs[i3][:, 0:1], rs[i3][:, 0:1]),
                                     reads=[("rs", i3)], writes=[("rs", i3)])
                                S.op("dve", lambda i3=i3: nc.vector.scalar_tensor_tensor(
                                    rs[i3][:, 1:2], mv[i3][:, 0:1], -1.0, rs[i3][:, 0:1], ALU.mult, ALU.mult),
                                    reads=[("rs", i3), ("mv", i3)], writes=[("rs", i3)])
                                S.op("act", lambda i3=i3, yv=yv: nc.scalar.activation(
                                    yn[i3][:], yv, AF.Identity, bias=rs[i3][:, 1:2], scale=rs[i3][:, 0:1]),
                                    reads=[pk[p_y], ("rs", i3)], writes=[("yn", i3)])
                                S.op("pool", lambda i3=i3, h=h, ygb=ygb, vi=vi: nc.gpsimd.tensor_tensor(
                                    ygb[:, h * 512:(h + 1) * 512], yn[i3][:], gs_[vi][:, h * 512:(h + 1) * 512], ALU.mult),
                                    reads=[("yn", i3), ("rg", vi)], writes=[ygk])
                                if n < NCH - 1:
                                    ktv = ps[p_st][:, 256:384].bitcast(BF16).rearrange("p (r t) -> p r t", r=2)
                                    for R in range(2):
                                        S.op("pe", lambda R=R, h=h, bi=bi, cols=cols, ktv=ktv: nc.tensor.transpose(
                                            ktv[:, R, :], ks[bi][:, h, R, cols], identb[:]),
                                            reads=[("rk", bi)], writes=[pk[p_st]], inc=(R == 1))
                                    kd = kds[i3]
                                    S.op("act", lambda kd=kd, ktv=ktv, h=h: nc.scalar.activation(
                                        kd[:], ktv, AF.Copy, scale=rconst[:, 8 + h:9 + h]),
                                        reads=[pk[p_st], "rconst"], writes=[("kd", i3)])
                                    for c in range(2):
                                        S.op("pe", lambda c=c, kd=kd, vh=vh: nc.tensor.matmul(
                                            ps[p_su][:], kd[:, c, :], vh, start=True, stop=True),
                                            reads=[("kd", i3), ("rv", vi)], writes=[pk[p_su]], inc=True)
                                        if n == 0:
                                            S.op("dve", lambda c=c, h=h: nc.vector.tensor_copy(Sf[:, h, c, :], ps[p_su][:]),
                                                 reads=[pk[p_su]], writes=[("Sf", h)])
                                        else:
                                            S.op("dve", lambda c=c, h=h: nc.vector.scalar_tensor_tensor(
                                                Sf[:, h, c, :], Sf[:, h, c, :], cdec[h], ps[p_su][:], ALU.mult, ALU.add),
                                                reads=[pk[p_su], ("Sf", h)], writes=[("Sf", h)])
                                    S.op("act", lambda h=h: nc.scalar.copy(Sb_[:, h, :, :], Sf[:, h, :, :]),
                                         reads=[("Sf", h)], writes=[("Sb", h)])
                            ot = ots[bi]
                            otk = ("ots", bi)
                            for f8 in range(4):
                                pb = 7
                                tv = ps[pb][:].bitcast(BF16).rearrange("p (f t) -> p f t", f=8)
                                for ff in range(8):
                                    f = f8 * 8 + ff
                                    S.op("pe", lambda f=f, ff=ff, tv=tv, ygb=ygb: nc.tensor.transpose(
                                        tv[:, ff, :], ygb[:, f * 128:(f + 1) * 128], identb[:]),
                                        reads=[ygk], writes=[pk[pb]], inc=(ff == 7))
                                if f8 % 2 == 0:
                                    S.op("act", lambda f8=f8, tv=tv, ot=ot, cols=cols: nc.scalar.copy(ot[:, f8 * 8:(f8 + 1) * 8, cols], tv),
                                         reads=[pk[pb]], writes=[otk])
                                else:
                                    S.op("dve", lambda f8=f8, tv=tv, ot=ot, cols=cols: nc.vector.tensor_copy(ot[:, f8 * 8:(f8 + 1) * 8, cols], tv),
                                         reads=[pk[pb]], writes=[otk])
                        S.dma("sp", oT[:, t0:t0 + GT].rearrange("(f p) t -> p f t", p=128), ots[bi][:], reads=[("ots", bi)], writes=[("oT", 0)])
                    S.barrier()

            with contextlib.ExitStack() as st:
                KO = 16 if is_attn else 32
                wo_src = wo_a[li] if is_attn else wo_r[li]
                ht = sb(st, "f_ht", [128, 16, 512], F32)
                rstd = sb(st, "f_rstd", [128, 512], F32)
                hn = sb(st, "f_hn", [128, 16, 512], BF16)
                actb = sb(st, "f_act", [128, NFF, 512], BF16)
                ot = actb
                sq = actb[:, 28:44, :]
                wos = [sb(st, f"f_wo{i}", [128, KO, 128], BF16) for i in range(2)]
                wus = [sb(st, f"f_wu{i}", [128, 2, 16, 128], BF16) for i in range(2)]
                wds = [sb(st, f"f_wd{i}", [128, NFF, 128], BF16) for i in range(2)]
                ug = [sb(st, f"f_ug{i}", [128, 2, 514], F32) for i in range(2)]
                cg = [sb(st, f"f_cg{i}", [128, 2, 512], F32) for i in range(2)]
                sgt = [sb(st, f"f_sg{i}", [128, 512], F32) for i in range(2)]
                utail = sb(st, "f_utail", [128, 88, 2], F32)
                S.op("pool", lambda: nc.gpsimd.memset(utail[:], 0.0), writes=["utail"])
                cwv = cw[:].rearrange("p (l j w) -> p l j w", l=4, j=88)
                cbv = cb[:].rearrange("p (l j) -> p l j", l=4)
                woc = 0
                wuc = 0
                wdc = 0
                uc = 0
                for tb in range(NTB):
                    t0 = tb * 512
                    S.dma("sp", ht[:], hT[:, t0:t0 + 512].rearrange("(c p) t -> p c t", p=128),
                          reads=[("hT", tb)], writes=["ht"])
                    S.dma("sp", ot[:, 0:KO, :], oT[0:KO * 128, t0:t0 + 512].rearrange("(c p) t -> p c t", p=128),
                          reads=[("oT", 0)], writes=["actb"])
                    for m in range(16):
                        wo = wos[woc % 2]
                        wok = ("wo", woc % 2)
                        woc += 1
                        S.dma("sp", wo[:], wo_src[m], writes=[wok])
                        pb = m % 2
                        for kc in range(KO):
                            S.op("pe", lambda kc=kc, wo=wo, pb=pb: nc.tensor.matmul(
                                ps[pb][:], wo[:, kc, :], ot[:, kc, :], start=(kc == 0), stop=(kc == KO - 1)),
                                reads=[wok, "actb"], writes=[pk[pb]], inc=(kc == KO - 1))
                        S.op("dve", lambda m=m, pb=pb: nc.vector.tensor_tensor(ht[:, m, :], ps[pb][:], ht[:, m, :], ALU.add),
                             reads=[pk[pb], "ht"], writes=["ht"])
                    rmsnorm(ht, "ht", sq, "actb", rstd, "rstd", lambda c: hn[:, c, :], "hn", 4 + l, 7)
                    for j in range(NFF):
                        wu = wus[wuc % 2]
                        wuk = ("wu", wuc % 2)
                        wuc += 1
                        S.dma("sp", wu[:, 0], wup_b[l, j], writes=[wuk])
                        S.dma("sp", wu[:, 1], wup_b[l, NFF + j], writes=[wuk])
                        u = ug[uc % 2]
                        uk = ("ug", uc % 2)
                        c_ = cg[uc % 2]
                        ck = ("cg", uc % 2)
                        sg_ = sgt[uc % 2]
                        sk = ("sgt", uc % 2)
                        pb = 2 + (uc % 2) * 2
                        uc += 1
                        for s in range(2):
                            for kc in range(16):
                                S.op("pe", lambda s=s, kc=kc, wu=wu, pb=pb: nc.tensor.matmul(
                                    ps[pb + s][:], wu[:, s, kc, :], hn[:, kc, :], start=(kc == 0), stop=(kc == 15)),
                                    reads=[wuk, "hn"], writes=[pk[pb + s]], inc=(kc == 15))
                        for s in range(2):
                            jj = s * NFF + j
                            S.op("pool", lambda s=s, jj=jj, u=u: nc.gpsimd.tensor_copy(u[:, s, 0:2], utail[:, jj, :]),
                                 reads=["utail"], writes=[uk])
                            S.op("act", lambda s=s, u=u, pb=pb: nc.scalar.copy(u[:, s, 2:514], ps[pb + s][:]),
                                 reads=[pk[pb + s]], writes=[uk])
                            S.op("act", lambda s=s, jj=jj, c_=c_, pb=pb: nc.scalar.activation(
                                c_[:, s, :], ps[pb + s][:], AF.Identity, bias=cbv[:, l, jj:jj + 1], scale=cwv[:, l, jj, 2:3]),
                                reads=[pk[pb + s], "cw", "cb"], writes=[ck])
                            S.op("pool", lambda s=s, jj=jj, u=u: nc.gpsimd.tensor_copy(utail[:, jj, :], u[:, s, 512:514]),
                                 reads=[uk], writes=["utail"])
                            for w_ in range(2):
                                S.op("dve", lambda s=s, jj=jj, c_=c_, u=u, w_=w_: nc.vector.scalar_tensor_tensor(
                                    c_[:, s, :], u[:, s, w_:w_ + 512], cwv[:, l, jj, w_:w_ + 1], c_[:, s, :], ALU.mult, ALU.add),
                                    reads=[uk, ck, "cw"], writes=[ck])
                        S.op("act", lambda c_=c_, sg_=sg_: nc.scalar.activation(sg_[:], c_[:, 0, :], AF.Silu), reads=[ck], writes=[sk])
                        S.op("pool", lambda j=j, c_=c_, sg_=sg_: nc.gpsimd.tensor_tensor(actb[:, j, :], sg_[:], c_[:, 1, :], ALU.mult),
                             reads=[ck, sk], writes=["actb"])
                    for m in range(16):
                        wd = wds[wdc % 2]
                        wdk = ("wd", wdc % 2)
                        wdc += 1
                        S.dma("sp", wd[:], wdn_b[l, m], writes=[wdk])
                        pb = m % 2
                        for kc in range(NFF):
                            S.op("pe", lambda kc=kc, wd=wd, pb=pb: nc.tensor.matmul(
                                ps[pb][:], wd[:, kc, :], actb[:, kc, :], start=(kc == 0), stop=(kc == NFF - 1)),
                                reads=[wdk, "actb"], writes=[pk[pb]], inc=(kc == NFF - 1))
                        S.op("dve", lambda m=m, pb=pb: nc.vector.tensor_tensor(ht[:, m, :], ps[pb][:], ht[:, m, :], ALU.add),
                             reads=[pk[pb], "ht"], writes=["ht"])
                    S.dma("sp", hT[:, t0:t0 + 512].rearrange("(c p) t -> p c t", p=128), ht[:],
                          reads=["ht"], writes=[("hT", tb)])
                S.barrier()

        with contextlib.ExitStack() as st:
            ht = sb(st, "z_ht", [128, 16, 512], F32)
            sq = sb(st, "z_sq", [128, 16, 512], BF16)
            rstd = sb(st, "z_rstd", [128, 512], F32)
            hn = sb(st, "z_hn", [128, 16, 512], F32)
            ob = [sb(st, f"z_ob{i}", [128, D], F32) for i in range(2)]
            oc = 0
            pc = 0
            for tb in range(NTB):
                t0 = tb * 512
                S.dma("sp", ht[:], hT[:, t0:t0 + 512].rearrange("(c p) t -> p c t", p=128),
                      reads=[("hT", tb)], writes=["ht"])
                rmsnorm(ht, "ht", sq, "sq", rstd, "rstd", lambda c: hn[:, c, :], "hn", 8, 7)
                for j in range(4):
                    o_ = ob[oc % 2]
                    ok_ = ("ob", oc % 2)
                    oc += 1
                    for c4 in range(4):
                        p = pc % 6
                        pc += 1
                        for cc in range(4):
                            c = c4 * 4 + cc
                            S.op("pe", lambda c=c, cc=cc, p=p, j=j: nc.tensor.transpose(
                                ps[p][:, cc * 128:(cc + 1) * 128], hn[:, c, j * 128:(j + 1) * 128], ident[:]),
                                reads=["hn"], writes=[pk[p]], inc=(cc == 3))
                        if c4 % 2 == 0:
                            S.op("act", lambda o_=o_, p=p, c4=c4: nc.scalar.copy(o_[:, c4 * 512:(c4 + 1) * 512], ps[p][:]),
                                 reads=[pk[p]], writes=[ok_])
                        else:
                            S.op("dve", lambda o_=o_, p=p, c4=c4: nc.vector.tensor_copy(o_[:, c4 * 512:(c4 + 1) * 512], ps[p][:]),
                                 reads=[pk[p]], writes=[ok_])
                    S.dma("sp", out[t0 + j * 128:t0 + (j + 1) * 128, :], o_[:], reads=[ok_], writes=[("out", 0)])
            S.barrier()
    return nc


def host_consts(T):
    pos = np.arange(T, dtype=np.float32)
    inv_a = (10000.0 ** (-np.arange(0, 128, 2, dtype=np.float32) / 128.0)).astype(np.float32)
    ang_a = (pos[None, :] * inv_a[:, None]).astype(np.float32)
    ca = np.concatenate([np.cos(ang_a), np.cos(ang_a)], 0).astype(np.float32)
    sa = np.concatenate([np.sin(ang_a), np.sin(ang_a)], 0).astype(np.float32)
    inv_r = (10000.0 ** (-np.linspace(0.0, 1.0, 128, dtype=np.float32))).astype(np.float32)
    ang_r = (pos[None, :] * inv_r[:, None]).astype(np.float32)
    cr, sr = np.cos(ang_r).astype(np.float32), np.sin(ang_r).astype(np.float32)
    j = np.arange(128)[:, None]
    i = np.arange(128)[None, :]
    amask = np.concatenate([(j >= i), (j <= i)], 1).astype(np.float32)
    rmask = (i >= j).astype(np.float32)
    log_g = np.log1p(-np.exp2(-5.0 - np.arange(8, dtype=np.float64)))
    jj = np.arange(128, dtype=np.float64)[:, None]
    rconst = np.zeros((128, 24), np.float64)
    rconst[:, 0:8] = np.exp(-log_g[None, :] * (jj + 1.0)) * 0.0625
    rconst[:, 8:16] = np.exp(log_g[None, :] * (127.0 - jj)) * 0.0625
    rconst[:, 16:24] = EPS * np.exp(-2.0 * log_g[None, :] * (jj + 1.0))
    return dict(ca=ca, sa=sa, cr=cr, sr=sr, amask=amask, rmask=rmask,
                rconst=rconst.astype(np.float32), ident=np.eye(128, dtype=np.float32))


def make_in_maps(inputs, T, n_cores=8):
    f = lambda a: np.ascontiguousarray(np.asarray(a, dtype=np.float32))
    hc = host_consts(T)
    gv = np.concatenate([f(inputs["norm_mix"]), f(inputs["norm_ffn"]), f(inputs["norm_final"])[None]], 0)
    gains = np.ascontiguousarray(gv.reshape(9, 16, 128).transpose(2, 0, 1).reshape(128, 144))
    cw = np.ascontiguousarray(f(inputs["conv_w"]).reshape(4, 3, 88, 128).transpose(3, 0, 2, 1).reshape(128, -1))
    cb = np.ascontiguousarray(f(inputs["conv_b"]).reshape(4, 88, 128).transpose(2, 0, 1).reshape(128, -1))
    x = f(inputs["x"])
    B = x.shape[0]
    shared = dict(gains=gains, cw=cw, cb=cb, **hc)
    for k in ("w_in_attn", "w_out_attn", "w_in_ret", "w_out_ret", "w_up", "w_down"):
        shared[k] = f(inputs[k])
    maps = []
    for c in range(n_cores):
        m = dict(shared)
        m["x"] = np.ascontiguousarray(x[c % B, :T])
        maps.append(m)
    return maps


_NC_CACHE = {}


def kernel(**inputs):
    T, depth = 8192, 4
    key = (T, depth)
    if key not in _NC_CACHE:
        _NC_CACHE[key] = build(T, depth)
    nc = _NC_CACHE[key]
    maps = make_in_maps(inputs, T)
    res = run_bass_kernel_spmd(nc, maps, core_ids=list(range(8)))
    B = np.asarray(inputs["x"]).shape[0]
    return np.stack([np.asarray(res.results[b]["out"], dtype=np.float32) for b in range(B)], 0)
```

```python
import contextlib
import numpy as np
import ml_dtypes
import concourse.bass as bass
import concourse.mybir as mybir
from concourse.bass_utils import run_bass_kernel_spmd

F32 = mybir.dt.float32
BF16 = mybir.dt.bfloat16
AF = mybir.ActivationFunctionType
ALU = mybir.AluOpType

D = 2048
DFF = 5632
NFF = DFF // 128
EPS = 1e-6
A_PAT = ((128, 1), (512, 4), (2048, 16))
A_PROJ = 18432
R_PROJ = 12288
QB = 2048


class Eng:
    def __init__(self, name, h, sem):
        self.name, self.h, self.sem = name, h, sem
        self.count = 0
        self.seen = {}


class Slot:
    def __init__(self, sem):
        self.sem = sem
        self.count = 0


class Sched:
    def __init__(self, nc, es):
        self.nc = nc
        sem = lambda n: es.enter_context(nc.semaphore(n))
        self.E = {
            "pe": Eng("pe", nc.tensor, sem("s_pe")),
            "act": Eng("act", nc.scalar, sem("s_act")),
            "dve": Eng("dve", nc.vector, sem("s_dve")),
            "pool": Eng("pool", nc.gpsimd, sem("s_pool")),
            "sp": Eng("sp", nc.sync, sem("s_sp")),
        }
        self.rings = {
            "sp": [Slot(sem(f"d_sp{i}")) for i in range(40)],
            "pool": [Slot(sem(f"d_pl{i}")) for i in range(32)],
            "act": [Slot(sem(f"d_ac{i}")) for i in range(12)],
        }
        self.rpos = {"sp": 0, "pool": 0, "act": 0}
        self.lw = {}
        self.rd = {}

    def _wait(self, E, dep):
        sem, v, owner = dep
        if v <= 0:
            return
        if owner == E.name:
            if E.name == "pe":
                return
            if v <= E.count - 3:
                return
        if E.seen.get(sem.name, 0) >= v:
            return
        E.h.wait_ge(sem, v)
        E.seen[sem.name] = v

    def _deps(self, E, reads, writes):
        for k in reads:
            if k in self.lw:
                self._wait(E, self.lw[k])
        for k in writes:
            if k in self.lw:
                self._wait(E, self.lw[k])
            for d in self.rd.get(k, {}).values():
                self._wait(E, d)

    def _stamp(self, stamp, reads, writes):
        for k in writes:
            self.lw[k] = stamp
            self.rd[k] = {}
        for k in reads:
            self.rd.setdefault(k, {})[stamp[0].name] = stamp

    def op(self, eng, emit, reads=(), writes=(), inc=True):
        E = self.E[eng]
        self._deps(E, reads, writes)
        ins = emit()
        if inc:
            E.count += 1
            ins.then_inc(E.sem, 1)
            stamp = (E.sem, E.count, eng)
        else:
            stamp = (E.sem, E.count + 1, eng)
        self._stamp(stamp, reads, writes)
        return ins

    def dma(self, q, out, in_, reads=(), writes=()):
        Q = self.E[q]
        ring = self.rings[q]
        slot = ring[self.rpos[q] % len(ring)]
        self.rpos[q] += 1
        self._wait(Q, (slot.sem, slot.count, None))
        self._deps(Q, reads, writes)
        ins = Q.h.dma_start(out=out, in_=in_)
        ins.then_inc(slot.sem, 16)
        slot.count += 16
        self._stamp((slot.sem, slot.count, None), reads, writes)
        return ins

    def barrier(self):
        for E in self.E.values():
            for F in self.E.values():
                if F is not E:
                    self._wait(E, (F.sem, F.count, F.name))
            for ring in self.rings.values():
                for s in ring:
                    self._wait(E, (s.sem, s.count, None))
        self.lw = {}
        self.rd = {}


def build(T, depth):
    nc = bass.Bass("TRN2", target_bir_lowering=False)
    NTB = T // 512
    n_attn = (depth + 1) // 2
    n_ret = depth // 2

    def din(name, shape, dt=F32):
        return nc.dram_tensor(name, list(shape), dt, kind="ExternalInput").ap()

    def dscr(name, shape, dt):
        return nc.dram_tensor(name, list(shape), dt).ap()

    x = din("x", [T, D])
    gains = din("gains", [128, 9 * 16])
    ident_in = din("ident", [128, 128])
    ca_in, sa_in = din("ca", [128, T]), din("sa", [128, T])
    cr_in, sr_in = din("cr", [128, T]), din("sr", [128, T])
    amask_in = din("amask", [128, 256])
    rmask_in = din("rmask", [128, 128])
    rconst_in = din("rconst", [128, 24])
    cw_in = din("cw", [128, 4 * 88 * 3])
    cb_in = din("cb", [128, 4 * 88])
    w_in_attn = din("w_in_attn", [2, D, A_PROJ])
    w_out_attn = din("w_out_attn", [2, D, D])
    w_in_ret = din("w_in_ret", [2, D, R_PROJ])
    w_out_ret = din("w_out_ret", [2, 4096, D])
    w_up = din("w_up", [4, D, 2 * DFF])
    w_down = din("w_down", [4, DFF, D])
    out = nc.dram_tensor("out", [T, D], F32, kind="ExternalOutput").ap()

    hT = dscr("hT", [D, T], F32)
    oT = dscr("oT", [4096, T], BF16)
    wqk_a = dscr("wqk_a", [max(n_attn, 1), 48, 128, 16, 256], BF16)
    wv_a = dscr("wv_a", [max(n_attn, 1), 12, 128, 16, 512], BF16)
    wo_a = dscr("wo_a", [max(n_attn, 1), 16, 128, 16, 128], BF16)
    wqk_r = dscr("wqk_r", [max(n_ret, 1), 16, 128, 16, 256], BF16)
    wvg_r = dscr("wvg_r", [max(n_ret, 1), 16, 128, 16, 512], BF16)
    wo_r = dscr("wo_r", [max(n_ret, 1), 16, 128, 32, 128], BF16)
    wup_b = dscr("wup_b", [max(depth, 1), 88, 128, 16, 128], BF16)
    wdn_b = dscr("wdn_b", [max(depth, 1), 16, 128, NFF, 128], BF16)
    qk_a = dscr("qk_a", [3, 2, 8, 2, 128, T], BF16)
    v_a = dscr("v_a", [3, T, D], BF16)
    qk_r = dscr("qk_r", [2, 8, 2, 128, T], BF16)
    v_r = dscr("v_r", [T, 4096], BF16)
    sg_r = dscr("sg_r", [T, 4096], BF16)

    es = contextlib.ExitStack()
    with es:
        S = Sched(nc, es)

        uid = [0]

        def sb(st, name, shape, dt):
            uid[0] += 1
            return st.enter_context(nc.sbuf_tensor(f"sb{uid[0]}_{name}", list(shape), dt))

        ps = [es.enter_context(nc.psum_tensor(f"ps{i}", [128, 512], F32)) for i in range(8)]
        pk = [("ps", i) for i in range(8)]

        ident = sb(es, "ident", [128, 128], F32)
        identb = sb(es, "identb", [128, 128], BF16)
        onesb = sb(es, "onesb", [128, 128], BF16)
        gsb = sb(es, "gsb", [128, 9 * 16], F32)
        amask = sb(es, "amask", [128, 256], BF16)
        rmask = sb(es, "rmask", [128, 128], F32)
        rconst = sb(es, "rconst", [128, 24], F32)
        cw = sb(es, "cw", [128, 4 * 88 * 3], F32)
        cb = sb(es, "cb", [128, 4 * 88], F32)
        with contextlib.ExitStack() as st:
            am32 = sb(st, "am32", [128, 256], F32)
            S.dma("sp", ident[:], ident_in, writes=["ident"])
            S.dma("sp", gsb[:], gains, writes=["gsb"])
            S.dma("sp", am32[:], amask_in, writes=["am32"])
            S.dma("sp", rmask[:], rmask_in, writes=["rmask"])
            S.dma("sp", rconst[:], rconst_in, writes=["rconst"])
            S.dma("sp", cw[:], cw_in, writes=["cw"])
            S.dma("sp", cb[:], cb_in, writes=["cb"])
            S.op("dve", lambda: nc.vector.tensor_copy(identb[:], ident[:]), reads=["ident"], writes=["identb"])
            S.op("dve", lambda: nc.vector.tensor_copy(amask[:], am32[:]), reads=["am32"], writes=["amask"])
            S.op("dve", lambda: nc.vector.memset(onesb[:], 1.0), writes=["onesb"])
            S.barrier()

        def cast(dst, src):
            S.dma("pool", dst, src)

        def kview(w, c0, n):
            return w[:, c0:c0 + n].rearrange("(kc p) c -> p kc c", p=128)

        for l in range(depth):
            li = l // 2
            if l % 2 == 0:
                w = w_in_attn[li]
                for g in range(3):
                    for ty in range(2):
                        for hp in range(8):
                            base = g * 6144 + ty * 2048 + hp * 256
                            blk = (g * 2 + ty) * 8 + hp
                            src = w[:, base:base + 256].rearrange("(kc p) (e x d) -> p kc x e d", p=128, e=2, x=2)
                            dstv = wqk_a[li, blk].rearrange("p kc (x e d) -> p kc x e d", x=2, e=2)
                            for xx in range(2):
                                for ee in range(2):
                                    cast(dstv[:, :, xx, ee], src[:, :, xx, ee])
                    for cbk in range(4):
                        cast(wv_a[li, g * 4 + cbk], kview(w, g * 6144 + 4096 + cbk * 512, 512))
                for m in range(16):
                    cast(wo_a[li, m], kview(w_out_attn[li], m * 128, 128))
            else:
                w = w_in_ret[li]
                for b in range(16):
                    cast(wqk_r[li, b], kview(w, b * 256, 256))
                for b in range(16):
                    cast(wvg_r[li, b], kview(w, 4096 + b * 512, 512))
                for m in range(16):
                    cast(wo_r[li, m], kview(w_out_ret[li], m * 128, 128))
            for j in range(88):
                cast(wup_b[l, j], kview(w_up[l], j * 128, 128))
            for m in range(16):
                cast(wdn_b[l, m], kview(w_down[l], m * 128, 128))

        with contextlib.ExitStack() as st:
            xs = [sb(st, f"xs{i}", [128, D], F32) for i in range(2)]
            stg = [sb(st, f"xstg{i}", [128, 16, 512], F32) for i in range(2)]
            for tb in range(NTB):
                sg = stg[tb % 2]
                sgk = ("xstg", tb % 2)
                for j in range(4):
                    ti = tb * 4 + j
                    xt = xs[ti % 2]
                    xk = ("xs", ti % 2)
                    S.dma("sp", xt[:], x[ti * 128:(ti + 1) * 128, :], writes=[xk])
                    for c4 in range(4):
                        p = (ti * 4 + c4) % 8
                        for cc in range(4):
                            c = c4 * 4 + cc
                            S.op("pe", lambda c=c, cc=cc, p=p, xt=xt: nc.tensor.transpose(
                                ps[p][:, cc * 128:(cc + 1) * 128], xt[:, c * 128:(c + 1) * 128], ident[:]),
                                reads=[xk], writes=[pk[p]], inc=(cc == 3))
                        eng = "act" if c4 % 2 == 0 else "dve"
                        src = ps[p][:].rearrange("p (c t) -> p c t", c=4)
                        dst = sg[:, c4 * 4:(c4 + 1) * 4, j * 128:(j + 1) * 128]
                        if eng == "act":
                            S.op("act", lambda dst=dst, src=src: nc.scalar.copy(dst, src), reads=[pk[p]], writes=[sgk])
                        else:
                            S.op("dve", lambda dst=dst, src=src: nc.vector.tensor_copy(dst, src), reads=[pk[p]], writes=[sgk])
                S.dma("sp", hT[:, tb * 512:(tb + 1) * 512].rearrange("(c p) t -> p c t", p=128), sg[:],
                      reads=[sgk], writes=[("hT", tb)])
            S.barrier()

        def rmsnorm(ht, htk, sq, sqk, rstd, rstdk, dst_fn, dstk, gidx, pbank):
            S.op("act", lambda: nc.scalar.activation(sq[:], ht[:], AF.Square), reads=[htk], writes=[sqk])
            for c in range(16):
                S.op("pe", lambda c=c: nc.tensor.matmul(ps[pbank][:], onesb[:], sq[:, c, :], start=(c == 0), stop=(c == 15)),
                     reads=[sqk], writes=[pk[pbank]], inc=(c == 15))
            S.op("act", lambda: nc.scalar.activation(rstd[:], ps[pbank][:], AF.Sqrt, bias=EPS, scale=1.0 / D),
                 reads=[pk[pbank]], writes=[rstdk])
            S.op("dve", lambda: nc.vector.reciprocal(rstd[:], rstd[:]), reads=[rstdk], writes=[rstdk])
            for c in range(16):
                S.op("dve", lambda c=c: nc.vector.scalar_tensor_tensor(
                    dst_fn(c), ht[:, c, :], gsb[:, gidx * 16 + c:gidx * 16 + c + 1], rstd[:], ALU.mult, ALU.mult),
                    reads=[htk, rstdk], writes=[dstk])

        def proj_rot(wb, wbk, hn, hnk, t0, ctab, stab, tabk, rot, rotk, pb):
            for xx in range(2):
                for kc in range(16):
                    S.op("pe", lambda xx=xx, kc=kc: nc.tensor.matmul(
                        ps[pb + xx][:], wb[:, kc, xx * 128:(xx + 1) * 128], hn[:, kc, t0:t0 + 512],
                        start=(kc == 0), stop=(kc == 15)),
                        reads=[wbk, hnk], writes=[pk[pb + xx]], inc=(kc == 15))
            return

        for l in range(depth):
            li = l // 2
            is_attn = (l % 2 == 0)
            with contextlib.ExitStack() as st:
                TBA = 1024
                ht = sb(st, "p_ht", [128, 16, 512], F32)
                sq = sb(st, "p_sq", [128, 16, 512], BF16)
                rstd = sb(st, "p_rstd", [128, 512], F32)
                hn = sb(st, "p_hn", [128, 16, TBA], BF16)
                ctab = sb(st, "p_ct", [128, TBA], F32)
                stab = sb(st, "p_st", [128, TBA], F32)
                wbs = [sb(st, f"p_wb{i}", [128, 16, 256], BF16) for i in range(3)]
                wvs = [sb(st, f"p_wv{i}", [128, 16, 512], BF16) for i in range(2)]
                rots = [sb(st, f"p_rot{i}", [128, 2, TBA], BF16) for i in range(2)]
                tmp = [sb(st, f"p_tmp{i}", [128, 4, 512], BF16) for i in range(2)]
                vst = [sb(st, f"p_vst{i}", [128, 512], BF16) for i in range(3)]
                c_in, s_in = (ca_in, sa_in) if is_attn else (cr_in, sr_in)
                if is_attn:
                    tiles = [(wqk_a[li, (g * 2 + ty) * 8 + hp], qk_a[g, ty, hp]) for g in range(3) for ty in range(2) for hp in range(8)]
                    vtl = [(wv_a[li, g * 4 + cbk], v_a[g], cbk * 512, False) for g in range(3) for cbk in range(4)]
                else:
                    tiles = [(wqk_r[li, ty * 8 + h], qk_r[ty, h]) for ty in range(2) for h in range(8)]
                    vtl = [(wvg_r[li, b], v_r, b * 512, False) for b in range(8)] + \
                          [(wvg_r[li, 8 + b], sg_r, b * 512, True) for b in range(8)]
                NB = T // TBA
                qk_all = [(tb, t_) for tb in range(NB) for t_ in tiles]
                v_all = [(tb, t_) for tb in range(NB) for t_ in vtl]
                ld = {"qk": 0, "v": 0}

                def load_qk(upto):
                    while ld["qk"] <= min(upto, len(qk_all) - 1):
                        i = ld["qk"]
                        S.dma("sp", wbs[i % 3][:], qk_all[i][1][0], writes=[("wb", i % 3)])
                        ld["qk"] += 1

                def load_v(upto):
                    while ld["v"] <= min(upto, len(v_all) - 1):
                        i = ld["v"]
                        S.dma("sp", wvs[i % 2][:], v_all[i][1][0], writes=[("wv", i % 2)])
                        ld["v"] += 1

                qi = 0
                vi_ = 0
                tctr = 0
                sctr = 0
                load_qk(1)
                for tb in range(NB):
                    tok0 = tb * TBA
                    S.dma("sp", ctab[:], c_in[:, tok0:tok0 + TBA], writes=["ctab"])
                    S.dma("sp", stab[:], s_in[:, tok0:tok0 + TBA], writes=["stab"])
                    for hf in range(TBA // 512):
                        t0 = tok0 + hf * 512
                        S.dma("sp", ht[:], hT[:, t0:t0 + 512].rearrange("(c p) t -> p c t", p=128),
                              reads=[("hT", t0 // 512)], writes=["ht"])
                        rmsnorm(ht, "ht", sq, "sq", rstd, "rstd",
                                lambda c, hf=hf: hn[:, c, hf * 512:(hf + 1) * 512], "hn", l, 7)
                    for _ in tiles:
                        (_, (wsrc, qdst)) = qk_all[qi]
                        load_qk(qi + 2)
                        load_v(vi_)
                        wb = wbs[qi % 3]
                        wbk = ("wb", qi % 3)
                        rot = rots[qi % 2]
                        rotk = ("rot", qi % 2)
                        qi += 1
                        for hf in range(TBA // 512):
                            pb = (tctr % 3) * 2
                            tm = tmp[tctr % 2]
                            tmk = ("tmp", tctr % 2)
                            tctr += 1
                            for xx in range(2):
                                for kc in range(16):
                                    S.op("pe", lambda xx=xx, kc=kc, wb=wb, pb=pb, hf=hf: nc.tensor.matmul(
                                        ps[pb + xx][:], wb[:, kc, xx * 128:(xx + 1) * 128], hn[:, kc, hf * 512:(hf + 1) * 512],
                                        start=(kc == 0), stop=(kc == 15)),
                                        reads=[wbk, "hn"], writes=[pk[pb + xx]], inc=(kc == 15))
                            cs = ctab[:, hf * 512:(hf + 1) * 512]
                            ss = stab[:, hf * 512:(hf + 1) * 512]
                            X1, X2 = ps[pb][:], ps[pb + 1][:]
                            for i, (a, b) in enumerate(((X1, cs), (X2, ss), (X1, ss), (X2, cs))):
                                S.op("dve", lambda i=i, a=a, b=b, tm=tm: nc.vector.tensor_tensor(tm[:, i, :], a, b, ALU.mult),
                                     reads=[pk[pb], pk[pb + 1], "ctab", "stab"], writes=[tmk])
                            S.op("pool", lambda tm=tm, rot=rot, hf=hf: nc.gpsimd.tensor_tensor(
                                rot[:, 0, hf * 512:(hf + 1) * 512], tm[:, 0, :], tm[:, 1, :], ALU.subtract),
                                reads=[tmk], writes=[rotk])
                            S.op("pool", lambda tm=tm, rot=rot, hf=hf: nc.gpsimd.tensor_tensor(
                                rot[:, 1, hf * 512:(hf + 1) * 512], tm[:, 2, :], tm[:, 3, :], ALU.add),
                                reads=[tmk], writes=[rotk])
                        S.dma("sp", qdst[:, :, tok0:tok0 + TBA].rearrange("r p t -> p r t"), rot[:],
                              reads=[rotk], writes=[("qk", tb)])
                    for _ in vtl:
                        (_, (wsrc, vdst, c0, silu)) = v_all[vi_]
                        load_v(vi_ + 1)
                        load_qk(qi + 1)
                        wv = wvs[vi_ % 2]
                        wvk = ("wv", vi_ % 2)
                        vi_ += 1
                        for tt in range(TBA // 128):
                            pb = 6 + (sctr % 2)
                            vs = vst[sctr % 3]
                            vsk = ("vst", sctr % 3)
                            sctr += 1
                            for kc in range(16):
                                S.op("pe", lambda kc=kc, wv=wv, pb=pb, tt=tt: nc.tensor.matmul(
                                    ps[pb][:], hn[:, kc, tt * 128:(tt + 1) * 128], wv[:, kc, :],
                                    start=(kc == 0), stop=(kc == 15)),
                                    reads=[wvk, "hn"], writes=[pk[pb]], inc=(kc == 15))
                            fn = AF.Silu if silu else AF.Copy
                            S.op("act", lambda vs=vs, pb=pb, fn=fn: nc.scalar.activation(vs[:], ps[pb][:], fn),
                                 reads=[pk[pb]], writes=[vsk])
                            S.dma("sp", vdst[tok0 + tt * 128:tok0 + (tt + 1) * 128, c0:c0 + 512], vs[:],
                                  reads=[vsk], writes=[("v", tb)])
                S.barrier()

            if is_attn:
                with contextlib.ExitStack() as st:
                    acc = [sb(st, f"a_acc{e}", [128, 2, QB], F32) for e in range(2)]
                    qt = [sb(st, f"a_q{i}", [128, 2, QB], BF16) for i in range(2)]
                    kt = [sb(st, f"a_k{i}", [128, 2, 2 * QB], BF16) for i in range(2)]
                    vt_ = [sb(st, f"a_v{i}", [128, 32, 256], BF16) for i in range(2)]
                    pts = [sb(st, f"a_pt{i}", [128, 2, 128], BF16) for i in range(4)]
                    rec = sb(st, "a_rec", [128, QB], F32)
                    osb = [sb(st, f"a_o{i}", [128, QB], BF16) for i in range(2)]
                    lctr = 0
                    bctr = 0
                    octr = 0
                    scale = 128.0 ** -0.5
                    for hp in range(8):
                        for sbk in range(T // QB):
                            q0 = sbk * QB
                            for g, (win, d) in enumerate(A_PAT):
                                span = 128 * d
                                halo = span if sbk > 0 else 0
                                bi = lctr % 2
                                lctr += 1
                                qk_, kk_, vk_ = ("aq", bi), ("ak", bi), ("av", bi)
                                S.dma("sp", qt[bi][:], qk_a[g, 0, hp][:, :, q0:q0 + QB].rearrange("r p t -> p r t"),
                                      writes=[qk_])
                                S.dma("sp", kt[bi][:, :, 0:halo + QB],
                                      qk_a[g, 1, hp][:, :, q0 - halo:q0 + QB].rearrange("r p t -> p r t"), writes=[kk_])
                                nrow = (halo + QB) // span
                                vsrc = v_a[g][q0 - halo:q0 + QB, hp * 256:(hp + 1) * 256].rearrange(
                                    "(n j r) c -> j n r c", j=128, r=d)
                                vdst = vt_[bi][:, 0:nrow * d, :].rearrange("j (n r) c -> j n r c", r=d)
                                for n_ in range(nrow):
                                    S.dma("sp", vdst[:, n_], vsrc[:, n_], writes=[vk_])
                                hrow = halo // span
                                for nl in range(QB // span):
                                    for r in range(d):
                                        for e in range(2):
                                            has_prev = (sbk > 0) or (nl > 0)
                                            pt = pts[bctr % 4]
                                            ptk = ("pt", bctr % 4)
                                            sp_ = bctr % 4
                                            np_ = 4 + bctr % 4
                                            bctr += 1
                                            qcols = slice(nl * span + r, nl * span + r + 127 * d + 1, d)
                                            kbs = ([0] if has_prev else []) + [1]
                                            stv = ps[sp_][:, 0:256].rearrange("p (k i) -> p k i", k=2)
                                            for kb in kbs:
                                                koff = (hrow + nl - 1 + kb) * span + r
                                                kcols = slice(koff, koff + 127 * d + 1, d)
                                                for R in range(2):
                                                    S.op("pe", lambda kb=kb, R=R, kcols=kcols, qcols=qcols, e=e, bi=bi, stv=stv: nc.tensor.matmul(
                                                        stv[:, kb, :], kt[bi][e * 64:(e + 1) * 64, R, kcols], qt[bi][e * 64:(e + 1) * 64, R, qcols],
                                                        start=(R == 0), stop=(R == 1)),
                                                        reads=[qk_, kk_], writes=[pk[sp_]], inc=(R == 1))
                                            k0 = kbs[0]
                                            S.op("act", lambda pt=pt, stv=stv, k0=k0: nc.scalar.activation(
                                                pt[:, k0:2, :], stv[:, k0:2, :], AF.Exp, scale=scale),
                                                reads=[pk[sp_]], writes=[ptk])
                                            S.op("pool", lambda pt=pt, k0=k0: nc.gpsimd.tensor_tensor(
                                                pt[:, k0:2, :], pt[:, k0:2, :],
                                                amask[:].rearrange("p (k i) -> p k i", k=2)[:, k0:2, :], ALU.mult),
                                                reads=[ptk, "amask"], writes=[ptk])
                                            ndv = ps[np_][:, 0:256].rearrange("p (k i) -> p k i", k=2)
                                            for idx, kb in enumerate(kbs):
                                                vrow = (hrow + nl - 1 + kb) * d + r
                                                S.op("pe", lambda kb=kb, vrow=vrow, e=e, bi=bi, pt=pt, ndv=ndv, idx=idx, kbs=kbs: nc.tensor.matmul(
                                                    ndv[:, 0, :], vt_[bi][:, vrow, e * 128:(e + 1) * 128], pt[:, kb, :],
                                                    start=(idx == 0), stop=(idx == len(kbs) - 1)),
                                                    reads=[vk_, ptk], writes=[pk[np_]], inc=False)
                                            for idx, kb in enumerate(kbs):
                                                S.op("pe", lambda kb=kb, pt=pt, ndv=ndv, idx=idx, kbs=kbs: nc.tensor.matmul(
                                                    ndv[:, 1, :], onesb[:], pt[:, kb, :],
                                                    start=(idx == 0), stop=(idx == len(kbs) - 1)),
                                                    reads=[ptk], writes=[pk[np_]], inc=(idx == len(kbs) - 1))
                                            av = acc[e][:, :, qcols]
                                            ak = ("acc", e)
                                            if g == 0:
                                                S.op("dve", lambda av=av, ndv=ndv: nc.vector.tensor_copy(av, ndv),
                                                     reads=[pk[np_]], writes=[ak])
                                            else:
                                                S.op("dve", lambda av=av, ndv=ndv: nc.vector.tensor_tensor(av, ndv, av, ALU.add),
                                                     reads=[pk[np_], ak], writes=[ak])
                            for e in range(2):
                                ak = ("acc", e)
                                ob = osb[octr % 2]
                                obk = ("osb", octr % 2)
                                octr += 1
                                S.op("dve", lambda e=e: nc.vector.reciprocal(rec[:], acc[e][:, 1, :]), reads=[ak], writes=["rec"])
                                S.op("dve", lambda e=e, ob=ob: nc.vector.tensor_tensor(ob[:], acc[e][:, 0, :], rec[:], ALU.mult),
                                     reads=[ak, "rec"], writes=[obk])
                                h = hp * 2 + e
                                S.dma("sp", oT[h * 128:(h + 1) * 128, q0:q0 + QB], ob[:], reads=[obk], writes=[("oT", 0)])
                    S.barrier()
            else:
                with contextlib.ExitStack() as st:
                    GT = 256
                    CPG = GT // 128
                    qs = [sb(st, f"r_q{i}", [128, 8, 2, GT], BF16) for i in range(2)]
                    ks = [sb(st, f"r_k{i}", [128, 8, 2, GT], BF16) for i in range(2)]
                    vs_ = [sb(st, f"r_v{i}", [128, 4096], BF16) for i in range(2)]
                    gs_ = [sb(st, f"r_g{i}", [128, 4096], BF16) for i in range(2)]
                    Sf = sb(st, "r_S", [128, 8, 2, 512], F32)
                    Sb_ = sb(st, "r_Sb", [128, 8, 2, 512], BF16)
                    pts = [sb(st, f"r_pt{i}", [128, 128], BF16) for i in range(3)]
                    kds = [sb(st, f"r_kd{i}", [128, 2, 128], BF16) for i in range(3)]
                    stt = [sb(st, f"r_st{i}", [128, 6], F32) for i in range(3)]
                    mv = [sb(st, f"r_mv{i}", [128, 2], F32) for i in range(3)]
                    rs = [sb(st, f"r_rs{i}", [128, 2], F32) for i in range(3)]
                    yn = [sb(st, f"r_yn{i}", [128, 512], BF16) for i in range(3)]
                    yg = [sb(st, f"r_yg{i}", [128, 4096], BF16) for i in range(2)]
                    ots = [sb(st, f"r_ot{i}", [128, 32, GT], BF16) for i in range(2)]
                    log_g = [float(np.log1p(-np.exp2(-5.0 - h))) for h in range(8)]
                    cdec = [float(np.exp(lg * 128.0)) for lg in log_g]
                    NCH = T // 128
                    NG = T // GT
                    NIT = NCH * 8

                    def load_grp(grp):
                        if grp >= NG:
                            return
                        bi = grp % 2
                        t0 = grp * GT
                        S.dma("sp", qs[bi][:], qk_r[0][:, :, :, t0:t0 + GT].rearrange("h r p t -> p h r t"), writes=[("rq", bi)])
                        S.dma("sp", ks[bi][:], qk_r[1][:, :, :, t0:t0 + GT].rearrange("h r p t -> p h r t"), writes=[("rk", bi)])

                    def load_chunk(n):
                        if n >= NCH:
                            return
                        vi = n % 2
                        S.dma("sp", vs_[vi][:], v_r[n * 128:(n + 1) * 128, :], writes=[("rv", vi)])
                        S.dma("sp", gs_[vi][:], sg_r[n * 128:(n + 1) * 128, :], writes=[("rg", vi)])

                    def rA(i):
                        n, h = divmod(i, 8)
                        grp, cl = divmod(n, CPG)
                        bi = grp % 2
                        cols = slice(cl * 128, (cl + 1) * 128)
                        i3 = i % 3
                        p_st = i3
                        stv = ps[p_st][:, 0:128]
                        for R in range(2):
                            S.op("pe", lambda R=R: nc.tensor.matmul(
                                stv, ks[bi][:, h, R, cols], qs[bi][:, h, R, cols], start=(R == 0), stop=(R == 1)),
                                reads=[("rq", bi), ("rk", bi)], writes=[pk[p_st]], inc=(R == 1))
                        pt = pts[i3]
                        S.op("dve", lambda: nc.vector.scalar_tensor_tensor(
                            pt[:], stv, rconst[:, h:h + 1], rmask[:], ALU.mult, ALU.mult),
                            reads=[pk[p_st], "rconst", "rmask"], writes=[("rpt", i3)])
                        if n < NCH - 1:
                            ktv = ps[p_st][:, 256:384].bitcast(BF16).rearrange("p (r t) -> p r t", r=2)
                            for R in range(2):
                                S.op("pe", lambda R=R: nc.tensor.transpose(ktv[:, R, :], ks[bi][:, h, R, cols], identb[:]),
                                     reads=[("rk", bi)], writes=[pk[p_st]], inc=(R == 1))
                            kd = kds[i3]
                            S.op("act", lambda: nc.scalar.activation(kd[:], ktv, AF.Copy, scale=rconst[:, 8 + h:9 + h]),
                                 reads=[pk[p_st], "rconst"], writes=[("kd", i3)])

                    def rB(i):
                        n, h = divmod(i, 8)
                        grp, cl = divmod(n, CPG)
                        bi = grp % 2
                        vi = n % 2
                        cols = slice(cl * 128, (cl + 1) * 128)
                        i3 = i % 3
                        p_y, p_su = 3 + (i % 2), 5 + (i % 2)
                        pt = pts[i3]
                        ygb = yg[n % 2]
                        ygk = ("yg", n % 2)
                        vh = vs_[vi][:, h * 512:(h + 1) * 512]
                        yv = ps[p_y][:]
                        S.op("pe", lambda: nc.tensor.matmul(yv, pt[:], vh, start=True, stop=(n == 0)),
                             reads=[("rpt", i3), ("rv", vi)], writes=[pk[p_y]], inc=(n == 0))
                        if n > 0:
                            for R in range(2):
                                S.op("pe", lambda R=R: nc.tensor.matmul(
                                    yv, qs[bi][:, h, R, cols], Sb_[:, h, R, :], start=False, stop=(R == 1)),
                                    reads=[("rq", bi), ("Sb", h)], writes=[pk[p_y]], inc=(R == 1))
                        S.op("dve", lambda: nc.vector.bn_stats(stt[i3][:], yv), reads=[pk[p_y]], writes=[("stt", i3)])
                        S.op("dve", lambda: nc.vector.bn_aggr(mv[i3][:], stt[i3][:]), reads=[("stt", i3)], writes=[("mv", i3)])
                        S.op("act", lambda: nc.scalar.activation(
                            rs[i3][:, 0:1], mv[i3][:, 1:2], AF.Sqrt, bias=rconst[:, 16 + h:17 + h], scale=1.0),
                            reads=[("mv", i3), "rconst"], writes=[("rs", i3)])
                        S.op("dve", lambda: nc.vector.reciprocal(rs[i3][:, 0:1], rs[i3][:, 0:1]),
                             reads=[("rs", i3)], writes=[("rs", i3)])
                        S.op("dve", lambda: nc.vector.scalar_tensor_tensor(
                            rs[i3][:, 1:2], mv[i3][:, 0:1], -1.0, rs[i3][:, 0:1], ALU.mult, ALU.mult),
                            reads=[("rs", i3), ("mv", i3)], writes=[("rs", i3)])
                        S.op("act", lambda: nc.scalar.activation(
                            yn[i3][:], yv, AF.Identity, bias=rs[i3][:, 1:2], scale=rs[i3][:, 0:1]),
                            reads=[pk[p_y], ("rs", i3)], writes=[("yn", i3)])
                        S.op("pool", lambda: nc.gpsimd.tensor_tensor(
                            ygb[:, h * 512:(h + 1) * 512], yn[i3][:], gs_[vi][:, h * 512:(h + 1) * 512], ALU.mult),
                            reads=[("yn", i3), ("rg", vi)], writes=[ygk])
                        if n < NCH - 1:
                            kd = kds[i3]
                            for c in range(2):
                                S.op("pe", lambda c=c: nc.tensor.matmul(ps[p_su][:], kd[:, c, :], vh, start=True, stop=True),
                                     reads=[("kd", i3), ("rv", vi)], writes=[pk[p_su]], inc=True)
                                if n == 0:
                                    S.op("dve", lambda c=c: nc.vector.tensor_copy(Sf[:, h, c, :], ps[p_su][:]),
                                         reads=[pk[p_su]], writes=[("Sf", h)])
                                else:
                                    S.op("dve", lambda c=c: nc.vector.scalar_tensor_tensor(
                                        Sf[:, h, c, :], Sf[:, h, c, :], cdec[h], ps[p_su][:], ALU.mult, ALU.add),
                                        reads=[pk[p_su], ("Sf", h)], writes=[("Sf", h)])
                            S.op("act", lambda: nc.scalar.copy(Sb_[:, h, :, :], Sf[:, h, :, :]),
                                 reads=[("Sf", h)], writes=[("Sb", h)])
                        if h == 7:
                            ot = ots[bi]
                            otk = ("ots", bi)
                            for f8 in range(4):
                                pb = 7
                                tv = ps[pb][:].bitcast(BF16).rearrange("p (f t) -> p f t", f=8)
                                for ff in range(8):
                                    f = f8 * 8 + ff
                                    S.op("pe", lambda f=f, ff=ff: nc.tensor.transpose(
                                        tv[:, ff, :], ygb[:, f * 128:(f + 1) * 128], identb[:]),
                                        reads=[ygk], writes=[pk[pb]], inc=(ff == 7))
                                if f8 % 2 == 0:
                                    S.op("act", lambda f8=f8: nc.scalar.copy(ot[:, f8 * 8:(f8 + 1) * 8, cols], tv),
                                         reads=[pk[pb]], writes=[otk])
                                else:
                                    S.op("dve", lambda f8=f8: nc.vector.tensor_copy(ot[:, f8 * 8:(f8 + 1) * 8, cols], tv),
                                         reads=[pk[pb]], writes=[otk])
                            if cl == CPG - 1:
                                t0 = grp * GT
                                S.dma("sp", oT[:, t0:t0 + GT].rearrange("(f p) t -> p f t", p=128), ot[:],
                                      reads=[otk], writes=[("oT", 0)])

                    load_grp(0)
                    load_grp(1)
                    load_chunk(0)
                    rA(0)
                    rA(1)
                    for i in range(NIT):
                        n, h = divmod(i, 8)
                        if h == 0:
                            load_chunk(n + 1)
                            if n % CPG == 0 and n // CPG >= 1:
                                load_grp(n // CPG + 1)
                        rB(i)
                        if i + 2 < NIT:
                            rA(i + 2)
                    S.barrier()

            with contextlib.ExitStack() as st:
                KO = 16 if is_attn else 32
                wo_src = wo_a[li] if is_attn else wo_r[li]
                ht = sb(st, "f_ht", [128, 16, 512], F32)
                rstd = sb(st, "f_rstd", [128, 512], F32)
                hn = sb(st, "f_hn", [128, 16, 512], BF16)
                actb = sb(st, "f_act", [128, NFF, 512], BF16)
                ot = actb
                sq = actb[:, 28:44, :]
                wos = [sb(st, f"f_wo{i}", [128, KO, 128], BF16) for i in range(2)]
                wus = [sb(st, f"f_wu{i}", [128, 2, 16, 128], BF16) for i in range(2)]
                wds = [sb(st, f"f_wd{i}", [128, NFF, 128], BF16) for i in range(2)]
                ug = [sb(st, f"f_ug{i}", [128, 2, 514], F32) for i in range(2)]
                cg = [sb(st, f"f_cg{i}", [128, 2, 512], F32) for i in range(2)]
                sgt = [sb(st, f"f_sg{i}", [128, 512], F32) for i in range(2)]
                utail = sb(st, "f_utail", [128, 88, 2], F32)
                S.op("pool", lambda: nc.gpsimd.memset(utail[:], 0.0), writes=["utail"])
                cwv = cw[:].rearrange("p (l j w) -> p l j w", l=4, j=88)
                cbv = cb[:].rearrange("p (l j) -> p l j", l=4)
                woc = 0
                wuc = 0
                wdc = 0
                uc = 0
                for tb in range(NTB):
                    t0 = tb * 512
                    S.dma("sp", ht[:], hT[:, t0:t0 + 512].rearrange("(c p) t -> p c t", p=128),
                          reads=[("hT", tb)], writes=["ht"])
                    S.dma("sp", ot[:, 0:KO, :], oT[0:KO * 128, t0:t0 + 512].rearrange("(c p) t -> p c t", p=128),
                          reads=[("oT", 0)], writes=["actb"])
                    for m in range(16):
                        wo = wos[woc % 2]
                        wok = ("wo", woc % 2)
                        woc += 1
                        S.dma("sp", wo[:], wo_src[m], writes=[wok])
                        pb = m % 2
                        for kc in range(KO):
                            S.op("pe", lambda kc=kc, wo=wo, pb=pb: nc.tensor.matmul(
                                ps[pb][:], wo[:, kc, :], ot[:, kc, :], start=(kc == 0), stop=(kc == KO - 1)),
                                reads=[wok, "actb"], writes=[pk[pb]], inc=(kc == KO - 1))
                        S.op("dve", lambda m=m, pb=pb: nc.vector.tensor_tensor(ht[:, m, :], ps[pb][:], ht[:, m, :], ALU.add),
                             reads=[pk[pb], "ht"], writes=["ht"])
                    rmsnorm(ht, "ht", sq, "actb", rstd, "rstd", lambda c: hn[:, c, :], "hn", 4 + l, 7)
                    for j in range(NFF):
                        wu = wus[wuc % 2]
                        wuk = ("wu", wuc % 2)
                        wuc += 1
                        S.dma("sp", wu[:, 0], wup_b[l, j], writes=[wuk])
                        S.dma("sp", wu[:, 1], wup_b[l, NFF + j], writes=[wuk])
                        u = ug[uc % 2]
                        uk = ("ug", uc % 2)
                        c_ = cg[uc % 2]
                        ck = ("cg", uc % 2)
                        sg_ = sgt[uc % 2]
                        sk = ("sgt", uc % 2)
                        pb = 2 + (uc % 2) * 2
                        uc += 1
                        for s in range(2):
                            for kc in range(16):
                                S.op("pe", lambda s=s, kc=kc, wu=wu, pb=pb: nc.tensor.matmul(
                                    ps[pb + s][:], wu[:, s, kc, :], hn[:, kc, :], start=(kc == 0), stop=(kc == 15)),
                                    reads=[wuk, "hn"], writes=[pk[pb + s]], inc=(kc == 15))
                        for s in range(2):
                            jj = s * NFF + j
                            S.op("pool", lambda s=s, jj=jj, u=u: nc.gpsimd.tensor_copy(u[:, s, 0:2], utail[:, jj, :]),
                                 reads=["utail"], writes=[uk])
                            S.op("act", lambda s=s, u=u, pb=pb: nc.scalar.copy(u[:, s, 2:514], ps[pb + s][:]),
                                 reads=[pk[pb + s]], writes=[uk])
                            S.op("act", lambda s=s, jj=jj, c_=c_, pb=pb: nc.scalar.activation(
                                c_[:, s, :], ps[pb + s][:], AF.Identity, bias=cbv[:, l, jj:jj + 1], scale=cwv[:, l, jj, 2:3]),
                                reads=[pk[pb + s], "cw", "cb"], writes=[ck])
                            S.op("pool", lambda s=s, jj=jj, u=u: nc.gpsimd.tensor_copy(utail[:, jj, :], u[:, s, 512:514]),
                                 reads=[uk], writes=["utail"])
                            for w_ in range(2):
                                S.op("dve", lambda s=s, jj=jj, c_=c_, u=u, w_=w_: nc.vector.scalar_tensor_tensor(
                                    c_[:, s, :], u[:, s, w_:w_ + 512], cwv[:, l, jj, w_:w_ + 1], c_[:, s, :], ALU.mult, ALU.add),
                                    reads=[uk, ck, "cw"], writes=[ck])
                        S.op("act", lambda c_=c_, sg_=sg_: nc.scalar.activation(sg_[:], c_[:, 0, :], AF.Silu), reads=[ck], writes=[sk])
                        S.op("pool", lambda j=j, c_=c_, sg_=sg_: nc.gpsimd.tensor_tensor(actb[:, j, :], sg_[:], c_[:, 1, :], ALU.mult),
                             reads=[ck, sk], writes=["actb"])
                    for m in range(16):
                        wd = wds[wdc % 2]
                        wdk = ("wd", wdc % 2)
                        wdc += 1
                        S.dma("sp", wd[:], wdn_b[l, m], writes=[wdk])
                        pb = m % 2
                        for kc in range(NFF):
                            S.op("pe", lambda kc=kc, wd=wd, pb=pb: nc.tensor.matmul(
                                ps[pb][:], wd[:, kc, :], actb[:, kc, :], start=(kc == 0), stop=(kc == NFF - 1)),
                                reads=[wdk, "actb"], writes=[pk[pb]], inc=(kc == NFF - 1))
                        S.op("dve", lambda m=m, pb=pb: nc.vector.tensor_tensor(ht[:, m, :], ps[pb][:], ht[:, m, :], ALU.add),
                             reads=[pk[pb], "ht"], writes=["ht"])
                    S.dma("sp", hT[:, t0:t0 + 512].rearrange("(c p) t -> p c t", p=128), ht[:],
                          reads=["ht"], writes=[("hT", tb)])
                S.barrier()

        with contextlib.ExitStack() as st:
            ht = sb(st, "z_ht", [128, 16, 512], F32)
            sq = sb(st, "z_sq", [128, 16, 512], BF16)
            rstd = sb(st, "z_rstd", [128, 512], F32)
            hn = sb(st, "z_hn", [128, 16, 512], F32)
            ob = [sb(st, f"z_ob{i}", [128, D], F32) for i in range(2)]
            oc = 0
            pc = 0
            for tb in range(NTB):
                t0 = tb * 512
                S.dma("sp", ht[:], hT[:, t0:t0 + 512].rearrange("(c p) t -> p c t", p=128),
                      reads=[("hT", tb)], writes=["ht"])
                rmsnorm(ht, "ht", sq, "sq", rstd, "rstd", lambda c: hn[:, c, :], "hn", 8, 7)
                for j in range(4):
                    o_ = ob[oc % 2]
                    ok_ = ("ob", oc % 2)
                    oc += 1
                    for c4 in range(4):
                        p = pc % 6
                        pc += 1
                        for cc in range(4):
                            c = c4 * 4 + cc
                            S.op("pe", lambda c=c, cc=cc, p=p, j=j: nc.tensor.transpose(
                                ps[p][:, cc * 128:(cc + 1) * 128], hn[:, c, j * 128:(j + 1) * 128], ident[:]),
                                reads=["hn"], writes=[pk[p]], inc=(cc == 3))
                        if c4 % 2 == 0:
                            S.op("act", lambda o_=o_, p=p, c4=c4: nc.scalar.copy(o_[:, c4 * 512:(c4 + 1) * 512], ps[p][:]),
                                 reads=[pk[p]], writes=[ok_])
                        else:
                            S.op("dve", lambda o_=o_, p=p, c4=c4: nc.vector.tensor_copy(o_[:, c4 * 512:(c4 + 1) * 512], ps[p][:]),
                                 reads=[pk[p]], writes=[ok_])
                    S.dma("sp", out[t0 + j * 128:t0 + (j + 1) * 128, :], o_[:], reads=[ok_], writes=[("out", 0)])
            S.barrier()
    return nc


def host_consts(T):
    pos = np.arange(T, dtype=np.float32)
    inv_a = (10000.0 ** (-np.arange(0, 128, 2, dtype=np.float32) / 128.0)).astype(np.float32)
    ang_a = (pos[None, :] * inv_a[:, None]).astype(np.float32)
    ca = np.concatenate([np.cos(ang_a), np.cos(ang_a)], 0).astype(np.float32)
    sa = np.concatenate([np.sin(ang_a), np.sin(ang_a)], 0).astype(np.float32)
    inv_r = (10000.0 ** (-np.linspace(0.0, 1.0, 128, dtype=np.float32))).astype(np.float32)
    ang_r = (pos[None, :] * inv_r[:, None]).astype(np.float32)
    cr, sr = np.cos(ang_r).astype(np.float32), np.sin(ang_r).astype(np.float32)
    j = np.arange(128)[:, None]
    i = np.arange(128)[None, :]
    amask = np.concatenate([(j >= i), (j <= i)], 1).astype(np.float32)
    rmask = (i >= j).astype(np.float32)
    log_g = np.log1p(-np.exp2(-5.0 - np.arange(8, dtype=np.float64)))
    jj = np.arange(128, dtype=np.float64)[:, None]
    rconst = np.zeros((128, 24), np.float64)
    rconst[:, 0:8] = np.exp(-log_g[None, :] * (jj + 1.0)) * 0.0625
    rconst[:, 8:16] = np.exp(log_g[None, :] * (127.0 - jj)) * 0.0625
    rconst[:, 16:24] = EPS * np.exp(-2.0 * log_g[None, :] * (jj + 1.0))
    return dict(ca=ca, sa=sa, cr=cr, sr=sr, amask=amask, rmask=rmask,
                rconst=rconst.astype(np.float32), ident=np.eye(128, dtype=np.float32))


def make_in_maps(inputs, T, n_cores=8):
    f = lambda a: np.ascontiguousarray(np.asarray(a, dtype=np.float32))
    hc = host_consts(T)
    gv = np.concatenate([f(inputs["norm_mix"]), f(inputs["norm_ffn"]), f(inputs["norm_final"])[None]], 0)
    gains = np.ascontiguousarray(gv.reshape(9, 16, 128).transpose(2, 0, 1).reshape(128, 144))
    cw = np.ascontiguousarray(f(inputs["conv_w"]).reshape(4, 3, 88, 128).transpose(3, 0, 2, 1).reshape(128, -1))
    cb = np.ascontiguousarray(f(inputs["conv_b"]).reshape(4, 88, 128).transpose(2, 0, 1).reshape(128, -1))
    x = f(inputs["x"])
    B = x.shape[0]
    shared = dict(gains=gains, cw=cw, cb=cb, **hc)
    for k in ("w_in_attn", "w_out_attn", "w_in_ret", "w_out_ret", "w_up", "w_down"):
        shared[k] = f(inputs[k])
    maps = []
    for c in range(n_cores):
        m = dict(shared)
        m["x"] = np.ascontiguousarray(x[c % B, :T])
        maps.append(m)
    return maps


_NC_CACHE = {}


def kernel(**inputs):
    T, depth = 8192, 4
    key = (T, depth)
    if key not in _NC_CACHE:
        _NC_CACHE[key] = build(T, depth)
    nc = _NC_CACHE[key]
    maps = make_in_maps(inputs, T)
    res = run_bass_kernel_spmd(nc, maps, core_ids=list(range(8)))
    B = np.asarray(inputs["x"]).shape[0]
    return np.stack([np.asarray(res.results[b]["out"], dtype=np.float32) for b in range(B)], 0)
```

```python
import contextlib
import numpy as np
import ml_dtypes
import concourse.bass as bass
import concourse.mybir as mybir
from concourse.bass_utils import run_bass_kernel_spmd

F32 = mybir.dt.float32
BF16 = mybir.dt.bfloat16
AF = mybir.ActivationFunctionType
ALU = mybir.AluOpType

D = 2048
DFF = 5632
NFF = DFF // 128
EPS = 1e-6
A_PAT = ((128, 1), (512, 4), (2048, 16))
A_PROJ = 18432
R_PROJ = 12288
QB = 2048
REAL_CORES = [0, 1, 4, 5]


class Eng:
    def __init__(self, name, h, sem):
        self.name, self.h, self.sem = name, h, sem
        self.count = 0
        self.seen = {}


class Slot:
    def __init__(self, sem):
        self.sem = sem
        self.count = 0


class Sched:
    def __init__(self, nc, es):
        self.nc = nc
        sem = lambda n: es.enter_context(nc.semaphore(n))
        self.E = {
            "pe": Eng("pe", nc.tensor, sem("s_pe")),
            "act": Eng("act", nc.scalar, sem("s_act")),
            "dve": Eng("dve", nc.vector, sem("s_dve")),
            "pool": Eng("pool", nc.gpsimd, sem("s_pool")),
            "sp": Eng("sp", nc.sync, sem("s_sp")),
        }
        self.rings = {
            "sp": [Slot(sem(f"d_sp{i}")) for i in range(40)],
            "pool": [Slot(sem(f"d_pl{i}")) for i in range(32)],
            "act": [Slot(sem(f"d_ac{i}")) for i in range(12)],
        }
        self.rpos = {"sp": 0, "pool": 0, "act": 0}
        self.lw = {}
        self.rd = {}

    def _wait(self, E, dep):
        sem, v, owner = dep
        if v <= 0:
            return
        if owner == E.name:
            if E.name == "pe":
                return
            if v <= E.count - 3:
                return
        if E.seen.get(sem.name, 0) >= v:
            return
        E.h.wait_ge(sem, v)
        E.seen[sem.name] = v

    def _deps(self, E, reads, writes):
        for k in reads:
            if k in self.lw:
                self._wait(E, self.lw[k])
        for k in writes:
            if k in self.lw:
                self._wait(E, self.lw[k])
            for d in self.rd.get(k, {}).values():
                self._wait(E, d)

    def _stamp(self, stamp, reads, writes):
        for k in writes:
            self.lw[k] = stamp
            self.rd[k] = {}
        for k in reads:
            self.rd.setdefault(k, {})[stamp[0].name] = stamp

    def op(self, eng, emit, reads=(), writes=(), inc=True):
        E = self.E[eng]
        self._deps(E, reads, writes)
        ins = emit()
        if inc:
            E.count += 1
            ins.then_inc(E.sem, 1)
            stamp = (E.sem, E.count, eng)
        else:
            stamp = (E.sem, E.count + 1, eng)
        self._stamp(stamp, reads, writes)
        return ins

    def dma(self, q, out, in_, reads=(), writes=()):
        Q = self.E[q]
        ring = self.rings[q]
        slot = ring[self.rpos[q] % len(ring)]
        self.rpos[q] += 1
        self._wait(Q, (slot.sem, slot.count, None))
        self._deps(Q, reads, writes)
        ins = Q.h.dma_start(out=out, in_=in_)
        ins.then_inc(slot.sem, 16)
        slot.count += 16
        self._stamp((slot.sem, slot.count, None), reads, writes)
        return ins

    def barrier(self):
        for E in self.E.values():
            for F in self.E.values():
                if F is not E:
                    self._wait(E, (F.sem, F.count, F.name))
            for ring in self.rings.values():
                for s in ring:
                    self._wait(E, (s.sem, s.count, None))
        self.lw = {}
        self.rd = {}


def build(T, depth):
    nc = bass.Bass("TRN2", target_bir_lowering=False)
    NTB = T // 512
    n_attn = (depth + 1) // 2
    n_ret = depth // 2

    def din(name, shape, dt=F32):
        return nc.dram_tensor(name, list(shape), dt, kind="ExternalInput").ap()

    def dscr(name, shape, dt):
        return nc.dram_tensor(name, list(shape), dt).ap()

    x = din("x", [T, D])
    gains = din("gains", [128, 9 * 16])
    ident_in = din("ident", [128, 128])
    ca_in, sa_in = din("ca", [128, T]), din("sa", [128, T])
    cr_in, sr_in = din("cr", [128, T]), din("sr", [128, T])
    amask_in = din("amask", [128, 256])
    rmask_in = din("rmask", [128, 128])
    rconst_in = din("rconst", [128, 24])
    cw_in = din("cw", [128, 4 * 88 * 3])
    cb_in = din("cb", [128, 4 * 88])
    w_in_attn = din("w_in_attn", [2, D, A_PROJ])
    w_out_attn = din("w_out_attn", [2, D, D])
    w_in_ret = din("w_in_ret", [2, D, R_PROJ])
    w_out_ret = din("w_out_ret", [2, 4096, D])
    w_up = din("w_up", [4, D, 2 * DFF])
    w_down = din("w_down", [4, DFF, D])
    out = nc.dram_tensor("out", [T, D], F32, kind="ExternalOutput").ap()

    hT = dscr("hT", [D, T], F32)
    oT = dscr("oT", [4096, T], BF16)
    wqk_a = dscr("wqk_a", [max(n_attn, 1), 48, 128, 16, 256], BF16)
    wv_a = dscr("wv_a", [max(n_attn, 1), 12, 128, 16, 512], BF16)
    wo_a = dscr("wo_a", [max(n_attn, 1), 16, 128, 16, 128], BF16)
    wqk_r = dscr("wqk_r", [max(n_ret, 1), 16, 128, 16, 256], BF16)
    wvg_r = dscr("wvg_r", [max(n_ret, 1), 16, 128, 16, 512], BF16)
    wo_r = dscr("wo_r", [max(n_ret, 1), 16, 128, 32, 128], BF16)
    wup_b = dscr("wup_b", [max(depth, 1), 88, 128, 16, 128], BF16)
    wdn_b = dscr("wdn_b", [max(depth, 1), 16, 128, NFF, 128], BF16)
    qk_a = dscr("qk_a", [3, 2, 8, 2, 128, T], BF16)
    v_a = dscr("v_a", [3, T, D], BF16)
    qk_r = dscr("qk_r", [2, 8, 2, 128, T], BF16)
    v_r = dscr("v_r", [T, 4096], BF16)
    sg_r = dscr("sg_r", [T, 4096], BF16)

    es = contextlib.ExitStack()
    with es:
        S = Sched(nc, es)

        uid = [0]

        def sb(st, name, shape, dt):
            uid[0] += 1
            return st.enter_context(nc.sbuf_tensor(f"sb{uid[0]}_{name}", list(shape), dt))

        ps = [es.enter_context(nc.psum_tensor(f"ps{i}", [128, 512], F32)) for i in range(8)]
        pk = [("ps", i) for i in range(8)]

        ident = sb(es, "ident", [128, 128], F32)
        identb = sb(es, "identb", [128, 128], BF16)
        onesb = sb(es, "onesb", [128, 128], BF16)
        gsb = sb(es, "gsb", [128, 9 * 16], F32)
        amask = sb(es, "amask", [128, 256], BF16)
        rmask = sb(es, "rmask", [128, 128], F32)
        rconst = sb(es, "rconst", [128, 24], F32)
        cw = sb(es, "cw", [128, 4 * 88 * 3], F32)
        cb = sb(es, "cb", [128, 4 * 88], F32)
        with contextlib.ExitStack() as st:
            am32 = sb(st, "am32", [128, 256], F32)
            S.dma("sp", ident[:], ident_in, writes=["ident"])
            S.dma("sp", gsb[:], gains, writes=["gsb"])
            S.dma("sp", am32[:], amask_in, writes=["am32"])
            S.dma("sp", rmask[:], rmask_in, writes=["rmask"])
            S.dma("sp", rconst[:], rconst_in, writes=["rconst"])
            S.dma("sp", cw[:], cw_in, writes=["cw"])
            S.dma("sp", cb[:], cb_in, writes=["cb"])
            S.op("dve", lambda: nc.vector.tensor_copy(identb[:], ident[:]), reads=["ident"], writes=["identb"])
            S.op("dve", lambda: nc.vector.tensor_copy(amask[:], am32[:]), reads=["am32"], writes=["amask"])
            S.op("dve", lambda: nc.vector.memset(onesb[:], 1.0), writes=["onesb"])
            S.barrier()

        def cast(dst, src):
            S.dma("pool", dst, src)

        def kview(w, c0, n):
            return w[:, c0:c0 + n].rearrange("(kc p) c -> p kc c", p=128)

        for l in range(depth):
            li = l // 2
            if l % 2 == 0:
                w = w_in_attn[li]
                for g in range(3):
                    for ty in range(2):
                        for hp in range(8):
                            base = g * 6144 + ty * 2048 + hp * 256
                            blk = (g * 2 + ty) * 8 + hp
                            src = w[:, base:base + 256].rearrange("(kc p) (e x d) -> p kc x e d", p=128, e=2, x=2)
                            dstv = wqk_a[li, blk].rearrange("p kc (x e d) -> p kc x e d", x=2, e=2)
                            for xx in range(2):
                                for ee in range(2):
                                    cast(dstv[:, :, xx, ee], src[:, :, xx, ee])
                    for cbk in range(4):
                        cast(wv_a[li, g * 4 + cbk], kview(w, g * 6144 + 4096 + cbk * 512, 512))
                for m in range(16):
                    cast(wo_a[li, m], kview(w_out_attn[li], m * 128, 128))
            else:
                w = w_in_ret[li]
                for b in range(16):
                    cast(wqk_r[li, b], kview(w, b * 256, 256))
                for b in range(16):
                    cast(wvg_r[li, b], kview(w, 4096 + b * 512, 512))
                for m in range(16):
                    cast(wo_r[li, m], kview(w_out_ret[li], m * 128, 128))
            for j in range(88):
                cast(wup_b[l, j], kview(w_up[l], j * 128, 128))
            for m in range(16):
                cast(wdn_b[l, m], kview(w_down[l], m * 128, 128))

        with contextlib.ExitStack() as st:
            xs = [sb(st, f"xs{i}", [128, D], F32) for i in range(2)]
            stg = [sb(st, f"xstg{i}", [128, 16, 512], F32) for i in range(2)]
            for tb in range(NTB):
                sg = stg[tb % 2]
                sgk = ("xstg", tb % 2)
                for j in range(4):
                    ti = tb * 4 + j
                    xt = xs[ti % 2]
                    xk = ("xs", ti % 2)
                    S.dma("sp", xt[:], x[ti * 128:(ti + 1) * 128, :], writes=[xk])
                    for c4 in range(4):
                        p = (ti * 4 + c4) % 8
                        for cc in range(4):
                            c = c4 * 4 + cc
                            S.op("pe", lambda c=c, cc=cc, p=p, xt=xt: nc.tensor.transpose(
                                ps[p][:, cc * 128:(cc + 1) * 128], xt[:, c * 128:(c + 1) * 128], ident[:]),
                                reads=[xk], writes=[pk[p]], inc=(cc == 3))
                        eng = "act" if c4 % 2 == 0 else "dve"
                        src = ps[p][:].rearrange("p (c t) -> p c t", c=4)
                        dst = sg[:, c4 * 4:(c4 + 1) * 4, j * 128:(j + 1) * 128]
                        if eng == "act":
                            S.op("act", lambda dst=dst, src=src: nc.scalar.copy(dst, src), reads=[pk[p]], writes=[sgk])
                        else:
                            S.op("dve", lambda dst=dst, src=src: nc.vector.tensor_copy(dst, src), reads=[pk[p]], writes=[sgk])
                S.dma("sp", hT[:, tb * 512:(tb + 1) * 512].rearrange("(c p) t -> p c t", p=128), sg[:],
                      reads=[sgk], writes=[("hT", tb)])
            S.barrier()

        def rmsnorm(ht, htk, sq, sqk, rstd, rstdk, dst_fn, dstk, gidx, pbank):
            S.op("act", lambda: nc.scalar.activation(sq[:], ht[:], AF.Square), reads=[htk], writes=[sqk])
            for c in range(16):
                S.op("pe", lambda c=c: nc.tensor.matmul(ps[pbank][:], onesb[:], sq[:, c, :], start=(c == 0), stop=(c == 15)),
                     reads=[sqk], writes=[pk[pbank]], inc=(c == 15))
            S.op("act", lambda: nc.scalar.activation(rstd[:], ps[pbank][:], AF.Sqrt, bias=EPS, scale=1.0 / D),
                 reads=[pk[pbank]], writes=[rstdk])
            S.op("dve", lambda: nc.vector.reciprocal(rstd[:], rstd[:]), reads=[rstdk], writes=[rstdk])
            for c in range(16):
                S.op("dve", lambda c=c: nc.vector.scalar_tensor_tensor(
                    dst_fn(c), ht[:, c, :], gsb[:, gidx * 16 + c:gidx * 16 + c + 1], rstd[:], ALU.mult, ALU.mult),
                    reads=[htk, rstdk], writes=[dstk])

        def proj_rot(wb, wbk, hn, hnk, t0, ctab, stab, tabk, rot, rotk, pb):
            for xx in range(2):
                for kc in range(16):
                    S.op("pe", lambda xx=xx, kc=kc: nc.tensor.matmul(
                        ps[pb + xx][:], wb[:, kc, xx * 128:(xx + 1) * 128], hn[:, kc, t0:t0 + 512],
                        start=(kc == 0), stop=(kc == 15)),
                        reads=[wbk, hnk], writes=[pk[pb + xx]], inc=(kc == 15))
            return

        for l in range(depth):
            li = l // 2
            is_attn = (l % 2 == 0)
            with contextlib.ExitStack() as st:
                TBA = 1024
                ht = sb(st, "p_ht", [128, 16, 512], F32)
                sq = sb(st, "p_sq", [128, 16, 512], BF16)
                rstd = sb(st, "p_rstd", [128, 512], F32)
                hn = sb(st, "p_hn", [128, 16, TBA], BF16)
                ctab = sb(st, "p_ct", [128, TBA], F32)
                stab = sb(st, "p_st", [128, TBA], F32)
                wbs = [sb(st, f"p_wb{i}", [128, 16, 256], BF16) for i in range(3)]
                wvs = [sb(st, f"p_wv{i}", [128, 16, 512], BF16) for i in range(2)]
                rots = [sb(st, f"p_rot{i}", [128, 2, TBA], BF16) for i in range(2)]
                tmp = [sb(st, f"p_tmp{i}", [128, 4, 512], BF16) for i in range(2)]
                vst = [sb(st, f"p_vst{i}", [128, 512], BF16) for i in range(3)]
                c_in, s_in = (ca_in, sa_in) if is_attn else (cr_in, sr_in)
                if is_attn:
                    tiles = [(wqk_a[li, (g * 2 + ty) * 8 + hp], qk_a[g, ty, hp]) for g in range(3) for ty in range(2) for hp in range(8)]
                    vtl = [(wv_a[li, g * 4 + cbk], v_a[g], cbk * 512, False) for g in range(3) for cbk in range(4)]
                else:
                    tiles = [(wqk_r[li, ty * 8 + h], qk_r[ty, h]) for ty in range(2) for h in range(8)]
                    vtl = [(wvg_r[li, b], v_r, b * 512, False) for b in range(8)] + \
                          [(wvg_r[li, 8 + b], sg_r, b * 512, True) for b in range(8)]
                NB = T // TBA
                qk_all = [(tb, t_) for tb in range(NB) for t_ in tiles]
                v_all = [(tb, t_) for tb in range(NB) for t_ in vtl]
                ld = {"qk": 0, "v": 0}

                def load_qk(upto):
                    while ld["qk"] <= min(upto, len(qk_all) - 1):
                        i = ld["qk"]
                        S.dma("sp", wbs[i % 3][:], qk_all[i][1][0], writes=[("wb", i % 3)])
                        ld["qk"] += 1

                def load_v(upto):
                    while ld["v"] <= min(upto, len(v_all) - 1):
                        i = ld["v"]
                        S.dma("sp", wvs[i % 2][:], v_all[i][1][0], writes=[("wv", i % 2)])
                        ld["v"] += 1

                qi = 0
                vi_ = 0
                tctr = 0
                sctr = 0
                load_qk(1)
                for tb in range(NB):
                    tok0 = tb * TBA
                    S.dma("sp", ctab[:], c_in[:, tok0:tok0 + TBA], writes=["ctab"])
                    S.dma("sp", stab[:], s_in[:, tok0:tok0 + TBA], writes=["stab"])
                    for hf in range(TBA // 512):
                        t0 = tok0 + hf * 512
                        S.dma("sp", ht[:], hT[:, t0:t0 + 512].rearrange("(c p) t -> p c t", p=128),
                              reads=[("hT", t0 // 512)], writes=["ht"])
                        rmsnorm(ht, "ht", sq, "sq", rstd, "rstd",
                                lambda c, hf=hf: hn[:, c, hf * 512:(hf + 1) * 512], "hn", l, 7)
                    for _ in tiles:
                        (_, (wsrc, qdst)) = qk_all[qi]
                        load_qk(qi + 2)
                        load_v(vi_)
                        wb = wbs[qi % 3]
                        wbk = ("wb", qi % 3)
                        rot = rots[qi % 2]
                        rotk = ("rot", qi % 2)
                        qi += 1
                        for hf in range(TBA // 512):
                            pb = (tctr % 3) * 2
                            tm = tmp[tctr % 2]
                            tmk = ("tmp", tctr % 2)
                            tctr += 1
                            for xx in range(2):
                                for kc in range(16):
                                    S.op("pe", lambda xx=xx, kc=kc, wb=wb, pb=pb, hf=hf: nc.tensor.matmul(
                                        ps[pb + xx][:], wb[:, kc, xx * 128:(xx + 1) * 128], hn[:, kc, hf * 512:(hf + 1) * 512],
                                        start=(kc == 0), stop=(kc == 15)),
                                        reads=[wbk, "hn"], writes=[pk[pb + xx]], inc=(kc == 15))
                            cs = ctab[:, hf * 512:(hf + 1) * 512]
                            ss = stab[:, hf * 512:(hf + 1) * 512]
                            X1, X2 = ps[pb][:], ps[pb + 1][:]
                            for i, (a, b) in enumerate(((X1, cs), (X2, ss), (X1, ss), (X2, cs))):
                                S.op("dve", lambda i=i, a=a, b=b, tm=tm: nc.vector.tensor_tensor(tm[:, i, :], a, b, ALU.mult),
                                     reads=[pk[pb], pk[pb + 1], "ctab", "stab"], writes=[tmk])
                            S.op("pool", lambda tm=tm, rot=rot, hf=hf: nc.gpsimd.tensor_tensor(
                                rot[:, 0, hf * 512:(hf + 1) * 512], tm[:, 0, :], tm[:, 1, :], ALU.subtract),
                                reads=[tmk], writes=[rotk])
                            S.op("pool", lambda tm=tm, rot=rot, hf=hf: nc.gpsimd.tensor_tensor(
                                rot[:, 1, hf * 512:(hf + 1) * 512], tm[:, 2, :], tm[:, 3, :], ALU.add),
                                reads=[tmk], writes=[rotk])
                        S.dma("sp", qdst[:, :, tok0:tok0 + TBA].rearrange("r p t -> p r t"), rot[:],
                              reads=[rotk], writes=[("qk", tb)])
                    for _ in vtl:
                        (_, (wsrc, vdst, c0, silu)) = v_all[vi_]
                        load_v(vi_ + 1)
                        load_qk(qi + 1)
                        wv = wvs[vi_ % 2]
                        wvk = ("wv", vi_ % 2)
                        vi_ += 1
                        for tt in range(TBA // 128):
                            pb = 6 + (sctr % 2)
                            vs = vst[sctr % 3]
                            vsk = ("vst", sctr % 3)
                            sctr += 1
                            for kc in range(16):
                                S.op("pe", lambda kc=kc, wv=wv, pb=pb, tt=tt: nc.tensor.matmul(
                                    ps[pb][:], hn[:, kc, tt * 128:(tt + 1) * 128], wv[:, kc, :],
                                    start=(kc == 0), stop=(kc == 15)),
                                    reads=[wvk, "hn"], writes=[pk[pb]], inc=(kc == 15))
                            fn = AF.Silu if silu else AF.Copy
                            S.op("act", lambda vs=vs, pb=pb, fn=fn: nc.scalar.activation(vs[:], ps[pb][:], fn),
                                 reads=[pk[pb]], writes=[vsk])
                            S.dma("sp", vdst[tok0 + tt * 128:tok0 + (tt + 1) * 128, c0:c0 + 512], vs[:],
                                  reads=[vsk], writes=[("v", tb)])
                S.barrier()

            if is_attn:
                with contextlib.ExitStack() as st:
                    acc = [sb(st, f"a_acc{e}", [128, 2, QB], F32) for e in range(2)]
                    qt = [sb(st, f"a_q{i}", [128, 2, QB], BF16) for i in range(2)]
                    kt = [sb(st, f"a_k{i}", [128, 2, 2 * QB], BF16) for i in range(2)]
                    vt_ = [sb(st, f"a_v{i}", [128, 32, 256], BF16) for i in range(2)]
                    pts = [sb(st, f"a_pt{i}", [128, 2, 128], BF16) for i in range(4)]
                    rec = sb(st, "a_rec", [128, QB], F32)
                    osb = [sb(st, f"a_o{i}", [128, QB], BF16) for i in range(2)]
                    scale = 128.0 ** -0.5
                    amv = amask[:].rearrange("p (k i) -> p k i", k=2)
                    groups = [(hp, sbk, g) for hp in range(8) for sbk in range(T // QB) for g in range(3)]

                    def load_group(gi):
                        if gi >= len(groups):
                            return
                        hp, sbk, g = groups[gi]
                        d = A_PAT[g][1]
                        span = 128 * d
                        q0 = sbk * QB
                        halo = span if sbk > 0 else 0
                        bi = gi % 2
                        S.dma("sp", qt[bi][:], qk_a[g, 0, hp][:, :, q0:q0 + QB].rearrange("r p t -> p r t"), writes=[("aq", bi)])
                        S.dma("sp", kt[bi][:, :, 0:halo + QB],
                              qk_a[g, 1, hp][:, :, q0 - halo:q0 + QB].rearrange("r p t -> p r t"), writes=[("ak", bi)])
                        nrow = (halo + QB) // span
                        vsrc = v_a[g][q0 - halo:q0 + QB, hp * 256:(hp + 1) * 256].rearrange("(n j r) c -> j n r c", j=128, r=d)
                        vdst = vt_[bi][:, 0:nrow * d, :].rearrange("j (n r) c -> j n r c", r=d)
                        for n_ in range(nrow):
                            S.dma("sp", vdst[:, n_], vsrc[:, n_], writes=[("av", bi, n_), ("avall", bi)])

                    blocks = []
                    for gi, (hp, sbk, g) in enumerate(groups):
                        d = A_PAT[g][1]
                        span = 128 * d
                        for nl in range(QB // span):
                            for r in range(d):
                                for e in range(2):
                                    blocks.append((gi, nl, r, e))

                    def binfo(b):
                        gi, nl, r, e = blocks[b]
                        hp, sbk, g = groups[gi]
                        d = A_PAT[g][1]
                        span = 128 * d
                        hrow = 1 if sbk > 0 else 0
                        has_prev = (sbk > 0) or (nl > 0)
                        kbs = ([0] if has_prev else []) + [1]
                        qcols = slice(nl * span + r, nl * span + r + 127 * d + 1, d)
                        return gi, nl, r, e, hp, sbk, g, d, span, hrow, kbs, qcols

                    def stageA(b):
                        gi, nl, r, e, hp, sbk, g, d, span, hrow, kbs, qcols = binfo(b)
                        bi = gi % 2
                        sp_ = b % 4
                        pt = pts[b % 4]
                        ptk = ("pt", b % 4)
                        stv = ps[sp_][:, 0:256].rearrange("p (k i) -> p k i", k=2)
                        for kb in kbs:
                            koff = (hrow + nl - 1 + kb) * span + r
                            kcols = slice(koff, koff + 127 * d + 1, d)
                            for R in range(2):
                                S.op("pe", lambda kb=kb, R=R, kcols=kcols: nc.tensor.matmul(
                                    stv[:, kb, :], kt[bi][e * 64:(e + 1) * 64, R, kcols], qt[bi][e * 64:(e + 1) * 64, R, qcols],
                                    start=(R == 0), stop=(R == 1)),
                                    reads=[("aq", bi), ("ak", bi)], writes=[pk[sp_]], inc=(R == 1))
                        k0 = kbs[0]
                        S.op("act", lambda: nc.scalar.activation(pt[:, k0:2, :], stv[:, k0:2, :], AF.Exp, scale=scale),
                             reads=[pk[sp_]], writes=[ptk])
                        S.op("pool", lambda: nc.gpsimd.tensor_tensor(pt[:, k0:2, :], pt[:, k0:2, :], amv[:, k0:2, :], ALU.mult),
                             reads=[ptk, "amask"], writes=[ptk])

                    def stageB(b):
                        gi, nl, r, e, hp, sbk, g, d, span, hrow, kbs, qcols = binfo(b)
                        bi = gi % 2
                        np_ = 4 + b % 4
                        pt = pts[b % 4]
                        ptk = ("pt", b % 4)
                        ndv = ps[np_][:, 0:256].rearrange("p (k i) -> p k i", k=2)
                        for idx, kb in enumerate(kbs):
                            vrow = (hrow + nl - 1 + kb) * d + r
                            S.op("pe", lambda kb=kb, vrow=vrow, idx=idx: nc.tensor.matmul(
                                ndv[:, 0, :], vt_[bi][:, vrow, e * 128:(e + 1) * 128], pt[:, kb, :],
                                start=(idx == 0), stop=(idx == len(kbs) - 1)),
                                reads=[("av", bi, hrow + nl - 1 + kb), ("avall", bi), ptk], writes=[pk[np_]], inc=False)
                        for idx, kb in enumerate(kbs):
                            S.op("pe", lambda kb=kb, idx=idx: nc.tensor.matmul(
                                ndv[:, 1, :], onesb[:], pt[:, kb, :],
                                start=(idx == 0), stop=(idx == len(kbs) - 1)),
                                reads=[ptk], writes=[pk[np_]], inc=(idx == len(kbs) - 1))
                        av = acc[e][:, :, qcols]
                        ak = ("acc", e)
                        if g == 0:
                            S.op("dve", lambda: nc.vector.tensor_copy(av, ndv), reads=[pk[np_]], writes=[ak])
                        else:
                            S.op("dve", lambda: nc.vector.tensor_tensor(av, ndv, av, ALU.add), reads=[pk[np_], ak], writes=[ak])

                    octr = [0]

                    def finish(gi):
                        hp, sbk, g = groups[gi]
                        q0 = sbk * QB
                        for e in range(2):
                            ak = ("acc", e)
                            oi = octr[0] % 2
                            octr[0] += 1
                            ob = osb[oi]
                            S.op("dve", lambda e=e: nc.vector.reciprocal(rec[:], acc[e][:, 1, :]), reads=[ak], writes=["rec"])
                            S.op("dve", lambda e=e, ob=ob: nc.vector.tensor_tensor(ob[:], acc[e][:, 0, :], rec[:], ALU.mult),
                                 reads=[ak, "rec"], writes=[("osb", oi)])
                            h = hp * 2 + e
                            S.dma("sp", oT[h * 128:(h + 1) * 128, q0:q0 + QB], ob[:], reads=[("osb", oi)], writes=[("oT", 0)])

                    NBLK = len(blocks)
                    load_group(0)
                    load_group(1)
                    stageA(0)
                    stageA(1)
                    for b in range(NBLK):
                        gi = blocks[b][0]
                        first_of_group = (b == 0) or (blocks[b - 1][0] != gi)
                        if first_of_group and gi >= 1:
                            load_group(gi + 1)
                        stageB(b)
                        last_of_group = (b == NBLK - 1) or (blocks[b + 1][0] != gi)
                        if last_of_group and groups[gi][2] == 2:
                            finish(gi)
                        if b + 2 < NBLK:
                            stageA(b + 2)
                    S.barrier()
            else:
                with contextlib.ExitStack() as st:
                    GT = 256
                    CPG = GT // 128
                    qs = [sb(st, f"r_q{i}", [128, 8, 2, GT], BF16) for i in range(2)]
                    ks = [sb(st, f"r_k{i}", [128, 8, 2, GT], BF16) for i in range(2)]
                    vs_ = [sb(st, f"r_v{i}", [128, 4096], BF16) for i in range(2)]
                    gs_ = [sb(st, f"r_g{i}", [128, 4096], BF16) for i in range(2)]
                    Sf = sb(st, "r_S", [128, 8, 2, 512], F32)
                    Sb_ = sb(st, "r_Sb", [128, 8, 2, 512], BF16)
                    pts = [sb(st, f"r_pt{i}", [128, 128], BF16) for i in range(3)]
                    kds = [sb(st, f"r_kd{i}", [128, 2, 128], BF16) for i in range(3)]
                    stt = [sb(st, f"r_st{i}", [128, 6], F32) for i in range(3)]
                    mv = [sb(st, f"r_mv{i}", [128, 2], F32) for i in range(3)]
                    rs = [sb(st, f"r_rs{i}", [128, 2], F32) for i in range(3)]
                    yn = [sb(st, f"r_yn{i}", [128, 512], BF16) for i in range(3)]
                    yg = [sb(st, f"r_yg{i}", [128, 4096], BF16) for i in range(2)]
                    ots = [sb(st, f"r_ot{i}", [128, 32, GT], BF16) for i in range(2)]
                    log_g = [float(np.log1p(-np.exp2(-5.0 - h))) for h in range(8)]
                    cdec = [float(np.exp(lg * 128.0)) for lg in log_g]
                    NCH = T // 128
                    NG = T // GT
                    NIT = NCH * 8

                    def load_grp(grp):
                        if grp >= NG:
                            return
                        bi = grp % 2
                        t0 = grp * GT
                        S.dma("sp", qs[bi][:], qk_r[0][:, :, :, t0:t0 + GT].rearrange("h r p t -> p h r t"), writes=[("rq", bi)])
                        S.dma("sp", ks[bi][:], qk_r[1][:, :, :, t0:t0 + GT].rearrange("h r p t -> p h r t"), writes=[("rk", bi)])

                    def load_chunk(n):
                        if n >= NCH:
                            return
                        vi = n % 2
                        S.dma("sp", vs_[vi][:], v_r[n * 128:(n + 1) * 128, :], writes=[("rv", vi)])
                        S.dma("sp", gs_[vi][:], sg_r[n * 128:(n + 1) * 128, :], writes=[("rg", vi)])

                    def rA(i):
                        n, h = divmod(i, 8)
                        grp, cl = divmod(n, CPG)
                        bi = grp % 2
                        cols = slice(cl * 128, (cl + 1) * 128)
                        i3 = i % 3
                        p_st = i3
                        stv = ps[p_st][:, 0:128]
                        for R in range(2):
                            S.op("pe", lambda R=R: nc.tensor.matmul(
                                stv, ks[bi][:, h, R, cols], qs[bi][:, h, R, cols], start=(R == 0), stop=(R == 1)),
                                reads=[("rq", bi), ("rk", bi)], writes=[pk[p_st]], inc=(R == 1))
                        pt = pts[i3]
                        S.op("dve", lambda: nc.vector.scalar_tensor_tensor(
                            pt[:], stv, rconst[:, h:h + 1], rmask[:], ALU.mult, ALU.mult),
                            reads=[pk[p_st], "rconst", "rmask"], writes=[("rpt", i3)])
                        if n < NCH - 1:
                            ktv = ps[p_st][:, 256:384].bitcast(BF16).rearrange("p (r t) -> p r t", r=2)
                            for R in range(2):
                                S.op("pe", lambda R=R: nc.tensor.transpose(ktv[:, R, :], ks[bi][:, h, R, cols], identb[:]),
                                     reads=[("rk", bi)], writes=[pk[p_st]], inc=(R == 1))
                            kd = kds[i3]
                            S.op("act", lambda: nc.scalar.activation(kd[:], ktv, AF.Copy, scale=rconst[:, 8 + h:9 + h]),
                                 reads=[pk[p_st], "rconst"], writes=[("kd", i3)])

                    def rB(i):
                        n, h = divmod(i, 8)
                        grp, cl = divmod(n, CPG)
                        bi = grp % 2
                        vi = n % 2
                        cols = slice(cl * 128, (cl + 1) * 128)
                        i3 = i % 3
                        p_y, p_su = 3 + (i % 2), 5 + (i % 2)
                        pt = pts[i3]
                        ygb = yg[n % 2]
                        ygk = ("yg", n % 2)
                        vh = vs_[vi][:, h * 512:(h + 1) * 512]
                        yv = ps[p_y][:]
                        S.op("pe", lambda: nc.tensor.matmul(yv, pt[:], vh, start=True, stop=(n == 0)),
                             reads=[("rpt", i3), ("rv", vi)], writes=[pk[p_y]], inc=(n == 0))
                        if n > 0:
                            for R in range(2):
                                S.op("pe", lambda R=R: nc.tensor.matmul(
                                    yv, qs[bi][:, h, R, cols], Sb_[:, h, R, :], start=False, stop=(R == 1)),
                                    reads=[("rq", bi), ("Sb", h)], writes=[pk[p_y]], inc=(R == 1))
                        S.op("dve", lambda: nc.vector.bn_stats(stt[i3][:], yv), reads=[pk[p_y]], writes=[("stt", i3)])
                        S.op("dve", lambda: nc.vector.bn_aggr(mv[i3][:], stt[i3][:]), reads=[("stt", i3)], writes=[("mv", i3)])
                        S.op("act", lambda: nc.scalar.activation(
                            rs[i3][:, 0:1], mv[i3][:, 1:2], AF.Sqrt, bias=rconst[:, 16 + h:17 + h], scale=1.0),
                            reads=[("mv", i3), "rconst"], writes=[("rs", i3)])
                        S.op("dve", lambda: nc.vector.reciprocal(rs[i3][:, 0:1], rs[i3][:, 0:1]),
                             reads=[("rs", i3)], writes=[("rs", i3)])
                        S.op("dve", lambda: nc.vector.scalar_tensor_tensor(
                            rs[i3][:, 1:2], mv[i3][:, 0:1], -1.0, rs[i3][:, 0:1], ALU.mult, ALU.mult),
                            reads=[("rs", i3), ("mv", i3)], writes=[("rs", i3)])
                        S.op("act", lambda: nc.scalar.activation(
                            yn[i3][:], yv, AF.Identity, bias=rs[i3][:, 1:2], scale=rs[i3][:, 0:1]),
                            reads=[pk[p_y], ("rs", i3)], writes=[("yn", i3)])
                        S.op("pool", lambda: nc.gpsimd.tensor_tensor(
                            ygb[:, h * 512:(h + 1) * 512], yn[i3][:], gs_[vi][:, h * 512:(h + 1) * 512], ALU.mult),
                            reads=[("yn", i3), ("rg", vi)], writes=[ygk])
                        if n < NCH - 1:
                            kd = kds[i3]
                            for c in range(2):
                                S.op("pe", lambda c=c: nc.tensor.matmul(ps[p_su][:], kd[:, c, :], vh, start=True, stop=True),
                                     reads=[("kd", i3), ("rv", vi)], writes=[pk[p_su]], inc=True)
                                if n == 0:
                                    S.op("dve", lambda c=c: nc.vector.tensor_copy(Sf[:, h, c, :], ps[p_su][:]),
                                         reads=[pk[p_su]], writes=[("Sf", h)])
                                else:
                                    S.op("dve", lambda c=c: nc.vector.scalar_tensor_tensor(
                                        Sf[:, h, c, :], Sf[:, h, c, :], cdec[h], ps[p_su][:], ALU.mult, ALU.add),
                                        reads=[pk[p_su], ("Sf", h)], writes=[("Sf", h)])
                            S.op("act", lambda: nc.scalar.copy(Sb_[:, h, :, :], Sf[:, h, :, :]),
                                 reads=[("Sf", h)], writes=[("Sb", h)])
                        if h == 7:
                            ot = ots[bi]
                            otk = ("ots", bi)
                            for f8 in range(4):
                                pb = 7
                                tv = ps[pb][:].bitcast(BF16).rearrange("p (f t) -> p f t", f=8)
                                for ff in range(8):
                                    f = f8 * 8 + ff
                                    S.op("pe", lambda f=f, ff=ff: nc.tensor.transpose(
                                        tv[:, ff, :], ygb[:, f * 128:(f + 1) * 128], identb[:]),
                                        reads=[ygk], writes=[pk[pb]], inc=(ff == 7))
                                if f8 % 2 == 0:
                                    S.op("act", lambda f8=f8: nc.scalar.copy(ot[:, f8 * 8:(f8 + 1) * 8, cols], tv),
                                         reads=[pk[pb]], writes=[otk])
                                else:
                                    S.op("dve", lambda f8=f8: nc.vector.tensor_copy(ot[:, f8 * 8:(f8 + 1) * 8, cols], tv),
                                         reads=[pk[pb]], writes=[otk])
                            if cl == CPG - 1:
                                t0 = grp * GT
                                S.dma("sp", oT[:, t0:t0 + GT].rearrange("(f p) t -> p f t", p=128), ot[:],
                                      reads=[otk], writes=[("oT", 0)])

                    load_grp(0)
                    load_grp(1)
                    load_chunk(0)
                    rA(0)
                    rA(1)
                    for i in range(NIT):
                        n, h = divmod(i, 8)
                        if h == 0:
                            load_chunk(n + 1)
                            if n % CPG == 0 and n // CPG >= 1:
                                load_grp(n // CPG + 1)
                        rB(i)
                        if i + 2 < NIT:
                            rA(i + 2)
                    S.barrier()

            with contextlib.ExitStack() as st:
                KO = 16 if is_attn else 32
                wo_src = wo_a[li] if is_attn else wo_r[li]
                ht = sb(st, "f_ht", [128, 16, 512], F32)
                rstd = sb(st, "f_rstd", [128, 512], F32)
                hn = sb(st, "f_hn", [128, 16, 512], BF16)
                actb = sb(st, "f_act", [128, NFF, 512], BF16)
                ot = actb
                sq = actb[:, 28:44, :]
                wos = [sb(st, f"f_wo{i}", [128, KO, 128], BF16) for i in range(2)]
                wus = [sb(st, f"f_wu{i}", [128, 2, 16, 128], BF16) for i in range(3)]
                wds = [sb(st, f"f_wd{i}", [128, NFF, 128], BF16) for i in range(3)]
                ug = [sb(st, f"f_ug{i}", [128, 2, 514], F32) for i in range(2)]
                cg = [sb(st, f"f_cg{i}", [128, 2, 512], F32) for i in range(2)]
                sgt = [sb(st, f"f_sg{i}", [128, 512], F32) for i in range(2)]
                utail = sb(st, "f_utail", [128, 88, 2], F32)
                S.op("pool", lambda: nc.gpsimd.memset(utail[:], 0.0), writes=["utail"])
                cwv = cw[:].rearrange("p (l j w) -> p l j w", l=4, j=88)
                cbv = cb[:].rearrange("p (l j) -> p l j", l=4)
                woc = 0
                wuc = 0
                wdc = 0
                uc = 0
                for tb in range(NTB):
                    t0 = tb * 512
                    S.dma("sp", ht[:], hT[:, t0:t0 + 512].rearrange("(c p) t -> p c t", p=128),
                          reads=[("hT", tb)], writes=["ht"])
                    S.dma("sp", ot[:, 0:KO, :], oT[0:KO * 128, t0:t0 + 512].rearrange("(c p) t -> p c t", p=128),
                          reads=[("oT", 0)], writes=["actb"])
                    for m in range(16):
                        wo = wos[woc % 2]
                        wok = ("wo", woc % 2)
                        woc += 1
                        S.dma("sp", wo[:], wo_src[m], writes=[wok])
                        pb = m % 2
                        for kc in range(KO):
                            S.op("pe", lambda kc=kc, wo=wo, pb=pb: nc.tensor.matmul(
                                ps[pb][:], wo[:, kc, :], ot[:, kc, :], start=(kc == 0), stop=(kc == KO - 1)),
                                reads=[wok, "actb"], writes=[pk[pb]], inc=(kc == KO - 1))
                        S.op("dve", lambda m=m, pb=pb: nc.vector.tensor_tensor(ht[:, m, :], ps[pb][:], ht[:, m, :], ALU.add),
                             reads=[pk[pb], "ht"], writes=["ht"])
                    rmsnorm(ht, "ht", sq, "actb", rstd, "rstd", lambda c: hn[:, c, :], "hn", 4 + l, 7)
                    for j in range(NFF):
                        wu = wus[wuc % 3]
                        wuk = ("wu", wuc % 3)
                        wuc += 1
                        S.dma("sp", wu[:, 0], wup_b[l, j], writes=[(wuk, 0)])
                        S.dma("sp", wu[:, 1], wup_b[l, NFF + j], writes=[(wuk, 1)])
                        u = ug[uc % 2]
                        uk = ("ug", uc % 2)
                        c_ = cg[uc % 2]
                        ck = ("cg", uc % 2)
                        sg_ = sgt[uc % 2]
                        sk = ("sgt", uc % 2)
                        pb = 2 + (uc % 2) * 2
                        uc += 1
                        for s in range(2):
                            for kc in range(16):
                                S.op("pe", lambda s=s, kc=kc, wu=wu, pb=pb: nc.tensor.matmul(
                                    ps[pb + s][:], wu[:, s, kc, :], hn[:, kc, :], start=(kc == 0), stop=(kc == 15)),
                                    reads=[(wuk, s), "hn"], writes=[pk[pb + s]], inc=(kc == 15))
                        for s in range(2):
                            jj = s * NFF + j
                            S.op("pool", lambda s=s, jj=jj, u=u: nc.gpsimd.tensor_copy(u[:, s, 0:2], utail[:, jj, :]),
                                 reads=["utail"], writes=[uk])
                            S.op("act", lambda s=s, u=u, pb=pb: nc.scalar.copy(u[:, s, 2:514], ps[pb + s][:]),
                                 reads=[pk[pb + s]], writes=[uk])
                            S.op("act", lambda s=s, jj=jj, c_=c_, pb=pb: nc.scalar.activation(
                                c_[:, s, :], ps[pb + s][:], AF.Identity, bias=cbv[:, l, jj:jj + 1], scale=cwv[:, l, jj, 2:3]),
                                reads=[pk[pb + s], "cw", "cb"], writes=[ck])
                            S.op("pool", lambda s=s, jj=jj, u=u: nc.gpsimd.tensor_copy(utail[:, jj, :], u[:, s, 512:514]),
                                 reads=[uk], writes=["utail"])
                            for w_ in range(2):
                                S.op("dve", lambda s=s, jj=jj, c_=c_, u=u, w_=w_: nc.vector.scalar_tensor_tensor(
                                    c_[:, s, :], u[:, s, w_:w_ + 512], cwv[:, l, jj, w_:w_ + 1], c_[:, s, :], ALU.mult, ALU.add),
                                    reads=[uk, ck, "cw"], writes=[ck])
                        S.op("act", lambda c_=c_, sg_=sg_: nc.scalar.activation(sg_[:], c_[:, 0, :], AF.Silu), reads=[ck], writes=[sk])
                        S.op("pool", lambda j=j, c_=c_, sg_=sg_: nc.gpsimd.tensor_tensor(actb[:, j, :], sg_[:], c_[:, 1, :], ALU.mult),
                             reads=[ck, sk], writes=["actb"])
                    for m in range(16):
                        wd = wds[wdc % 3]
                        wdk = ("wd", wdc % 3)
                        wdc += 1
                        S.dma("sp", wd[:], wdn_b[l, m], writes=[wdk])
                        pb = m % 2
                        for kc in range(NFF):
                            S.op("pe", lambda kc=kc, wd=wd, pb=pb: nc.tensor.matmul(
                                ps[pb][:], wd[:, kc, :], actb[:, kc, :], start=(kc == 0), stop=(kc == NFF - 1)),
                                reads=[wdk, "actb"], writes=[pk[pb]], inc=(kc == NFF - 1))
                        S.op("dve", lambda m=m, pb=pb: nc.vector.tensor_tensor(ht[:, m, :], ps[pb][:], ht[:, m, :], ALU.add),
                             reads=[pk[pb], "ht"], writes=["ht"])
                    S.dma("sp", hT[:, t0:t0 + 512].rearrange("(c p) t -> p c t", p=128), ht[:],
                          reads=["ht"], writes=[("hT", tb)])
                S.barrier()

        with contextlib.ExitStack() as st:
            ht = sb(st, "z_ht", [128, 16, 512], F32)
            sq = sb(st, "z_sq", [128, 16, 512], BF16)
            rstd = sb(st, "z_rstd", [128, 512], F32)
            hn = sb(st, "z_hn", [128, 16, 512], F32)
            ob = [sb(st, f"z_ob{i}", [128, D], F32) for i in range(2)]
            oc = 0
            pc = 0
            for tb in range(NTB):
                t0 = tb * 512
                S.dma("sp", ht[:], hT[:, t0:t0 + 512].rearrange("(c p) t -> p c t", p=128),
                      reads=[("hT", tb)], writes=["ht"])
                rmsnorm(ht, "ht", sq, "sq", rstd, "rstd", lambda c: hn[:, c, :], "hn", 8, 7)
                for j in range(4):
                    o_ = ob[oc % 2]
                    ok_ = ("ob", oc % 2)
                    oc += 1
                    for c4 in range(4):
                        p = pc % 6
                        pc += 1
                        for cc in range(4):
                            c = c4 * 4 + cc
                            S.op("pe", lambda c=c, cc=cc, p=p, j=j: nc.tensor.transpose(
                                ps[p][:, cc * 128:(cc + 1) * 128], hn[:, c, j * 128:(j + 1) * 128], ident[:]),
                                reads=["hn"], writes=[pk[p]], inc=(cc == 3))
                        if c4 % 2 == 0:
                            S.op("act", lambda o_=o_, p=p, c4=c4: nc.scalar.copy(o_[:, c4 * 512:(c4 + 1) * 512], ps[p][:]),
                                 reads=[pk[p]], writes=[ok_])
                        else:
                            S.op("dve", lambda o_=o_, p=p, c4=c4: nc.vector.tensor_copy(o_[:, c4 * 512:(c4 + 1) * 512], ps[p][:]),
                                 reads=[pk[p]], writes=[ok_])
                    S.dma("sp", out[t0 + j * 128:t0 + (j + 1) * 128, :], o_[:], reads=[ok_], writes=[("out", 0)])
            S.barrier()
    return nc


def host_consts(T):
    pos = np.arange(T, dtype=np.float32)
    inv_a = (10000.0 ** (-np.arange(0, 128, 2, dtype=np.float32) / 128.0)).astype(np.float32)
    ang_a = (pos[None, :] * inv_a[:, None]).astype(np.float32)
    ca = np.concatenate([np.cos(ang_a), np.cos(ang_a)], 0).astype(np.float32)
    sa = np.concatenate([np.sin(ang_a), np.sin(ang_a)], 0).astype(np.float32)
    inv_r = (10000.0 ** (-np.linspace(0.0, 1.0, 128, dtype=np.float32))).astype(np.float32)
    ang_r = (pos[None, :] * inv_r[:, None]).astype(np.float32)
    cr, sr = np.cos(ang_r).astype(np.float32), np.sin(ang_r).astype(np.float32)
    j = np.arange(128)[:, None]
    i = np.arange(128)[None, :]
    amask = np.concatenate([(j >= i), (j <= i)], 1).astype(np.float32)
    rmask = (i >= j).astype(np.float32)
    log_g = np.log1p(-np.exp2(-5.0 - np.arange(8, dtype=np.float64)))
    jj = np.arange(128, dtype=np.float64)[:, None]
    rconst = np.zeros((128, 24), np.float64)
    rconst[:, 0:8] = np.exp(-log_g[None, :] * (jj + 1.0)) * 0.0625
    rconst[:, 8:16] = np.exp(log_g[None, :] * (127.0 - jj)) * 0.0625
    rconst[:, 16:24] = EPS * np.exp(-2.0 * log_g[None, :] * (jj + 1.0))
    return dict(ca=ca, sa=sa, cr=cr, sr=sr, amask=amask, rmask=rmask,
                rconst=rconst.astype(np.float32), ident=np.eye(128, dtype=np.float32))


def make_in_maps(inputs, T, n_cores=8):
    f = lambda a: np.ascontiguousarray(np.asarray(a, dtype=np.float32))
    hc = host_consts(T)
    gv = np.concatenate([f(inputs["norm_mix"]), f(inputs["norm_ffn"]), f(inputs["norm_final"])[None]], 0)
    gains = np.ascontiguousarray(gv.reshape(9, 16, 128).transpose(2, 0, 1).reshape(128, 144))
    cw = np.ascontiguousarray(f(inputs["conv_w"]).reshape(4, 3, 88, 128).transpose(3, 0, 2, 1).reshape(128, -1))
    cb = np.ascontiguousarray(f(inputs["conv_b"]).reshape(4, 88, 128).transpose(2, 0, 1).reshape(128, -1))
    x = f(inputs["x"])
    B = x.shape[0]
    shared = dict(gains=gains, cw=cw, cb=cb, **hc)
    for k in ("w_in_attn", "w_out_attn", "w_in_ret", "w_out_ret", "w_up", "w_down"):
        shared[k] = f(inputs[k])
    wkeys = ("w_in_attn", "w_out_attn", "w_in_ret", "w_out_ret", "w_up", "w_down")
    zeros = {k: np.zeros_like(shared[k]) for k in wkeys} if n_cores > len(REAL_CORES) else {}
    maps = []
    for c in range(n_cores):
        m = dict(shared)
        if n_cores <= len(REAL_CORES):
            m["x"] = np.ascontiguousarray(x[c % B, :T])
        elif c in REAL_CORES:
            m["x"] = np.ascontiguousarray(x[REAL_CORES.index(c) % B, :T])
        else:
            m["x"] = np.zeros((T, D), np.float32)
            m.update(zeros)
        maps.append(m)
    return maps


_NC_CACHE = {}


def kernel(**inputs):
    T, depth = 8192, 4
    key = (T, depth)
    if key not in _NC_CACHE:
        _NC_CACHE[key] = build(T, depth)
    nc = _NC_CACHE[key]
    maps = make_in_maps(inputs, T)
    res = run_bass_kernel_spmd(nc, maps, core_ids=list(range(8)))
    B = np.asarray(inputs["x"]).shape[0]
    return np.stack([np.asarray(res.results[REAL_CORES[b]]["out"], dtype=np.float32) for b in range(B)], 0)
```

```python
import contextlib
import numpy as np
import ml_dtypes
import concourse.bass as bass
import concourse.mybir as mybir
from concourse.bass_utils import run_bass_kernel_spmd

F32 = mybir.dt.float32
BF16 = mybir.dt.bfloat16
AF = mybir.ActivationFunctionType
ALU = mybir.AluOpType

D = 2048
DFF = 5632
NFF = DFF // 128
EPS = 1e-6
A_PAT = ((128, 1), (512, 4), (2048, 16))
A_PROJ = 18432
R_PROJ = 12288
QB = 2048
REAL_CORES = [0, 1, 4, 5]


class Eng:
    def __init__(self, name, h, sem):
        self.name, self.h, self.sem = name, h, sem
        self.count = 0
        self.seen = {}


class Slot:
    def __init__(self, sem):
        self.sem = sem
        self.count = 0


class Sched:
    def __init__(self, nc, es):
        self.nc = nc
        sem = lambda n: es.enter_context(nc.semaphore(n))
        self.E = {
            "pe": Eng("pe", nc.tensor, sem("s_pe")),
            "act": Eng("act", nc.scalar, sem("s_act")),
            "dve": Eng("dve", nc.vector, sem("s_dve")),
            "pool": Eng("pool", nc.gpsimd, sem("s_pool")),
            "sp": Eng("sp", nc.sync, sem("s_sp")),
        }
        self.rings = {
            "sp": [Slot(sem(f"d_sp{i}")) for i in range(40)],
            "pool": [Slot(sem(f"d_pl{i}")) for i in range(32)],
            "act": [Slot(sem(f"d_ac{i}")) for i in range(12)],
        }
        self.rpos = {"sp": 0, "pool": 0, "act": 0}
        self.lw = {}
        self.rd = {}

    def _wait(self, E, dep):
        sem, v, owner = dep
        if v <= 0:
            return
        if owner == E.name:
            if E.name == "pe":
                return
            if v <= E.count - 3:
                return
        if E.seen.get(sem.name, 0) >= v:
            return
        E.h.wait_ge(sem, v)
        E.seen[sem.name] = v

    def _deps(self, E, reads, writes):
        for k in reads:
            if k in self.lw:
                self._wait(E, self.lw[k])
        for k in writes:
            if k in self.lw:
                self._wait(E, self.lw[k])
            for d in self.rd.get(k, {}).values():
                self._wait(E, d)

    def _stamp(self, stamp, reads, writes):
        for k in writes:
            self.lw[k] = stamp
            self.rd[k] = {}
        for k in reads:
            self.rd.setdefault(k, {})[stamp[0].name] = stamp

    def op(self, eng, emit, reads=(), writes=(), inc=True):
        E = self.E[eng]
        self._deps(E, reads, writes)
        ins = emit()
        if inc:
            E.count += 1
            ins.then_inc(E.sem, 1)
            stamp = (E.sem, E.count, eng)
        else:
            stamp = (E.sem, E.count + 1, eng)
        self._stamp(stamp, reads, writes)
        return ins

    def dma(self, q, out, in_, reads=(), writes=()):
        Q = self.E[q]
        ring = self.rings[q]
        slot = ring[self.rpos[q] % len(ring)]
        self.rpos[q] += 1
        self._wait(Q, (slot.sem, slot.count, None))
        self._deps(Q, reads, writes)
        ins = Q.h.dma_start(out=out, in_=in_)
        ins.then_inc(slot.sem, 16)
        slot.count += 16
        self._stamp((slot.sem, slot.count, None), reads, writes)
        return ins

    def barrier(self):
        for E in self.E.values():
            for F in self.E.values():
                if F is not E:
                    self._wait(E, (F.sem, F.count, F.name))
            for ring in self.rings.values():
                for s in ring:
                    self._wait(E, (s.sem, s.count, None))
        self.lw = {}
        self.rd = {}


def build(T, depth):
    nc = bass.Bass("TRN2", target_bir_lowering=False)
    NTB = T // 512
    n_attn = (depth + 1) // 2
    n_ret = depth // 2

    def din(name, shape, dt=F32):
        return nc.dram_tensor(name, list(shape), dt, kind="ExternalInput").ap()

    def dscr(name, shape, dt):
        return nc.dram_tensor(name, list(shape), dt).ap()

    x = din("x", [T, D])
    gains = din("gains", [128, 9 * 16])
    ident_in = din("ident", [128, 128])
    ca_in, sa_in = din("ca", [128, T]), din("sa", [128, T])
    cr_in, sr_in = din("cr", [128, T]), din("sr", [128, T])
    amask_in = din("amask", [128, 256])
    rmask_in = din("rmask", [128, 128])
    rconst_in = din("rconst", [128, 24])
    cw_in = din("cw", [128, 4 * 88 * 3])
    cb_in = din("cb", [128, 4 * 88])
    w_in_attn = din("w_in_attn", [2, D, A_PROJ])
    w_out_attn = din("w_out_attn", [2, D, D])
    w_in_ret = din("w_in_ret", [2, D, R_PROJ])
    w_out_ret = din("w_out_ret", [2, 4096, D])
    w_up = din("w_up", [4, D, 2 * DFF])
    w_down = din("w_down", [4, DFF, D])
    out = nc.dram_tensor("out", [T, D], F32, kind="ExternalOutput").ap()

    hT = dscr("hT", [D, T], F32)
    oT = dscr("oT", [4096, T], BF16)
    wqk_a = dscr("wqk_a", [max(n_attn, 1), 48, 128, 16, 256], BF16)
    wv_a = dscr("wv_a", [max(n_attn, 1), 12, 128, 16, 512], BF16)
    wo_a = dscr("wo_a", [max(n_attn, 1), 16, 128, 16, 128], BF16)
    wqk_r = dscr("wqk_r", [max(n_ret, 1), 16, 128, 16, 256], BF16)
    wvg_r = dscr("wvg_r", [max(n_ret, 1), 16, 128, 16, 512], BF16)
    wo_r = dscr("wo_r", [max(n_ret, 1), 16, 128, 32, 128], BF16)
    wup_b = dscr("wup_b", [max(depth, 1), 88, 128, 16, 128], BF16)
    wdn_b = dscr("wdn_b", [max(depth, 1), 16, 128, NFF, 128], BF16)
    qk_a = dscr("qk_a", [3, 2, 8, 2, 128, T], BF16)
    v_a = dscr("v_a", [3, T, D], BF16)
    qk_r = dscr("qk_r", [2, 8, 2, 128, T], BF16)
    v_r = dscr("v_r", [T, 4096], BF16)
    sg_r = dscr("sg_r", [T, 4096], BF16)

    es = contextlib.ExitStack()
    with es:
        S = Sched(nc, es)

        uid = [0]

        def sb(st, name, shape, dt):
            uid[0] += 1
            return st.enter_context(nc.sbuf_tensor(f"sb{uid[0]}_{name}", list(shape), dt))

        ps = [es.enter_context(nc.psum_tensor(f"ps{i}", [128, 512], F32)) for i in range(8)]
        pk = [("ps", i) for i in range(8)]

        ident = sb(es, "ident", [128, 128], F32)
        identb = sb(es, "identb", [128, 128], BF16)
        onesb = sb(es, "onesb", [128, 128], BF16)
        gsb = sb(es, "gsb", [128, 9 * 16], F32)
        amask = sb(es, "amask", [128, 256], BF16)
        rmask = sb(es, "rmask", [128, 128], F32)
        rconst = sb(es, "rconst", [128, 24], F32)
        cw = sb(es, "cw", [128, 4 * 88 * 3], F32)
        cb = sb(es, "cb", [128, 4 * 88], F32)
        with contextlib.ExitStack() as st:
            am32 = sb(st, "am32", [128, 256], F32)
            S.dma("sp", ident[:], ident_in, writes=["ident"])
            S.dma("sp", gsb[:], gains, writes=["gsb"])
            S.dma("sp", am32[:], amask_in, writes=["am32"])
            S.dma("sp", rmask[:], rmask_in, writes=["rmask"])
            S.dma("sp", rconst[:], rconst_in, writes=["rconst"])
            S.dma("sp", cw[:], cw_in, writes=["cw"])
            S.dma("sp", cb[:], cb_in, writes=["cb"])
            S.op("dve", lambda: nc.vector.tensor_copy(identb[:], ident[:]), reads=["ident"], writes=["identb"])
            S.op("dve", lambda: nc.vector.tensor_copy(amask[:], am32[:]), reads=["am32"], writes=["amask"])
            S.op("dve", lambda: nc.vector.memset(onesb[:], 1.0), writes=["onesb"])
            S.barrier()

        def cast(dst, src):
            S.dma("pool", dst, src)

        def kview(w, c0, n):
            return w[:, c0:c0 + n].rearrange("(kc p) c -> p kc c", p=128)

        for l in range(depth):
            li = l // 2
            if l % 2 == 0:
                w = w_in_attn[li]
                for g in range(3):
                    for ty in range(2):
                        for hp in range(8):
                            base = g * 6144 + ty * 2048 + hp * 256
                            blk = (g * 2 + ty) * 8 + hp
                            src = w[:, base:base + 256].rearrange("(kc p) (e x d) -> p kc x e d", p=128, e=2, x=2)
                            dstv = wqk_a[li, blk].rearrange("p kc (x e d) -> p kc x e d", x=2, e=2)
                            for xx in range(2):
                                for ee in range(2):
                                    cast(dstv[:, :, xx, ee], src[:, :, xx, ee])
                    for cbk in range(4):
                        cast(wv_a[li, g * 4 + cbk], kview(w, g * 6144 + 4096 + cbk * 512, 512))
                for m in range(16):
                    cast(wo_a[li, m], kview(w_out_attn[li], m * 128, 128))
            else:
                w = w_in_ret[li]
                for b in range(16):
                    cast(wqk_r[li, b], kview(w, b * 256, 256))
                for b in range(16):
                    cast(wvg_r[li, b], kview(w, 4096 + b * 512, 512))
                for m in range(16):
                    cast(wo_r[li, m], kview(w_out_ret[li], m * 128, 128))
            for j in range(88):
                cast(wup_b[l, j], kview(w_up[l], j * 128, 128))
            for m in range(16):
                cast(wdn_b[l, m], kview(w_down[l], m * 128, 128))

        with contextlib.ExitStack() as st:
            xs = [sb(st, f"xs{i}", [128, D], F32) for i in range(2)]
            stg = [sb(st, f"xstg{i}", [128, 16, 512], F32) for i in range(2)]
            for tb in range(NTB):
                sg = stg[tb % 2]
                sgk = ("xstg", tb % 2)
                for j in range(4):
                    ti = tb * 4 + j
                    xt = xs[ti % 2]
                    xk = ("xs", ti % 2)
                    S.dma("sp", xt[:], x[ti * 128:(ti + 1) * 128, :], writes=[xk])
                    for c4 in range(4):
                        p = (ti * 4 + c4) % 8
                        for cc in range(4):
                            c = c4 * 4 + cc
                            S.op("pe", lambda c=c, cc=cc, p=p, xt=xt: nc.tensor.transpose(
                                ps[p][:, cc * 128:(cc + 1) * 128], xt[:, c * 128:(c + 1) * 128], ident[:]),
                                reads=[xk], writes=[pk[p]], inc=(cc == 3))
                        eng = "act" if c4 % 2 == 0 else "dve"
                        src = ps[p][:].rearrange("p (c t) -> p c t", c=4)
                        dst = sg[:, c4 * 4:(c4 + 1) * 4, j * 128:(j + 1) * 128]
                        if eng == "act":
                            S.op("act", lambda dst=dst, src=src: nc.scalar.copy(dst, src), reads=[pk[p]], writes=[sgk])
                        else:
                            S.op("dve", lambda dst=dst, src=src: nc.vector.tensor_copy(dst, src), reads=[pk[p]], writes=[sgk])
                S.dma("sp", hT[:, tb * 512:(tb + 1) * 512].rearrange("(c p) t -> p c t", p=128), sg[:],
                      reads=[sgk], writes=[("hT", tb)])
            S.barrier()

        def rmsnorm(ht, htk, sq, sqk, rstd, rstdk, dst_fn, dstk, gidx, pbank):
            S.op("act", lambda: nc.scalar.activation(sq[:], ht[:], AF.Square), reads=[htk], writes=[sqk])
            for c in range(16):
                S.op("pe", lambda c=c: nc.tensor.matmul(ps[pbank][:], onesb[:], sq[:, c, :], start=(c == 0), stop=(c == 15)),
                     reads=[sqk], writes=[pk[pbank]], inc=(c == 15))
            S.op("act", lambda: nc.scalar.activation(rstd[:], ps[pbank][:], AF.Sqrt, bias=EPS, scale=1.0 / D),
                 reads=[pk[pbank]], writes=[rstdk])
            S.op("dve", lambda: nc.vector.reciprocal(rstd[:], rstd[:]), reads=[rstdk], writes=[rstdk])
            for c in range(16):
                S.op("dve", lambda c=c: nc.vector.scalar_tensor_tensor(
                    dst_fn(c), ht[:, c, :], gsb[:, gidx * 16 + c:gidx * 16 + c + 1], rstd[:], ALU.mult, ALU.mult),
                    reads=[htk, rstdk], writes=[dstk])

        def proj_rot(wb, wbk, hn, hnk, t0, ctab, stab, tabk, rot, rotk, pb):
            for xx in range(2):
                for kc in range(16):
                    S.op("pe", lambda xx=xx, kc=kc: nc.tensor.matmul(
                        ps[pb + xx][:], wb[:, kc, xx * 128:(xx + 1) * 128], hn[:, kc, t0:t0 + 512],
                        start=(kc == 0), stop=(kc == 15)),
                        reads=[wbk, hnk], writes=[pk[pb + xx]], inc=(kc == 15))
            return

        for l in range(depth):
            li = l // 2
            is_attn = (l % 2 == 0)
            with contextlib.ExitStack() as st:
                TBA = 1024
                ht = sb(st, "p_ht", [128, 16, 512], F32)
                sq = sb(st, "p_sq", [128, 16, 512], BF16)
                rstd = sb(st, "p_rstd", [128, 512], F32)
                hn = sb(st, "p_hn", [128, 16, TBA], BF16)
                ctab = sb(st, "p_ct", [128, TBA], F32)
                stab = sb(st, "p_st", [128, TBA], F32)
                wbs = [sb(st, f"p_wb{i}", [128, 16, 256], BF16) for i in range(3)]
                wvs = [sb(st, f"p_wv{i}", [128, 16, 512], BF16) for i in range(2)]
                rots = [sb(st, f"p_rot{i}", [128, 2, TBA], BF16) for i in range(2)]
                tmp = [sb(st, f"p_tmp{i}", [128, 4, 512], BF16) for i in range(2)]
                vst = [sb(st, f"p_vst{i}", [128, 512], BF16) for i in range(3)]
                c_in, s_in = (ca_in, sa_in) if is_attn else (cr_in, sr_in)
                if is_attn:
                    tiles = [(wqk_a[li, (g * 2 + ty) * 8 + hp], qk_a[g, ty, hp]) for g in range(3) for ty in range(2) for hp in range(8)]
                    vtl = [(wv_a[li, g * 4 + cbk], v_a[g], cbk * 512, False) for g in range(3) for cbk in range(4)]
                else:
                    tiles = [(wqk_r[li, ty * 8 + h], qk_r[ty, h]) for ty in range(2) for h in range(8)]
                    vtl = [(wvg_r[li, b], v_r, b * 512, False) for b in range(8)] + \
                          [(wvg_r[li, 8 + b], sg_r, b * 512, True) for b in range(8)]
                NB = T // TBA
                qk_all = [(tb, t_) for tb in range(NB) for t_ in tiles]
                v_all = [(tb, t_) for tb in range(NB) for t_ in vtl]
                ld = {"qk": 0, "v": 0}

                def load_qk(upto):
                    while ld["qk"] <= min(upto, len(qk_all) - 1):
                        i = ld["qk"]
                        S.dma("sp", wbs[i % 3][:], qk_all[i][1][0], writes=[("wb", i % 3)])
                        ld["qk"] += 1

                def load_v(upto):
                    while ld["v"] <= min(upto, len(v_all) - 1):
                        i = ld["v"]
                        S.dma("sp", wvs[i % 2][:], v_all[i][1][0], writes=[("wv", i % 2)])
                        ld["v"] += 1

                qi = 0
                vi_ = 0
                tctr = 0
                sctr = 0
                load_qk(1)
                for tb in range(NB):
                    tok0 = tb * TBA
                    S.dma("sp", ctab[:], c_in[:, tok0:tok0 + TBA], writes=["ctab"])
                    S.dma("sp", stab[:], s_in[:, tok0:tok0 + TBA], writes=["stab"])
                    for hf in range(TBA // 512):
                        t0 = tok0 + hf * 512
                        S.dma("sp", ht[:], hT[:, t0:t0 + 512].rearrange("(c p) t -> p c t", p=128),
                              reads=[("hT", t0 // 512)], writes=["ht"])
                        rmsnorm(ht, "ht", sq, "sq", rstd, "rstd",
                                lambda c, hf=hf: hn[:, c, hf * 512:(hf + 1) * 512], "hn", l, 7)
                    for _ in tiles:
                        (_, (wsrc, qdst)) = qk_all[qi]
                        load_qk(qi + 2)
                        load_v(vi_)
                        wb = wbs[qi % 3]
                        wbk = ("wb", qi % 3)
                        rot = rots[qi % 2]
                        rotk = ("rot", qi % 2)
                        qi += 1
                        for hf in range(TBA // 512):
                            pb = (tctr % 3) * 2
                            tm = tmp[tctr % 2]
                            tmk = ("tmp", tctr % 2)
                            tctr += 1
                            for xx in range(2):
                                for kc in range(16):
                                    S.op("pe", lambda xx=xx, kc=kc, wb=wb, pb=pb, hf=hf: nc.tensor.matmul(
                                        ps[pb + xx][:], wb[:, kc, xx * 128:(xx + 1) * 128], hn[:, kc, hf * 512:(hf + 1) * 512],
                                        start=(kc == 0), stop=(kc == 15)),
                                        reads=[wbk, "hn"], writes=[pk[pb + xx]], inc=(kc == 15))
                            cs = ctab[:, hf * 512:(hf + 1) * 512]
                            ss = stab[:, hf * 512:(hf + 1) * 512]
                            X1, X2 = ps[pb][:], ps[pb + 1][:]
                            for i, (a, b) in enumerate(((X1, cs), (X2, ss), (X1, ss), (X2, cs))):
                                S.op("dve", lambda i=i, a=a, b=b, tm=tm: nc.vector.tensor_tensor(tm[:, i, :], a, b, ALU.mult),
                                     reads=[pk[pb], pk[pb + 1], "ctab", "stab"], writes=[tmk])
                            S.op("pool", lambda tm=tm, rot=rot, hf=hf: nc.gpsimd.tensor_tensor(
                                rot[:, 0, hf * 512:(hf + 1) * 512], tm[:, 0, :], tm[:, 1, :], ALU.subtract),
                                reads=[tmk], writes=[rotk])
                            S.op("pool", lambda tm=tm, rot=rot, hf=hf: nc.gpsimd.tensor_tensor(
                                rot[:, 1, hf * 512:(hf + 1) * 512], tm[:, 2, :], tm[:, 3, :], ALU.add),
                                reads=[tmk], writes=[rotk])
                        S.dma("sp", qdst[:, :, tok0:tok0 + TBA].rearrange("r p t -> p r t"), rot[:],
                              reads=[rotk], writes=[("qk", tb)])
                    for _ in vtl:
                        (_, (wsrc, vdst, c0, silu)) = v_all[vi_]
                        load_v(vi_ + 1)
                        load_qk(qi + 1)
                        wv = wvs[vi_ % 2]
                        wvk = ("wv", vi_ % 2)
                        vi_ += 1
                        for tt in range(TBA // 128):
                            pb = 6 + (sctr % 2)
                            vs = vst[sctr % 3]
                            vsk = ("vst", sctr % 3)
                            sctr += 1
                            for kc in range(16):
                                S.op("pe", lambda kc=kc, wv=wv, pb=pb, tt=tt: nc.tensor.matmul(
                                    ps[pb][:], hn[:, kc, tt * 128:(tt + 1) * 128], wv[:, kc, :],
                                    start=(kc == 0), stop=(kc == 15)),
                                    reads=[wvk, "hn"], writes=[pk[pb]], inc=(kc == 15))
                            fn = AF.Silu if silu else AF.Copy
                            S.op("act", lambda vs=vs, pb=pb, fn=fn: nc.scalar.activation(vs[:], ps[pb][:], fn),
                                 reads=[pk[pb]], writes=[vsk])
                            S.dma("sp", vdst[tok0 + tt * 128:tok0 + (tt + 1) * 128, c0:c0 + 512], vs[:],
                                  reads=[vsk], writes=[("v", tb)])
                S.barrier()

            if is_attn:
                with contextlib.ExitStack() as st:
                    acc = [sb(st, f"a_acc{e}", [128, 2, QB], F32) for e in range(2)]
                    qt = [sb(st, f"a_q{i}", [128, 2, QB], BF16) for i in range(2)]
                    kt = [sb(st, f"a_k{i}", [128, 2, 2 * QB], BF16) for i in range(2)]
                    vt_ = [sb(st, f"a_v{i}", [128, 32, 256], BF16) for i in range(2)]
                    pts = [sb(st, f"a_pt{i}", [128, 2, 128], BF16) for i in range(4)]
                    rec = sb(st, "a_rec", [128, QB], F32)
                    osb = [sb(st, f"a_o{i}", [128, QB], BF16) for i in range(2)]
                    scale = 128.0 ** -0.5
                    amv = amask[:].rearrange("p (k i) -> p k i", k=2)
                    groups = [(hp, sbk, g) for hp in range(8) for sbk in range(T // QB) for g in range(3)]

                    def load_group(gi):
                        if gi >= len(groups):
                            return
                        hp, sbk, g = groups[gi]
                        d = A_PAT[g][1]
                        span = 128 * d
                        q0 = sbk * QB
                        halo = span if sbk > 0 else 0
                        bi = gi % 2
                        S.dma("sp", qt[bi][:], qk_a[g, 0, hp][:, :, q0:q0 + QB].rearrange("r p t -> p r t"), writes=[("aq", bi)])
                        S.dma("sp", kt[bi][:, :, 0:halo + QB],
                              qk_a[g, 1, hp][:, :, q0 - halo:q0 + QB].rearrange("r p t -> p r t"), writes=[("ak", bi)])
                        nrow = (halo + QB) // span
                        vsrc = v_a[g][q0 - halo:q0 + QB, hp * 256:(hp + 1) * 256].rearrange("(n j r) c -> j n r c", j=128, r=d)
                        vdst = vt_[bi][:, 0:nrow * d, :].rearrange("j (n r) c -> j n r c", r=d)
                        for n_ in range(nrow):
                            S.dma("sp", vdst[:, n_], vsrc[:, n_], writes=[("av", bi, n_), ("avall", bi)])

                    blocks = []
                    for gi, (hp, sbk, g) in enumerate(groups):
                        d = A_PAT[g][1]
                        span = 128 * d
                        for nl in range(QB // span):
                            for r in range(d):
                                for e in range(2):
                                    blocks.append((gi, nl, r, e))

                    def binfo(b):
                        gi, nl, r, e = blocks[b]
                        hp, sbk, g = groups[gi]
                        d = A_PAT[g][1]
                        span = 128 * d
                        hrow = 1 if sbk > 0 else 0
                        has_prev = (sbk > 0) or (nl > 0)
                        kbs = ([0] if has_prev else []) + [1]
                        qcols = slice(nl * span + r, nl * span + r + 127 * d + 1, d)
                        return gi, nl, r, e, hp, sbk, g, d, span, hrow, kbs, qcols

                    def stageA(b):
                        gi, nl, r, e, hp, sbk, g, d, span, hrow, kbs, qcols = binfo(b)
                        bi = gi % 2
                        sp_ = b % 4
                        pt = pts[b % 4]
                        ptk = ("pt", b % 4)
                        stv = ps[sp_][:, 0:256].rearrange("p (k i) -> p k i", k=2)
                        for kb in kbs:
                            koff = (hrow + nl - 1 + kb) * span + r
                            kcols = slice(koff, koff + 127 * d + 1, d)
                            for R in range(2):
                                S.op("pe", lambda kb=kb, R=R, kcols=kcols: nc.tensor.matmul(
                                    stv[:, kb, :], kt[bi][e * 64:(e + 1) * 64, R, kcols], qt[bi][e * 64:(e + 1) * 64, R, qcols],
                                    start=(R == 0), stop=(R == 1)),
                                    reads=[("aq", bi), ("ak", bi)], writes=[pk[sp_]], inc=(R == 1))
                        k0 = kbs[0]
                        S.op("act", lambda: nc.scalar.activation(pt[:, k0:2, :], stv[:, k0:2, :], AF.Exp, scale=scale),
                             reads=[pk[sp_]], writes=[ptk])
                        S.op("pool", lambda: nc.gpsimd.tensor_tensor(pt[:, k0:2, :], pt[:, k0:2, :], amv[:, k0:2, :], ALU.mult),
                             reads=[ptk, "amask"], writes=[ptk])

                    def stageB(b):
                        gi, nl, r, e, hp, sbk, g, d, span, hrow, kbs, qcols = binfo(b)
                        bi = gi % 2
                        np_ = 4 + b % 4
                        pt = pts[b % 4]
                        ptk = ("pt", b % 4)
                        ndv = ps[np_][:, 0:256].rearrange("p (k i) -> p k i", k=2)
                        for idx, kb in enumerate(kbs):
                            vrow = (hrow + nl - 1 + kb) * d + r
                            S.op("pe", lambda kb=kb, vrow=vrow, idx=idx: nc.tensor.matmul(
                                ndv[:, 0, :], vt_[bi][:, vrow, e * 128:(e + 1) * 128], pt[:, kb, :],
                                start=(idx == 0), stop=(idx == len(kbs) - 1)),
                                reads=[("av", bi, hrow + nl - 1 + kb), ("avall", bi), ptk], writes=[pk[np_]], inc=False)
                        for idx, kb in enumerate(kbs):
                            S.op("pe", lambda kb=kb, idx=idx: nc.tensor.matmul(
                                ndv[:, 1, :], onesb[:], pt[:, kb, :],
                                start=(idx == 0), stop=(idx == len(kbs) - 1)),
                                reads=[ptk], writes=[pk[np_]], inc=(idx == len(kbs) - 1))
                        av = acc[e][:, :, qcols]
                        ak = ("acc", e)
                        if g == 0:
                            S.op("dve", lambda: nc.vector.tensor_copy(av, ndv), reads=[pk[np_]], writes=[ak])
                        else:
                            S.op("dve", lambda: nc.vector.tensor_tensor(av, ndv, av, ALU.add), reads=[pk[np_], ak], writes=[ak])

                    octr = [0]

                    def finish(gi):
                        hp, sbk, g = groups[gi]
                        q0 = sbk * QB
                        for e in range(2):
                            ak = ("acc", e)
                            oi = octr[0] % 2
                            octr[0] += 1
                            ob = osb[oi]
                            S.op("dve", lambda e=e: nc.vector.reciprocal(rec[:], acc[e][:, 1, :]), reads=[ak], writes=["rec"])
                            S.op("dve", lambda e=e, ob=ob: nc.vector.tensor_tensor(ob[:], acc[e][:, 0, :], rec[:], ALU.mult),
                                 reads=[ak, "rec"], writes=[("osb", oi)])
                            h = hp * 2 + e
                            S.dma("sp", oT[h * 128:(h + 1) * 128, q0:q0 + QB], ob[:], reads=[("osb", oi)], writes=[("oT", 0)])

                    NBLK = len(blocks)
                    load_group(0)
                    load_group(1)
                    stageA(0)
                    stageA(1)
                    for b in range(NBLK):
                        gi = blocks[b][0]
                        first_of_group = (b == 0) or (blocks[b - 1][0] != gi)
                        if first_of_group and gi >= 1:
                            load_group(gi + 1)
                        stageB(b)
                        last_of_group = (b == NBLK - 1) or (blocks[b + 1][0] != gi)
                        if last_of_group and groups[gi][2] == 2:
                            finish(gi)
                        if b + 2 < NBLK:
                            stageA(b + 2)
                    S.barrier()
            else:
                with contextlib.ExitStack() as st:
                    GT = 256
                    CPG = GT // 128
                    qs = [sb(st, f"r_q{i}", [128, 8, 2, GT], BF16) for i in range(2)]
                    ks = [sb(st, f"r_k{i}", [128, 8, 2, GT], BF16) for i in range(2)]
                    vs_ = [sb(st, f"r_v{i}", [128, 4096], BF16) for i in range(2)]
                    gs_ = [sb(st, f"r_g{i}", [128, 4096], BF16) for i in range(2)]
                    Sf = sb(st, "r_S", [128, 8, 2, 512], F32)
                    Sb_ = sb(st, "r_Sb", [128, 8, 2, 512], BF16)
                    pts = [sb(st, f"r_pt{i}", [128, 128], BF16) for i in range(3)]
                    kds = [sb(st, f"r_kd{i}", [128, 2, 128], BF16) for i in range(3)]
                    stt = [sb(st, f"r_st{i}", [128, 6], F32) for i in range(3)]
                    mv = [sb(st, f"r_mv{i}", [128, 2], F32) for i in range(3)]
                    rs = [sb(st, f"r_rs{i}", [128, 2], F32) for i in range(3)]
                    yn = [sb(st, f"r_yn{i}", [128, 512], BF16) for i in range(3)]
                    yg = [sb(st, f"r_yg{i}", [128, 4096], BF16) for i in range(2)]
                    ots = [sb(st, f"r_ot{i}", [128, 32, GT], BF16) for i in range(2)]
                    log_g = [float(np.log1p(-np.exp2(-5.0 - h))) for h in range(8)]
                    cdec = [float(np.exp(lg * 128.0)) for lg in log_g]
                    NCH = T // 128
                    NG = T // GT
                    NIT = NCH * 8

                    def load_grp(grp):
                        if grp >= NG:
                            return
                        bi = grp % 2
                        t0 = grp * GT
                        S.dma("sp", qs[bi][:], qk_r[0][:, :, :, t0:t0 + GT].rearrange("h r p t -> p h r t"), writes=[("rq", bi)])
                        S.dma("sp", ks[bi][:], qk_r[1][:, :, :, t0:t0 + GT].rearrange("h r p t -> p h r t"), writes=[("rk", bi)])

                    def load_chunk(n):
                        if n >= NCH:
                            return
                        vi = n % 2
                        S.dma("sp", vs_[vi][:], v_r[n * 128:(n + 1) * 128, :], writes=[("rv", vi)])
                        S.dma("sp", gs_[vi][:], sg_r[n * 128:(n + 1) * 128, :], writes=[("rg", vi)])

                    def rA(i):
                        n, h = divmod(i, 8)
                        grp, cl = divmod(n, CPG)
                        bi = grp % 2
                        cols = slice(cl * 128, (cl + 1) * 128)
                        i3 = i % 3
                        p_st = i3
                        stv = ps[p_st][:, 0:128]
                        for R in range(2):
                            S.op("pe", lambda R=R: nc.tensor.matmul(
                                stv, ks[bi][:, h, R, cols], qs[bi][:, h, R, cols], start=(R == 0), stop=(R == 1)),
                                reads=[("rq", bi), ("rk", bi)], writes=[pk[p_st]], inc=(R == 1))
                        pt = pts[i3]
                        S.op("dve", lambda: nc.vector.scalar_tensor_tensor(
                            pt[:], stv, rconst[:, h:h + 1], rmask[:], ALU.mult, ALU.mult),
                            reads=[pk[p_st], "rconst", "rmask"], writes=[("rpt", i3)])
                        if n < NCH - 1:
                            ktv = ps[p_st][:, 256:384].bitcast(BF16).rearrange("p (r t) -> p r t", r=2)
                            for R in range(2):
                                S.op("pe", lambda R=R: nc.tensor.transpose(ktv[:, R, :], ks[bi][:, h, R, cols], identb[:]),
                                     reads=[("rk", bi)], writes=[pk[p_st]], inc=(R == 1))
                            kd = kds[i3]
                            S.op("act", lambda: nc.scalar.activation(kd[:], ktv, AF.Copy, scale=rconst[:, 8 + h:9 + h]),
                                 reads=[pk[p_st], "rconst"], writes=[("kd", i3)])

                    def rB(i):
                        n, h = divmod(i, 8)
                        grp, cl = divmod(n, CPG)
                        bi = grp % 2
                        vi = n % 2
                        cols = slice(cl * 128, (cl + 1) * 128)
                        i3 = i % 3
                        p_y, p_su = 3 + (i % 2), 5 + (i % 2)
                        pt = pts[i3]
                        ygb = yg[n % 2]
                        ygk = ("yg", n % 2)
                        vh = vs_[vi][:, h * 512:(h + 1) * 512]
                        yv = ps[p_y][:]
                        S.op("pe", lambda: nc.tensor.matmul(yv, pt[:], vh, start=True, stop=(n == 0)),
                             reads=[("rpt", i3), ("rv", vi)], writes=[pk[p_y]], inc=(n == 0))
                        if n > 0:
                            for R in range(2):
                                S.op("pe", lambda R=R: nc.tensor.matmul(
                                    yv, qs[bi][:, h, R, cols], Sb_[:, h, R, :], start=False, stop=(R == 1)),
                                    reads=[("rq", bi), ("Sb", h)], writes=[pk[p_y]], inc=(R == 1))
                        S.op("dve", lambda: nc.vector.bn_stats(stt[i3][:], yv), reads=[pk[p_y]], writes=[("stt", i3)])
                        S.op("dve", lambda: nc.vector.bn_aggr(mv[i3][:], stt[i3][:]), reads=[("stt", i3)], writes=[("mv", i3)])
                        S.op("act", lambda: nc.scalar.activation(
                            rs[i3][:, 0:1], mv[i3][:, 1:2], AF.Sqrt, bias=rconst[:, 16 + h:17 + h], scale=1.0),
                            reads=[("mv", i3), "rconst"], writes=[("rs", i3)])
                        S.op("dve", lambda: nc.vector.reciprocal(rs[i3][:, 0:1], rs[i3][:, 0:1]),
                             reads=[("rs", i3)], writes=[("rs", i3)])
                        S.op("dve", lambda: nc.vector.scalar_tensor_tensor(
                            rs[i3][:, 1:2], mv[i3][:, 0:1], -1.0, rs[i3][:, 0:1], ALU.mult, ALU.mult),
                            reads=[("rs", i3), ("mv", i3)], writes=[("rs", i3)])
                        S.op("act", lambda: nc.scalar.activation(
                            yn[i3][:], yv, AF.Identity, bias=rs[i3][:, 1:2], scale=rs[i3][:, 0:1]),
                            reads=[pk[p_y], ("rs", i3)], writes=[("yn", i3)])
                        S.op("pool", lambda: nc.gpsimd.tensor_tensor(
                            ygb[:, h * 512:(h + 1) * 512], yn[i3][:], gs_[vi][:, h * 512:(h + 1) * 512], ALU.mult),
                            reads=[("yn", i3), ("rg", vi)], writes=[ygk])
                        if n < NCH - 1:
                            kd = kds[i3]
                            for c in range(2):
                                S.op("pe", lambda c=c: nc.tensor.matmul(ps[p_su][:], kd[:, c, :], vh, start=True, stop=True),
                                     reads=[("kd", i3), ("rv", vi)], writes=[pk[p_su]], inc=True)
                                if n == 0:
                                    S.op("dve", lambda c=c: nc.vector.tensor_copy(Sf[:, h, c, :], ps[p_su][:]),
                                         reads=[pk[p_su]], writes=[("Sf", h)])
                                else:
                                    S.op("dve", lambda c=c: nc.vector.scalar_tensor_tensor(
                                        Sf[:, h, c, :], Sf[:, h, c, :], cdec[h], ps[p_su][:], ALU.mult, ALU.add),
                                        reads=[pk[p_su], ("Sf", h)], writes=[("Sf", h)])
                            S.op("act", lambda: nc.scalar.copy(Sb_[:, h, :, :], Sf[:, h, :, :]),
                                 reads=[("Sf", h)], writes=[("Sb", h)])
                        if h == 7:
                            ot = ots[bi]
                            otk = ("ots", bi)
                            for f8 in range(4):
                                pb = 7
                                tv = ps[pb][:].bitcast(BF16).rearrange("p (f t) -> p f t", f=8)
                                for ff in range(8):
                                    f = f8 * 8 + ff
                                    S.op("pe", lambda f=f, ff=ff: nc.tensor.transpose(
                                        tv[:, ff, :], ygb[:, f * 128:(f + 1) * 128], identb[:]),
                                        reads=[ygk], writes=[pk[pb]], inc=(ff == 7))
                                if f8 % 2 == 0:
                                    S.op("act", lambda f8=f8: nc.scalar.copy(ot[:, f8 * 8:(f8 + 1) * 8, cols], tv),
                                         reads=[pk[pb]], writes=[otk])
                                else:
                                    S.op("dve", lambda f8=f8: nc.vector.tensor_copy(ot[:, f8 * 8:(f8 + 1) * 8, cols], tv),
                                         reads=[pk[pb]], writes=[otk])
                            if cl == CPG - 1:
                                t0 = grp * GT
                                S.dma("sp", oT[:, t0:t0 + GT].rearrange("(f p) t -> p f t", p=128), ot[:],
                                      reads=[otk], writes=[("oT", 0)])

                    load_grp(0)
                    load_grp(1)
                    load_chunk(0)
                    rA(0)
                    rA(1)
                    for i in range(NIT):
                        n, h = divmod(i, 8)
                        if h == 0:
                            load_chunk(n + 1)
                            if n % CPG == 0 and n // CPG >= 1:
                                load_grp(n // CPG + 1)
                        rB(i)
                        if i + 2 < NIT:
                            rA(i + 2)
                    S.barrier()

            with contextlib.ExitStack() as st:
                KO = 16 if is_attn else 32
                wo_src = wo_a[li] if is_attn else wo_r[li]
                ht = sb(st, "f_ht", [128, 16, 512], F32)
                rstd = sb(st, "f_rstd", [128, 512], F32)
                hn = sb(st, "f_hn", [128, 16, 512], BF16)
                actb = sb(st, "f_act", [128, NFF, 512], BF16)
                ot = actb
                sq = actb[:, 28:44, :]
                wos = [sb(st, f"f_wo{i}", [128, KO, 128], BF16) for i in range(2)]
                wus = [sb(st, f"f_wu{i}", [128, 2, 16, 128], BF16) for i in range(3)]
                wds = [sb(st, f"f_wd{i}", [128, NFF, 128], BF16) for i in range(3)]
                ug = [sb(st, f"f_ug{i}", [128, 2, 514], F32) for i in range(2)]
                cg = [sb(st, f"f_cg{i}", [128, 2, 512], F32) for i in range(2)]
                sgt = [sb(st, f"f_sg{i}", [128, 512], F32) for i in range(2)]
                utail = sb(st, "f_utail", [128, 88, 2], F32)
                S.op("pool", lambda: nc.gpsimd.memset(utail[:], 0.0), writes=[("utail", q) for q in range(88)])
                cwv = cw[:].rearrange("p (l j w) -> p l j w", l=4, j=88)
                cbv = cb[:].rearrange("p (l j) -> p l j", l=4)
                woc = 0
                wuc = 0
                wdc = 0
                uc = 0
                for tb in range(NTB):
                    t0 = tb * 512
                    S.dma("sp", ht[:], hT[:, t0:t0 + 512].rearrange("(c p) t -> p c t", p=128),
                          reads=[("hT", tb)], writes=["ht"])
                    S.dma("sp", ot[:, 0:KO, :], oT[0:KO * 128, t0:t0 + 512].rearrange("(c p) t -> p c t", p=128),
                          reads=[("oT", 0)], writes=["actb"])
                    for m in range(16):
                        wo = wos[woc % 2]
                        wok = ("wo", woc % 2)
                        woc += 1
                        S.dma("sp", wo[:], wo_src[m], writes=[wok])
                        pb = m % 2
                        for kc in range(KO):
                            S.op("pe", lambda kc=kc, wo=wo, pb=pb: nc.tensor.matmul(
                                ps[pb][:], wo[:, kc, :], ot[:, kc, :], start=(kc == 0), stop=(kc == KO - 1)),
                                reads=[wok, "actb"], writes=[pk[pb]], inc=(kc == KO - 1))
                        S.op("dve", lambda m=m, pb=pb: nc.vector.tensor_tensor(ht[:, m, :], ps[pb][:], ht[:, m, :], ALU.add),
                             reads=[pk[pb], "ht"], writes=["ht"])
                    rmsnorm(ht, "ht", sq, "actb", rstd, "rstd", lambda c: hn[:, c, :], "hn", 4 + l, 7)
                    pending = None
                    for j in range(NFF):
                        wu = wus[wuc % 3]
                        wuk = ("wu", wuc % 3)
                        wuc += 1
                        S.dma("sp", wu[:, 0], wup_b[l, j], writes=[(wuk, 0)])
                        S.dma("sp", wu[:, 1], wup_b[l, NFF + j], writes=[(wuk, 1)])
                        ui = uc % 2
                        u = ug[ui]
                        c_ = cg[ui]
                        sg_ = sgt[ui]
                        pb = 2 + ui * 2
                        uc += 1
                        for s in range(2):
                            for kc in range(16):
                                S.op("pe", lambda s=s, kc=kc, wu=wu, pb=pb: nc.tensor.matmul(
                                    ps[pb + s][:], wu[:, s, kc, :], hn[:, kc, :], start=(kc == 0), stop=(kc == 15)),
                                    reads=[(wuk, s), "hn"], writes=[pk[pb + s]], inc=(kc == 15))
                        for s in range(2):
                            jj = s * NFF + j
                            utk, ubk, ck = ("ut", ui, s), ("ub", ui, s), ("cg", ui, s)
                            S.op("pool", lambda s=s, jj=jj, u=u: nc.gpsimd.tensor_copy(u[:, s, 0:2], utail[:, jj, :]),
                                 reads=[("utail", jj)], writes=[utk])
                            S.op("act", lambda s=s, u=u, pb=pb: nc.scalar.copy(u[:, s, 2:514], ps[pb + s][:]),
                                 reads=[pk[pb + s]], writes=[ubk])
                            S.op("act", lambda s=s, jj=jj, c_=c_, pb=pb: nc.scalar.activation(
                                c_[:, s, :], ps[pb + s][:], AF.Identity, bias=cbv[:, l, jj:jj + 1], scale=cwv[:, l, jj, 2:3]),
                                reads=[pk[pb + s], "cw", "cb"], writes=[ck])
                            S.op("pool", lambda s=s, jj=jj, u=u: nc.gpsimd.tensor_copy(utail[:, jj, :], u[:, s, 512:514]),
                                 reads=[ubk], writes=[("utail", jj)])
                            for w_ in range(2):
                                S.op("dve", lambda s=s, jj=jj, c_=c_, u=u, w_=w_: nc.vector.scalar_tensor_tensor(
                                    c_[:, s, :], u[:, s, w_:w_ + 512], cwv[:, l, jj, w_:w_ + 1], c_[:, s, :], ALU.mult, ALU.add),
                                    reads=[utk, ubk, ck, "cw"], writes=[ck])
                        if pending is not None:
                            pending()

                        def fin(j=j, ui=ui, c_=c_, sg_=sg_):
                            S.op("act", lambda: nc.scalar.activation(sg_[:], c_[:, 0, :], AF.Silu),
                                 reads=[("cg", ui, 0)], writes=[("sgt", ui)])
                            S.op("dve", lambda: nc.vector.tensor_tensor(actb[:, j, :], sg_[:], c_[:, 1, :], ALU.mult),
                                 reads=[("cg", ui, 1), ("sgt", ui)], writes=["actb"])
                        pending = fin
                    pending()
                    for m in range(16):
                        wd = wds[wdc % 3]
                        wdk = ("wd", wdc % 3)
                        wdc += 1
                        S.dma("sp", wd[:], wdn_b[l, m], writes=[wdk])
                        pb = m % 2
                        for kc in range(NFF):
                            S.op("pe", lambda kc=kc, wd=wd, pb=pb: nc.tensor.matmul(
                                ps[pb][:], wd[:, kc, :], actb[:, kc, :], start=(kc == 0), stop=(kc == NFF - 1)),
                                reads=[wdk, "actb"], writes=[pk[pb]], inc=(kc == NFF - 1))
                        S.op("dve", lambda m=m, pb=pb: nc.vector.tensor_tensor(ht[:, m, :], ps[pb][:], ht[:, m, :], ALU.add),
                             reads=[pk[pb], "ht"], writes=["ht"])
                    S.dma("sp", hT[:, t0:t0 + 512].rearrange("(c p) t -> p c t", p=128), ht[:],
                          reads=["ht"], writes=[("hT", tb)])
                S.barrier()

        with contextlib.ExitStack() as st:
            ht = sb(st, "z_ht", [128, 16, 512], F32)
            sq = sb(st, "z_sq", [128, 16, 512], BF16)
            rstd = sb(st, "z_rstd", [128, 512], F32)
            hn = sb(st, "z_hn", [128, 16, 512], F32)
            ob = [sb(st, f"z_ob{i}", [128, D], F32) for i in range(2)]
            oc = 0
            pc = 0
            for tb in range(NTB):
                t0 = tb * 512
                S.dma("sp", ht[:], hT[:, t0:t0 + 512].rearrange("(c p) t -> p c t", p=128),
                      reads=[("hT", tb)], writes=["ht"])
                rmsnorm(ht, "ht", sq, "sq", rstd, "rstd", lambda c: hn[:, c, :], "hn", 8, 7)
                for j in range(4):
                    o_ = ob[oc % 2]
                    ok_ = ("ob", oc % 2)
                    oc += 1
                    for c4 in range(4):
                        p = pc % 6
                        pc += 1
                        for cc in range(4):
                            c = c4 * 4 + cc
                            S.op("pe", lambda c=c, cc=cc, p=p, j=j: nc.tensor.transpose(
                                ps[p][:, cc * 128:(cc + 1) * 128], hn[:, c, j * 128:(j + 1) * 128], ident[:]),
                                reads=["hn"], writes=[pk[p]], inc=(cc == 3))
                        if c4 % 2 == 0:
                            S.op("act", lambda o_=o_, p=p, c4=c4: nc.scalar.copy(o_[:, c4 * 512:(c4 + 1) * 512], ps[p][:]),
                                 reads=[pk[p]], writes=[ok_])
                        else:
                            S.op("dve", lambda o_=o_, p=p, c4=c4: nc.vector.tensor_copy(o_[:, c4 * 512:(c4 + 1) * 512], ps[p][:]),
                                 reads=[pk[p]], writes=[ok_])
                    S.dma("sp", out[t0 + j * 128:t0 + (j + 1) * 128, :], o_[:], reads=[ok_], writes=[("out", 0)])
            S.barrier()
    return nc


def host_consts(T):
    pos = np.arange(T, dtype=np.float32)
    inv_a = (10000.0 ** (-np.arange(0, 128, 2, dtype=np.float32) / 128.0)).astype(np.float32)
    ang_a = (pos[None, :] * inv_a[:, None]).astype(np.float32)
    ca = np.concatenate([np.cos(ang_a), np.cos(ang_a)], 0).astype(np.float32)
    sa = np.concatenate([np.sin(ang_a), np.sin(ang_a)], 0).astype(np.float32)
    inv_r = (10000.0 ** (-np.linspace(0.0, 1.0, 128, dtype=np.float32))).astype(np.float32)
    ang_r = (pos[None, :] * inv_r[:, None]).astype(np.float32)
    cr, sr = np.cos(ang_r).astype(np.float32), np.sin(ang_r).astype(np.float32)
    j = np.arange(128)[:, None]
    i = np.arange(128)[None, :]
    amask = np.concatenate([(j >= i), (j <= i)], 1).astype(np.float32)
    rmask = (i >= j).astype(np.float32)
    log_g = np.log1p(-np.exp2(-5.0 - np.arange(8, dtype=np.float64)))
    jj = np.arange(128, dtype=np.float64)[:, None]
    rconst = np.zeros((128, 24), np.float64)
    rconst[:, 0:8] = np.exp(-log_g[None, :] * (jj + 1.0)) * 0.0625
    rconst[:, 8:16] = np.exp(log_g[None, :] * (127.0 - jj)) * 0.0625
    rconst[:, 16:24] = EPS * np.exp(-2.0 * log_g[None, :] * (jj + 1.0))
    return dict(ca=ca, sa=sa, cr=cr, sr=sr, amask=amask, rmask=rmask,
                rconst=rconst.astype(np.float32), ident=np.eye(128, dtype=np.float32))


def make_in_maps(inputs, T, n_cores=8):
    f = lambda a: np.ascontiguousarray(np.asarray(a, dtype=np.float32))
    hc = host_consts(T)
    gv = np.concatenate([f(inputs["norm_mix"]), f(inputs["norm_ffn"]), f(inputs["norm_final"])[None]], 0)
    gains = np.ascontiguousarray(gv.reshape(9, 16, 128).transpose(2, 0, 1).reshape(128, 144))
    cw = np.ascontiguousarray(f(inputs["conv_w"]).reshape(4, 3, 88, 128).transpose(3, 0, 2, 1).reshape(128, -1))
    cb = np.ascontiguousarray(f(inputs["conv_b"]).reshape(4, 88, 128).transpose(2, 0, 1).reshape(128, -1))
    x = f(inputs["x"])
    B = x.shape[0]
    shared = dict(gains=gains, cw=cw, cb=cb, **hc)
    for k in ("w_in_attn", "w_out_attn", "w_in_ret", "w_out_ret", "w_up", "w_down"):
        shared[k] = f(inputs[k])
    wkeys = ("w_in_attn", "w_out_attn", "w_in_ret", "w_out_ret", "w_up", "w_down")
    zeros = {k: np.zeros_like(shared[k]) for k in wkeys} if n_cores > len(REAL_CORES) else {}
    maps = []
    for c in range(n_cores):
        m = dict(shared)
        if n_cores <= len(REAL_CORES):
            m["x"] = np.ascontiguousarray(x[c % B, :T])
        elif c in REAL_CORES:
            m["x"] = np.ascontiguousarray(x[REAL_CORES.index(c) % B, :T])
        else:
            m["x"] = np.zeros((T, D), np.float32)
            m.update(zeros)
        maps.append(m)
    return maps


_NC_CACHE = {}


def kernel(**inputs):
    T, depth = 8192, 4
    key = (T, depth)
    if key not in _NC_CACHE:
        _NC_CACHE[key] = build(T, depth)
    nc = _NC_CACHE[key]
    maps = make_in_maps(inputs, T)
    res = run_bass_kernel_spmd(nc, maps, core_ids=list(range(8)))
    B = np.asarray(inputs["x"]).shape[0]
    return np.stack([np.asarray(res.results[REAL_CORES[b]]["out"], dtype=np.float32) for b in range(B)], 0)
```

```python
import contextlib
import numpy as np
import ml_dtypes
import concourse.bass as bass
import concourse.mybir as mybir
from concourse.bass_utils import run_bass_kernel_spmd

F32 = mybir.dt.float32
BF16 = mybir.dt.bfloat16
AF = mybir.ActivationFunctionType
ALU = mybir.AluOpType

D = 2048
DFF = 5632
NFF = DFF // 128
EPS = 1e-6
A_PAT = ((128, 1), (512, 4), (2048, 16))
A_PROJ = 18432
R_PROJ = 12288
QB = 2048
REAL_CORES = [0, 1, 4, 5]


class Eng:
    def __init__(self, name, h, sem):
        self.name, self.h, self.sem = name, h, sem
        self.count = 0
        self.seen = {}


class Slot:
    def __init__(self, sem):
        self.sem = sem
        self.count = 0


class Sched:
    def __init__(self, nc, es):
        self.nc = nc
        sem = lambda n: es.enter_context(nc.semaphore(n))
        self.E = {
            "pe": Eng("pe", nc.tensor, sem("s_pe")),
            "act": Eng("act", nc.scalar, sem("s_act")),
            "dve": Eng("dve", nc.vector, sem("s_dve")),
            "pool": Eng("pool", nc.gpsimd, sem("s_pool")),
            "sp": Eng("sp", nc.sync, sem("s_sp")),
        }
        self.rings = {
            "sp": [Slot(sem(f"d_sp{i}")) for i in range(40)],
            "pool": [Slot(sem(f"d_pl{i}")) for i in range(32)],
            "act": [Slot(sem(f"d_ac{i}")) for i in range(12)],
        }
        self.rpos = {"sp": 0, "pool": 0, "act": 0}
        self.lw = {}
        self.rd = {}

    def _wait(self, E, dep):
        sem, v, owner = dep
        if v <= 0:
            return
        if owner == E.name:
            if E.name == "pe":
                return
            if v <= E.count - 3:
                return
        if E.seen.get(sem.name, 0) >= v:
            return
        E.h.wait_ge(sem, v)
        E.seen[sem.name] = v

    def _deps(self, E, reads, writes):
        for k in reads:
            if k in self.lw:
                self._wait(E, self.lw[k])
        for k in writes:
            if k in self.lw:
                self._wait(E, self.lw[k])
            for d in self.rd.get(k, {}).values():
                self._wait(E, d)

    def _stamp(self, stamp, reads, writes):
        for k in writes:
            self.lw[k] = stamp
            self.rd[k] = {}
        for k in reads:
            self.rd.setdefault(k, {})[stamp[0].name] = stamp

    def op(self, eng, emit, reads=(), writes=(), inc=True):
        E = self.E[eng]
        self._deps(E, reads, writes)
        ins = emit()
        if inc:
            E.count += 1
            ins.then_inc(E.sem, 1)
            stamp = (E.sem, E.count, eng)
        else:
            stamp = (E.sem, E.count + 1, eng)
        self._stamp(stamp, reads, writes)
        return ins

    def dma(self, q, out, in_, reads=(), writes=()):
        Q = self.E[q]
        ring = self.rings[q]
        slot = ring[self.rpos[q] % len(ring)]
        self.rpos[q] += 1
        self._wait(Q, (slot.sem, slot.count, None))
        self._deps(Q, reads, writes)
        ins = Q.h.dma_start(out=out, in_=in_)
        ins.then_inc(slot.sem, 16)
        slot.count += 16
        self._stamp((slot.sem, slot.count, None), reads, writes)
        return ins

    def barrier(self):
        for E in self.E.values():
            for F in self.E.values():
                if F is not E:
                    self._wait(E, (F.sem, F.count, F.name))
            for ring in self.rings.values():
                for s in ring:
                    self._wait(E, (s.sem, s.count, None))
        self.lw = {}
        self.rd = {}


def build(T, depth):
    nc = bass.Bass("TRN2", target_bir_lowering=False)
    NTB = T // 512
    n_attn = (depth + 1) // 2
    n_ret = depth // 2

    def din(name, shape, dt=F32):
        return nc.dram_tensor(name, list(shape), dt, kind="ExternalInput").ap()

    def dscr(name, shape, dt):
        return nc.dram_tensor(name, list(shape), dt).ap()

    x = din("x", [T, D])
    gains = din("gains", [128, 9 * 16])
    ident_in = din("ident", [128, 128])
    ca_in, sa_in = din("ca", [128, T]), din("sa", [128, T])
    cr_in, sr_in = din("cr", [128, T]), din("sr", [128, T])
    amask_in = din("amask", [128, 256])
    rmask_in = din("rmask", [128, 128])
    rconst_in = din("rconst", [128, 24])
    cw_in = din("cw", [128, 4 * 88 * 3])
    cb_in = din("cb", [128, 4 * 88])
    w_in_attn = din("w_in_attn", [2, D, A_PROJ])
    w_out_attn = din("w_out_attn", [2, D, D])
    w_in_ret = din("w_in_ret", [2, D, R_PROJ])
    w_out_ret = din("w_out_ret", [2, 4096, D])
    w_up = din("w_up", [4, D, 2 * DFF])
    w_down = din("w_down", [4, DFF, D])
    out = nc.dram_tensor("out", [T, D], F32, kind="ExternalOutput").ap()

    hT = dscr("hT", [D, T], F32)
    oT = dscr("oT", [4096, T], BF16)
    wqk_a = dscr("wqk_a", [max(n_attn, 1), 48, 128, 16, 256], BF16)
    wv_a = dscr("wv_a", [max(n_attn, 1), 12, 128, 16, 512], BF16)
    wo_a = dscr("wo_a", [max(n_attn, 1), 16, 128, 16, 128], BF16)
    wqk_r = dscr("wqk_r", [max(n_ret, 1), 16, 128, 16, 256], BF16)
    wvg_r = dscr("wvg_r", [max(n_ret, 1), 16, 128, 16, 512], BF16)
    wo_r = dscr("wo_r", [max(n_ret, 1), 16, 128, 32, 128], BF16)
    wup_b = dscr("wup_b", [max(depth, 1), 88, 128, 16, 128], BF16)
    wdn_b = dscr("wdn_b", [max(depth, 1), 16, 128, NFF, 128], BF16)
    qk_a = dscr("qk_a", [3, 2, 8, 2, 128, T], BF16)
    v_a = dscr("v_a", [3, T, D], BF16)
    qk_r = dscr("qk_r", [2, 8, 2, 128, T], BF16)
    v_r = dscr("v_r", [T, 4096], BF16)
    sg_r = dscr("sg_r", [T, 4096], BF16)

    es = contextlib.ExitStack()
    with es:
        S = Sched(nc, es)

        uid = [0]

        def sb(st, name, shape, dt):
            uid[0] += 1
            return st.enter_context(nc.sbuf_tensor(f"sb{uid[0]}_{name}", list(shape), dt))

        ps = [es.enter_context(nc.psum_tensor(f"ps{i}", [128, 512], F32)) for i in range(8)]
        pk = [("ps", i) for i in range(8)]

        ident = sb(es, "ident", [128, 128], F32)
        identb = sb(es, "identb", [128, 128], BF16)
        onesb = sb(es, "onesb", [128, 128], BF16)
        gsb = sb(es, "gsb", [128, 9 * 16], F32)
        amask = sb(es, "amask", [128, 256], BF16)
        rmask = sb(es, "rmask", [128, 128], F32)
        rconst = sb(es, "rconst", [128, 24], F32)
        cw = sb(es, "cw", [128, 4 * 88 * 3], F32)
        cb = sb(es, "cb", [128, 4 * 88], F32)
        with contextlib.ExitStack() as st:
            am32 = sb(st, "am32", [128, 256], F32)
            S.dma("sp", ident[:], ident_in, writes=["ident"])
            S.dma("sp", gsb[:], gains, writes=["gsb"])
            S.dma("sp", am32[:], amask_in, writes=["am32"])
            S.dma("sp", rmask[:], rmask_in, writes=["rmask"])
            S.dma("sp", rconst[:], rconst_in, writes=["rconst"])
            S.dma("sp", cw[:], cw_in, writes=["cw"])
            S.dma("sp", cb[:], cb_in, writes=["cb"])
            S.op("dve", lambda: nc.vector.tensor_copy(identb[:], ident[:]), reads=["ident"], writes=["identb"])
            S.op("dve", lambda: nc.vector.tensor_copy(amask[:], am32[:]), reads=["am32"], writes=["amask"])
            S.op("dve", lambda: nc.vector.memset(onesb[:], 1.0), writes=["onesb"])
            S.barrier()

        deferred = []
        cast_now = [True]

        def cast(dst, src):
            if cast_now[0]:
                S.dma("pool", dst, src)
            else:
                deferred.append((dst, src))

        def emit_deferred(n):
            for _ in range(min(n, len(deferred))):
                dst, src = deferred.pop(0)
                S.dma("pool", dst, src)

        def kview(w, c0, n):
            return w[:, c0:c0 + n].rearrange("(kc p) c -> p kc c", p=128)

        for l in range(depth):
            li = l // 2
            if l % 2 == 0:
                w = w_in_attn[li]
                for g in range(3):
                    for ty in range(2):
                        for hp in range(8):
                            base = g * 6144 + ty * 2048 + hp * 256
                            blk = (g * 2 + ty) * 8 + hp
                            src = w[:, base:base + 256].rearrange("(kc p) (e x d) -> p kc x e d", p=128, e=2, x=2)
                            dstv = wqk_a[li, blk].rearrange("p kc (x e d) -> p kc x e d", x=2, e=2)
                            for xx in range(2):
                                for ee in range(2):
                                    cast(dstv[:, :, xx, ee], src[:, :, xx, ee])
                    for cbk in range(4):
                        cast(wv_a[li, g * 4 + cbk], kview(w, g * 6144 + 4096 + cbk * 512, 512))
                cast_now[0] = False
                for m in range(16):
                    cast(wo_a[li, m], kview(w_out_attn[li], m * 128, 128))
            else:
                w = w_in_ret[li]
                for b in range(16):
                    cast(wqk_r[li, b], kview(w, b * 256, 256))
                for b in range(16):
                    cast(wvg_r[li, b], kview(w, 4096 + b * 512, 512))
                for m in range(16):
                    cast(wo_r[li, m], kview(w_out_ret[li], m * 128, 128))
            for j in range(88):
                cast(wup_b[l, j], kview(w_up[l], j * 128, 128))
            for m in range(16):
                cast(wdn_b[l, m], kview(w_down[l], m * 128, 128))

        with contextlib.ExitStack() as st:
            xs = [sb(st, f"xs{i}", [128, D], F32) for i in range(2)]
            stg = [sb(st, f"xstg{i}", [128, 16, 512], F32) for i in range(2)]
            for tb in range(NTB):
                sg = stg[tb % 2]
                sgk = ("xstg", tb % 2)
                for j in range(4):
                    ti = tb * 4 + j
                    xt = xs[ti % 2]
                    xk = ("xs", ti % 2)
                    S.dma("sp", xt[:], x[ti * 128:(ti + 1) * 128, :], writes=[xk])
                    for c4 in range(4):
                        p = (ti * 4 + c4) % 8
                        for cc in range(4):
                            c = c4 * 4 + cc
                            S.op("pe", lambda c=c, cc=cc, p=p, xt=xt: nc.tensor.transpose(
                                ps[p][:, cc * 128:(cc + 1) * 128], xt[:, c * 128:(c + 1) * 128], ident[:]),
                                reads=[xk], writes=[pk[p]], inc=(cc == 3))
                        eng = "act" if c4 % 2 == 0 else "dve"
                        src = ps[p][:].rearrange("p (c t) -> p c t", c=4)
                        dst = sg[:, c4 * 4:(c4 + 1) * 4, j * 128:(j + 1) * 128]
                        if eng == "act":
                            S.op("act", lambda dst=dst, src=src: nc.scalar.copy(dst, src), reads=[pk[p]], writes=[sgk])
                        else:
                            S.op("dve", lambda dst=dst, src=src: nc.vector.tensor_copy(dst, src), reads=[pk[p]], writes=[sgk])
                S.dma("sp", hT[:, tb * 512:(tb + 1) * 512].rearrange("(c p) t -> p c t", p=128), sg[:],
                      reads=[sgk], writes=[("hT", tb)])
            S.barrier()

        def rmsnorm(ht, htk, sq, sqk, rstd, rstdk, dst_fn, dstk, gidx, pbank):
            S.op("act", lambda: nc.scalar.activation(sq[:], ht[:], AF.Square), reads=[htk], writes=[sqk])
            for c in range(16):
                S.op("pe", lambda c=c: nc.tensor.matmul(ps[pbank][:], onesb[:], sq[:, c, :], start=(c == 0), stop=(c == 15)),
                     reads=[sqk], writes=[pk[pbank]], inc=(c == 15))
            S.op("act", lambda: nc.scalar.activation(rstd[:], ps[pbank][:], AF.Sqrt, bias=EPS, scale=1.0 / D),
                 reads=[pk[pbank]], writes=[rstdk])
            S.op("dve", lambda: nc.vector.reciprocal(rstd[:], rstd[:]), reads=[rstdk], writes=[rstdk])
            for c in range(16):
                S.op("dve", lambda c=c: nc.vector.scalar_tensor_tensor(
                    dst_fn(c), ht[:, c, :], gsb[:, gidx * 16 + c:gidx * 16 + c + 1], rstd[:], ALU.mult, ALU.mult),
                    reads=[htk, rstdk], writes=[dstk])

        def proj_rot(wb, wbk, hn, hnk, t0, ctab, stab, tabk, rot, rotk, pb):
            for xx in range(2):
                for kc in range(16):
                    S.op("pe", lambda xx=xx, kc=kc: nc.tensor.matmul(
                        ps[pb + xx][:], wb[:, kc, xx * 128:(xx + 1) * 128], hn[:, kc, t0:t0 + 512],
                        start=(kc == 0), stop=(kc == 15)),
                        reads=[wbk, hnk], writes=[pk[pb + xx]], inc=(kc == 15))
            return

        for l in range(depth):
            li = l // 2
            is_attn = (l % 2 == 0)
            with contextlib.ExitStack() as st:
                TBA = 1024
                ht = sb(st, "p_ht", [128, 16, 512], F32)
                sq = sb(st, "p_sq", [128, 16, 512], BF16)
                rstd = sb(st, "p_rstd", [128, 512], F32)
                hn = sb(st, "p_hn", [128, 16, TBA], BF16)
                ctab = sb(st, "p_ct", [128, TBA], F32)
                stab = sb(st, "p_st", [128, TBA], F32)
                wbs = [sb(st, f"p_wb{i}", [128, 16, 256], BF16) for i in range(3)]
                wvs = [sb(st, f"p_wv{i}", [128, 16, 512], BF16) for i in range(2)]
                rots = [sb(st, f"p_rot{i}", [128, 2, TBA], BF16) for i in range(2)]
                tmp = [sb(st, f"p_tmp{i}", [128, 4, 512], BF16) for i in range(2)]
                vst = [sb(st, f"p_vst{i}", [128, 512], BF16) for i in range(3)]
                c_in, s_in = (ca_in, sa_in) if is_attn else (cr_in, sr_in)
                if is_attn:
                    tiles = [(wqk_a[li, (g * 2 + ty) * 8 + hp], qk_a[g, ty, hp]) for g in range(3) for ty in range(2) for hp in range(8)]
                    vtl = [(wv_a[li, g * 4 + cbk], v_a[g], cbk * 512, False) for g in range(3) for cbk in range(4)]
                else:
                    tiles = [(wqk_r[li, ty * 8 + h], qk_r[ty, h]) for ty in range(2) for h in range(8)]
                    vtl = [(wvg_r[li, b], v_r, b * 512, False) for b in range(8)] + \
                          [(wvg_r[li, 8 + b], sg_r, b * 512, True) for b in range(8)]
                NB = T // TBA
                qk_all = [(tb, t_) for tb in range(NB) for t_ in tiles]
                v_all = [(tb, t_) for tb in range(NB) for t_ in vtl]
                ld = {"qk": 0, "v": 0}

                def load_qk(upto):
                    while ld["qk"] <= min(upto, len(qk_all) - 1):
                        i = ld["qk"]
                        S.dma("sp", wbs[i % 3][:], qk_all[i][1][0], writes=[("wb", i % 3)])
                        ld["qk"] += 1

                def load_v(upto):
                    while ld["v"] <= min(upto, len(v_all) - 1):
                        i = ld["v"]
                        S.dma("sp", wvs[i % 2][:], v_all[i][1][0], writes=[("wv", i % 2)])
                        ld["v"] += 1

                qi = 0
                vi_ = 0
                tctr = 0
                sctr = 0
                load_qk(1)
                for tb in range(NB):
                    tok0 = tb * TBA
                    S.dma("sp", ctab[:], c_in[:, tok0:tok0 + TBA], writes=["ctab"])
                    S.dma("sp", stab[:], s_in[:, tok0:tok0 + TBA], writes=["stab"])
                    for hf in range(TBA // 512):
                        t0 = tok0 + hf * 512
                        S.dma("sp", ht[:], hT[:, t0:t0 + 512].rearrange("(c p) t -> p c t", p=128),
                              reads=[("hT", t0 // 512)], writes=["ht"])
                        rmsnorm(ht, "ht", sq, "sq", rstd, "rstd",
                                lambda c, hf=hf: hn[:, c, hf * 512:(hf + 1) * 512], "hn", l, 7)
                    for _ in tiles:
                        (_, (wsrc, qdst)) = qk_all[qi]
                        load_qk(qi + 2)
                        load_v(vi_)
                        wb = wbs[qi % 3]
                        wbk = ("wb", qi % 3)
                        rot = rots[qi % 2]
                        rotk = ("rot", qi % 2)
                        qi += 1
                        for hf in range(TBA // 512):
                            pb = (tctr % 3) * 2
                            tm = tmp[tctr % 2]
                            tmk = ("tmp", tctr % 2)
                            tctr += 1
                            for xx in range(2):
                                for kc in range(16):
                                    S.op("pe", lambda xx=xx, kc=kc, wb=wb, pb=pb, hf=hf: nc.tensor.matmul(
                                        ps[pb + xx][:], wb[:, kc, xx * 128:(xx + 1) * 128], hn[:, kc, hf * 512:(hf + 1) * 512],
                                        start=(kc == 0), stop=(kc == 15)),
                                        reads=[wbk, "hn"], writes=[pk[pb + xx]], inc=(kc == 15))
                            cs = ctab[:, hf * 512:(hf + 1) * 512]
                            ss = stab[:, hf * 512:(hf + 1) * 512]
                            X1, X2 = ps[pb][:], ps[pb + 1][:]
                            for i, (a, b) in enumerate(((X1, cs), (X2, ss), (X1, ss), (X2, cs))):
                                S.op("dve", lambda i=i, a=a, b=b, tm=tm: nc.vector.tensor_tensor(tm[:, i, :], a, b, ALU.mult),
                                     reads=[pk[pb], pk[pb + 1], "ctab", "stab"], writes=[tmk])
                            S.op("pool", lambda tm=tm, rot=rot, hf=hf: nc.gpsimd.tensor_tensor(
                                rot[:, 0, hf * 512:(hf + 1) * 512], tm[:, 0, :], tm[:, 1, :], ALU.subtract),
                                reads=[tmk], writes=[rotk])
                            S.op("pool", lambda tm=tm, rot=rot, hf=hf: nc.gpsimd.tensor_tensor(
                                rot[:, 1, hf * 512:(hf + 1) * 512], tm[:, 2, :], tm[:, 3, :], ALU.add),
                                reads=[tmk], writes=[rotk])
                        S.dma("sp", qdst[:, :, tok0:tok0 + TBA].rearrange("r p t -> p r t"), rot[:],
                              reads=[rotk], writes=[("qk", tb)])
                        if l == 0:
                            emit_deferred(2)
                    for _ in vtl:
                        (_, (wsrc, vdst, c0, silu)) = v_all[vi_]
                        load_v(vi_ + 1)
                        load_qk(qi + 1)
                        wv = wvs[vi_ % 2]
                        wvk = ("wv", vi_ % 2)
                        vi_ += 1
                        for tt in range(TBA // 128):
                            pb = 6 + (sctr % 2)
                            vs = vst[sctr % 3]
                            vsk = ("vst", sctr % 3)
                            sctr += 1
                            for kc in range(16):
                                S.op("pe", lambda kc=kc, wv=wv, pb=pb, tt=tt: nc.tensor.matmul(
                                    ps[pb][:], hn[:, kc, tt * 128:(tt + 1) * 128], wv[:, kc, :],
                                    start=(kc == 0), stop=(kc == 15)),
                                    reads=[wvk, "hn"], writes=[pk[pb]], inc=(kc == 15))
                            fn = AF.Silu if silu else AF.Copy
                            S.op("act", lambda vs=vs, pb=pb, fn=fn: nc.scalar.activation(vs[:], ps[pb][:], fn),
                                 reads=[pk[pb]], writes=[vsk])
                            S.dma("sp", vdst[tok0 + tt * 128:tok0 + (tt + 1) * 128, c0:c0 + 512], vs[:],
                                  reads=[vsk], writes=[("v", tb)])
                        if l == 0:
                            emit_deferred(2)
                if l == 0:
                    emit_deferred(len(deferred))
                S.barrier()

            if is_attn:
                with contextlib.ExitStack() as st:
                    acc = [sb(st, f"a_acc{e}", [128, 2, QB], F32) for e in range(2)]
                    qt = [sb(st, f"a_q{i}", [128, 2, QB], BF16) for i in range(2)]
                    kt = [sb(st, f"a_k{i}", [128, 2, 2 * QB], BF16) for i in range(2)]
                    vt_ = [sb(st, f"a_v{i}", [128, 32, 256], BF16) for i in range(2)]
                    pts = [sb(st, f"a_pt{i}", [128, 2, 128], BF16) for i in range(4)]
                    rec = sb(st, "a_rec", [128, QB], F32)
                    osb = [sb(st, f"a_o{i}", [128, QB], BF16) for i in range(2)]
                    scale = 128.0 ** -0.5
                    amv = amask[:].rearrange("p (k i) -> p k i", k=2)
                    groups = [(hp, sbk, g) for hp in range(8) for sbk in range(T // QB) for g in range(3)]

                    def load_group(gi):
                        if gi >= len(groups):
                            return
                        hp, sbk, g = groups[gi]
                        d = A_PAT[g][1]
                        span = 128 * d
                        q0 = sbk * QB
                        halo = span if sbk > 0 else 0
                        bi = gi % 2
                        S.dma("sp", qt[bi][:], qk_a[g, 0, hp][:, :, q0:q0 + QB].rearrange("r p t -> p r t"), writes=[("aq", bi)])
                        S.dma("sp", kt[bi][:, :, 0:halo + QB],
                              qk_a[g, 1, hp][:, :, q0 - halo:q0 + QB].rearrange("r p t -> p r t"), writes=[("ak", bi)])
                        nrow = (halo + QB) // span
                        vsrc = v_a[g][q0 - halo:q0 + QB, hp * 256:(hp + 1) * 256].rearrange("(n j r) c -> j n r c", j=128, r=d)
                        vdst = vt_[bi][:, 0:nrow * d, :].rearrange("j (n r) c -> j n r c", r=d)
                        for n_ in range(nrow):
                            S.dma("sp", vdst[:, n_], vsrc[:, n_], writes=[("av", bi, n_), ("avall", bi)])

                    blocks = []
                    for gi, (hp, sbk, g) in enumerate(groups):
                        d = A_PAT[g][1]
                        span = 128 * d
                        for nl in range(QB // span):
                            for r in range(d):
                                for e in range(2):
                                    blocks.append((gi, nl, r, e))

                    def binfo(b):
                        gi, nl, r, e = blocks[b]
                        hp, sbk, g = groups[gi]
                        d = A_PAT[g][1]
                        span = 128 * d
                        hrow = 1 if sbk > 0 else 0
                        has_prev = (sbk > 0) or (nl > 0)
                        kbs = ([0] if has_prev else []) + [1]
                        qcols = slice(nl * span + r, nl * span + r + 127 * d + 1, d)
                        return gi, nl, r, e, hp, sbk, g, d, span, hrow, kbs, qcols

                    def stageA(b):
                        gi, nl, r, e, hp, sbk, g, d, span, hrow, kbs, qcols = binfo(b)
                        bi = gi % 2
                        sp_ = b % 4
                        pt = pts[b % 4]
                        ptk = ("pt", b % 4)
                        stv = ps[sp_][:, 0:256].rearrange("p (k i) -> p k i", k=2)
                        for kb in kbs:
                            koff = (hrow + nl - 1 + kb) * span + r
                            kcols = slice(koff, koff + 127 * d + 1, d)
                            for R in range(2):
                                S.op("pe", lambda kb=kb, R=R, kcols=kcols: nc.tensor.matmul(
                                    stv[:, kb, :], kt[bi][e * 64:(e + 1) * 64, R, kcols], qt[bi][e * 64:(e + 1) * 64, R, qcols],
                                    start=(R == 0), stop=(R == 1)),
                                    reads=[("aq", bi), ("ak", bi)], writes=[pk[sp_]], inc=(R == 1))
                        k0 = kbs[0]
                        S.op("act", lambda: nc.scalar.activation(pt[:, k0:2, :], stv[:, k0:2, :], AF.Exp, scale=scale),
                             reads=[pk[sp_]], writes=[ptk])
                        S.op("pool", lambda: nc.gpsimd.tensor_tensor(pt[:, k0:2, :], pt[:, k0:2, :], amv[:, k0:2, :], ALU.mult),
                             reads=[ptk, "amask"], writes=[ptk])

                    def stageB(b):
                        gi, nl, r, e, hp, sbk, g, d, span, hrow, kbs, qcols = binfo(b)
                        bi = gi % 2
                        np_ = 4 + b % 4
                        pt = pts[b % 4]
                        ptk = ("pt", b % 4)
                        ndv = ps[np_][:, 0:256].rearrange("p (k i) -> p k i", k=2)
                        for idx, kb in enumerate(kbs):
                            vrow = (hrow + nl - 1 + kb) * d + r
                            S.op("pe", lambda kb=kb, vrow=vrow, idx=idx: nc.tensor.matmul(
                                ndv[:, 0, :], vt_[bi][:, vrow, e * 128:(e + 1) * 128], pt[:, kb, :],
                                start=(idx == 0), stop=(idx == len(kbs) - 1)),
                                reads=[("av", bi, hrow + nl - 1 + kb), ("avall", bi), ptk], writes=[pk[np_]], inc=False)
                        for idx, kb in enumerate(kbs):
                            S.op("pe", lambda kb=kb, idx=idx: nc.tensor.matmul(
                                ndv[:, 1, :], onesb[:], pt[:, kb, :],
                                start=(idx == 0), stop=(idx == len(kbs) - 1)),
                                reads=[ptk], writes=[pk[np_]], inc=(idx == len(kbs) - 1))
                        av = acc[e][:, :, qcols]
                        ak = ("acc", e)
                        if g == 0:
                            S.op("dve", lambda: nc.vector.tensor_copy(av, ndv), reads=[pk[np_]], writes=[ak])
                        else:
                            S.op("dve", lambda: nc.vector.tensor_tensor(av, ndv, av, ALU.add), reads=[pk[np_], ak], writes=[ak])

                    octr = [0]

                    def finish(gi):
                        hp, sbk, g = groups[gi]
                        q0 = sbk * QB
                        for e in range(2):
                            ak = ("acc", e)
                            oi = octr[0] % 2
                            octr[0] += 1
                            ob = osb[oi]
                            S.op("dve", lambda e=e: nc.vector.reciprocal(rec[:], acc[e][:, 1, :]), reads=[ak], writes=["rec"])
                            S.op("dve", lambda e=e, ob=ob: nc.vector.tensor_tensor(ob[:], acc[e][:, 0, :], rec[:], ALU.mult),
                                 reads=[ak, "rec"], writes=[("osb", oi)])
                            h = hp * 2 + e
                            S.dma("sp", oT[h * 128:(h + 1) * 128, q0:q0 + QB], ob[:], reads=[("osb", oi)], writes=[("oT", 0)])

                    NBLK = len(blocks)
                    load_group(0)
                    load_group(1)
                    stageA(0)
                    stageA(1)
                    stageA(2)
                    for b in range(NBLK):
                        gi = blocks[b][0]
                        first_of_group = (b == 0) or (blocks[b - 1][0] != gi)
                        if first_of_group and gi >= 1:
                            load_group(gi + 1)
                        stageB(b)
                        last_of_group = (b == NBLK - 1) or (blocks[b + 1][0] != gi)
                        if last_of_group and groups[gi][2] == 2:
                            finish(gi)
                        if b + 3 < NBLK:
                            stageA(b + 3)
                    S.barrier()
            else:
                with contextlib.ExitStack() as st:
                    GT = 256
                    CPG = GT // 128
                    qs = [sb(st, f"r_q{i}", [128, 8, 2, GT], BF16) for i in range(2)]
                    ks = [sb(st, f"r_k{i}", [128, 8, 2, GT], BF16) for i in range(2)]
                    vs_ = [sb(st, f"r_v{i}", [128, 4096], BF16) for i in range(2)]
                    gs_ = [sb(st, f"r_g{i}", [128, 4096], BF16) for i in range(2)]
                    Sf = sb(st, "r_S", [128, 8, 2, 512], F32)
                    Sb_ = sb(st, "r_Sb", [128, 8, 2, 512], BF16)
                    pts = [sb(st, f"r_pt{i}", [128, 128], BF16) for i in range(3)]
                    kds = [sb(st, f"r_kd{i}", [128, 2, 128], BF16) for i in range(3)]
                    stt = [sb(st, f"r_st{i}", [128, 6], F32) for i in range(3)]
                    mv = [sb(st, f"r_mv{i}", [128, 2], F32) for i in range(3)]
                    rs = [sb(st, f"r_rs{i}", [128, 2], F32) for i in range(3)]
                    yn = [sb(st, f"r_yn{i}", [128, 512], BF16) for i in range(3)]
                    yg = [sb(st, f"r_yg{i}", [128, 4096], BF16) for i in range(2)]
                    ots = [sb(st, f"r_ot{i}", [128, 32, GT], BF16) for i in range(2)]
                    log_g = [float(np.log1p(-np.exp2(-5.0 - h))) for h in range(8)]
                    cdec = [float(np.exp(lg * 128.0)) for lg in log_g]
                    NCH = T // 128
                    NG = T // GT
                    NIT = NCH * 8

                    def load_grp(grp):
                        if grp >= NG:
                            return
                        bi = grp % 2
                        t0 = grp * GT
                        S.dma("sp", qs[bi][:], qk_r[0][:, :, :, t0:t0 + GT].rearrange("h r p t -> p h r t"), writes=[("rq", bi)])
                        S.dma("sp", ks[bi][:], qk_r[1][:, :, :, t0:t0 + GT].rearrange("h r p t -> p h r t"), writes=[("rk", bi)])

                    def load_chunk(n):
                        if n >= NCH:
                            return
                        vi = n % 2
                        S.dma("sp", vs_[vi][:], v_r[n * 128:(n + 1) * 128, :], writes=[("rv", vi)])
                        S.dma("sp", gs_[vi][:], sg_r[n * 128:(n + 1) * 128, :], writes=[("rg", vi)])

                    def rA(i):
                        n, h = divmod(i, 8)
                        grp, cl = divmod(n, CPG)
                        bi = grp % 2
                        cols = slice(cl * 128, (cl + 1) * 128)
                        i3 = i % 3
                        p_st = i3
                        stv = ps[p_st][:, 0:128]
                        for R in range(2):
                            S.op("pe", lambda R=R: nc.tensor.matmul(
                                stv, ks[bi][:, h, R, cols], qs[bi][:, h, R, cols], start=(R == 0), stop=(R == 1)),
                                reads=[("rq", bi), ("rk", bi)], writes=[pk[p_st]], inc=(R == 1))
                        pt = pts[i3]
                        S.op("dve", lambda: nc.vector.scalar_tensor_tensor(
                            pt[:], stv, rconst[:, h:h + 1], rmask[:], ALU.mult, ALU.mult),
                            reads=[pk[p_st], "rconst", "rmask"], writes=[("rpt", i3)])
                        if n < NCH - 1:
                            ktv = ps[p_st][:, 256:384].bitcast(BF16).rearrange("p (r t) -> p r t", r=2)
                            for R in range(2):
                                S.op("pe", lambda R=R: nc.tensor.transpose(ktv[:, R, :], ks[bi][:, h, R, cols], identb[:]),
                                     reads=[("rk", bi)], writes=[pk[p_st]], inc=(R == 1))
                            kd = kds[i3]
                            S.op("act", lambda: nc.scalar.activation(kd[:], ktv, AF.Copy, scale=rconst[:, 8 + h:9 + h]),
                                 reads=[pk[p_st], "rconst"], writes=[("kd", i3)])

                    def rB(i):
                        n, h = divmod(i, 8)
                        grp, cl = divmod(n, CPG)
                        bi = grp % 2
                        vi = n % 2
                        cols = slice(cl * 128, (cl + 1) * 128)
                        i3 = i % 3
                        p_y = 3 + (i % 2)
                        pt = pts[i3]
                        ygb = yg[n % 2]
                        ygk = ("yg", n % 2)
                        vh = vs_[vi][:, h * 512:(h + 1) * 512]
                        yv = ps[p_y][:]
                        S.op("pe", lambda: nc.tensor.matmul(yv, pt[:], vh, start=True, stop=(n == 0)),
                             reads=[("rpt", i3), ("rv", vi)], writes=[pk[p_y]], inc=(n == 0))
                        if n > 0:
                            for R in range(2):
                                S.op("pe", lambda R=R: nc.tensor.matmul(
                                    yv, qs[bi][:, h, R, cols], Sb_[:, h, R, :], start=False, stop=(R == 1)),
                                    reads=[("rq", bi), ("Sb", h)], writes=[pk[p_y]], inc=(R == 1))
                        S.op("dve", lambda: nc.vector.bn_stats(stt[i3][:], yv), reads=[pk[p_y]], writes=[("stt", i3)])
                        S.op("dve", lambda: nc.vector.bn_aggr(mv[i3][:], stt[i3][:]), reads=[("stt", i3)], writes=[("mv", i3)])
                        S.op("act", lambda: nc.scalar.activation(
                            rs[i3][:, 0:1], mv[i3][:, 1:2], AF.Sqrt, bias=rconst[:, 16 + h:17 + h], scale=1.0),
                            reads=[("mv", i3), "rconst"], writes=[("rs", i3)])
                        S.op("dve", lambda: nc.vector.reciprocal(rs[i3][:, 0:1], rs[i3][:, 0:1]),
                             reads=[("rs", i3)], writes=[("rs", i3)])
                        S.op("dve", lambda: nc.vector.scalar_tensor_tensor(
                            rs[i3][:, 1:2], mv[i3][:, 0:1], -1.0, rs[i3][:, 0:1], ALU.mult, ALU.mult),
                            reads=[("rs", i3), ("mv", i3)], writes=[("rs", i3)])
                        S.op("act", lambda: nc.scalar.activation(
                            yn[i3][:], yv, AF.Identity, bias=rs[i3][:, 1:2], scale=rs[i3][:, 0:1]),
                            reads=[pk[p_y], ("rs", i3)], writes=[("yn", i3)])
                        S.op("pool", lambda: nc.gpsimd.tensor_tensor(
                            ygb[:, h * 512:(h + 1) * 512], yn[i3][:], gs_[vi][:, h * 512:(h + 1) * 512], ALU.mult),
                            reads=[("yn", i3), ("rg", vi)], writes=[ygk])
                        if n < NCH - 1:
                            kd = kds[i3]
                            for c in range(2):
                                p_su = 5 + c
                                S.op("pe", lambda c=c, p_su=p_su: nc.tensor.matmul(ps[p_su][:], kd[:, c, :], vh, start=True, stop=True),
                                     reads=[("kd", i3), ("rv", vi)], writes=[pk[p_su]], inc=True)
                                if n == 0:
                                    S.op("dve", lambda c=c, p_su=p_su: nc.vector.tensor_copy(Sf[:, h, c, :], ps[p_su][:]),
                                         reads=[pk[p_su]], writes=[("Sf", h)])
                                else:
                                    S.op("dve", lambda c=c, p_su=p_su: nc.vector.scalar_tensor_tensor(
                                        Sf[:, h, c, :], Sf[:, h, c, :], cdec[h], ps[p_su][:], ALU.mult, ALU.add),
                                        reads=[pk[p_su], ("Sf", h)], writes=[("Sf", h)])
                            S.op("act", lambda: nc.scalar.copy(Sb_[:, h, :, :], Sf[:, h, :, :]),
                                 reads=[("Sf", h)], writes=[("Sb", h)])
                        if h == 7:
                            ot = ots[bi]
                            otk = ("ots", bi)
                            for f8 in range(4):
                                pb = 7
                                tv = ps[pb][:].bitcast(BF16).rearrange("p (f t) -> p f t", f=8)
                                for ff in range(8):
                                    f = f8 * 8 + ff
                                    S.op("pe", lambda f=f, ff=ff: nc.tensor.transpose(
                                        tv[:, ff, :], ygb[:, f * 128:(f + 1) * 128], identb[:]),
                                        reads=[ygk], writes=[pk[pb]], inc=(ff == 7))
                                if f8 % 2 == 0:
                                    S.op("act", lambda f8=f8: nc.scalar.copy(ot[:, f8 * 8:(f8 + 1) * 8, cols], tv),
                                         reads=[pk[pb]], writes=[otk])
                                else:
                                    S.op("dve", lambda f8=f8: nc.vector.tensor_copy(ot[:, f8 * 8:(f8 + 1) * 8, cols], tv),
                                         reads=[pk[pb]], writes=[otk])
                            if cl == CPG - 1:
                                t0 = grp * GT
                                S.dma("sp", oT[:, t0:t0 + GT].rearrange("(f p) t -> p f t", p=128), ot[:],
                                      reads=[otk], writes=[("oT", 0)])

                    load_grp(0)
                    load_grp(1)
                    load_chunk(0)
                    rA(0)
                    rA(1)
                    for i in range(NIT):
                        n, h = divmod(i, 8)
                        if h == 0:
                            load_chunk(n + 1)
                            if n % CPG == 0 and n // CPG >= 1:
                                load_grp(n // CPG + 1)
                        rB(i)
                        if i + 2 < NIT:
                            rA(i + 2)
                    S.barrier()

            with contextlib.ExitStack() as st:
                KO = 16 if is_attn else 32
                wo_src = wo_a[li] if is_attn else wo_r[li]
                ht = sb(st, "f_ht", [128, 16, 512], F32)
                rstd = sb(st, "f_rstd", [128, 512], F32)
                hn = sb(st, "f_hn", [128, 16, 512], BF16)
                actb = sb(st, "f_act", [128, NFF, 512], BF16)
                ot = actb
                sq = actb[:, 28:44, :]
                wos = [sb(st, f"f_wo{i}", [128, KO, 128], BF16) for i in range(2)]
                wus = [sb(st, f"f_wu{i}", [128, 2, 16, 128], BF16) for i in range(3)]
                wds = [sb(st, f"f_wd{i}", [128, NFF, 128], BF16) for i in range(3)]
                ug = [sb(st, f"f_ug{i}", [128, 2, 514], F32) for i in range(2)]
                cg = [sb(st, f"f_cg{i}", [128, 2, 512], F32) for i in range(2)]
                sgt = [sb(st, f"f_sg{i}", [128, 512], F32) for i in range(2)]
                utail = sb(st, "f_utail", [128, 88, 2], F32)
                S.op("pool", lambda: nc.gpsimd.memset(utail[:], 0.0), writes=[("utail", q) for q in range(88)])
                cwv = cw[:].rearrange("p (l j w) -> p l j w", l=4, j=88)
                cbv = cb[:].rearrange("p (l j) -> p l j", l=4)
                woc = 0
                wuc = 0
                wdc = 0
                uc = 0
                for tb in range(NTB):
                    t0 = tb * 512
                    S.dma("sp", ht[:], hT[:, t0:t0 + 512].rearrange("(c p) t -> p c t", p=128),
                          reads=[("hT", tb)], writes=["ht"])
                    S.dma("sp", ot[:, 0:KO, :], oT[0:KO * 128, t0:t0 + 512].rearrange("(c p) t -> p c t", p=128),
                          reads=[("oT", 0)], writes=["actb"])
                    for m in range(16):
                        wo = wos[woc % 2]
                        wok = ("wo", woc % 2)
                        woc += 1
                        S.dma("sp", wo[:], wo_src[m], writes=[wok])
                        pb = m % 2
                        for kc in range(KO):
                            S.op("pe", lambda kc=kc, wo=wo, pb=pb: nc.tensor.matmul(
                                ps[pb][:], wo[:, kc, :], ot[:, kc, :], start=(kc == 0), stop=(kc == KO - 1)),
                                reads=[wok, "actb"], writes=[pk[pb]], inc=(kc == KO - 1))
                        S.op("dve", lambda m=m, pb=pb: nc.vector.tensor_tensor(ht[:, m, :], ps[pb][:], ht[:, m, :], ALU.add),
                             reads=[pk[pb], "ht"], writes=["ht"])
                    rmsnorm(ht, "ht", sq, "actb", rstd, "rstd", lambda c: hn[:, c, :], "hn", 4 + l, 7)
                    pending = None
                    for j in range(NFF):
                        wu = wus[wuc % 3]
                        wuk = ("wu", wuc % 3)
                        wuc += 1
                        S.dma("sp", wu[:, 0], wup_b[l, j], writes=[(wuk, 0)])
                        S.dma("sp", wu[:, 1], wup_b[l, NFF + j], writes=[(wuk, 1)])
                        ui = uc % 2
                        u = ug[ui]
                        c_ = cg[ui]
                        sg_ = sgt[ui]
                        pb = 2 + ui * 2
                        uc += 1
                        for s in range(2):
                            for kc in range(16):
                                S.op("pe", lambda s=s, kc=kc, wu=wu, pb=pb: nc.tensor.matmul(
                                    ps[pb + s][:], wu[:, s, kc, :], hn[:, kc, :], start=(kc == 0), stop=(kc == 15)),
                                    reads=[(wuk, s), "hn"], writes=[pk[pb + s]], inc=(kc == 15))
                        for s in range(2):
                            jj = s * NFF + j
                            utk, ubk, ck = ("ut", ui, s), ("ub", ui, s), ("cg", ui, s)
                            S.op("pool", lambda s=s, jj=jj, u=u: nc.gpsimd.tensor_copy(u[:, s, 0:2], utail[:, jj, :]),
                                 reads=[("utail", jj)], writes=[utk])
                            S.op("act", lambda s=s, u=u, pb=pb: nc.scalar.copy(u[:, s, 2:514], ps[pb + s][:]),
                                 reads=[pk[pb + s]], writes=[ubk])
                            S.op("act", lambda s=s, jj=jj, c_=c_, pb=pb: nc.scalar.activation(
                                c_[:, s, :], ps[pb + s][:], AF.Identity, bias=cbv[:, l, jj:jj + 1], scale=cwv[:, l, jj, 2:3]),
                                reads=[pk[pb + s], "cw", "cb"], writes=[ck])
                            S.op("pool", lambda s=s, jj=jj, u=u: nc.gpsimd.tensor_copy(utail[:, jj, :], u[:, s, 512:514]),
                                 reads=[ubk], writes=[("utail", jj)])
                            for w_ in range(2):
                                S.op("dve", lambda s=s, jj=jj, c_=c_, u=u, w_=w_: nc.vector.scalar_tensor_tensor(
                                    c_[:, s, :], u[:, s, w_:w_ + 512], cwv[:, l, jj, w_:w_ + 1], c_[:, s, :], ALU.mult, ALU.add),
                                    reads=[utk, ubk, ck, "cw"], writes=[ck])
                        if pending is not None:
                            pending()

                        def fin(j=j, ui=ui, c_=c_, sg_=sg_):
                            S.op("act", lambda: nc.scalar.activation(sg_[:], c_[:, 0, :], AF.Silu),
                                 reads=[("cg", ui, 0)], writes=[("sgt", ui)])
                            S.op("dve", lambda: nc.vector.tensor_tensor(actb[:, j, :], sg_[:], c_[:, 1, :], ALU.mult),
                                 reads=[("cg", ui, 1), ("sgt", ui)], writes=["actb"])
                        pending = fin
                    pending()
                    for m in range(16):
                        wd = wds[wdc % 3]
                        wdk = ("wd", wdc % 3)
                        wdc += 1
                        S.dma("sp", wd[:], wdn_b[l, m], writes=[wdk])
                        pb = m % 2
                        for kc in range(NFF):
                            S.op("pe", lambda kc=kc, wd=wd, pb=pb: nc.tensor.matmul(
                                ps[pb][:], wd[:, kc, :], actb[:, kc, :], start=(kc == 0), stop=(kc == NFF - 1)),
                                reads=[wdk, "actb"], writes=[pk[pb]], inc=(kc == NFF - 1))
                        S.op("dve", lambda m=m, pb=pb: nc.vector.tensor_tensor(ht[:, m, :], ps[pb][:], ht[:, m, :], ALU.add),
                             reads=[pk[pb], "ht"], writes=["ht"])
                    S.dma("sp", hT[:, t0:t0 + 512].rearrange("(c p) t -> p c t", p=128), ht[:],
                          reads=["ht"], writes=[("hT", tb)])
                S.barrier()

        with contextlib.ExitStack() as st:
            ht = sb(st, "z_ht", [128, 16, 512], F32)
            sq = sb(st, "z_sq", [128, 16, 512], BF16)
            rstd = sb(st, "z_rstd", [128, 512], F32)
            hn = sb(st, "z_hn", [128, 16, 512], F32)
            ob = [sb(st, f"z_ob{i}", [128, D], F32) for i in range(2)]
            oc = 0
            pc = 0
            for tb in range(NTB):
                t0 = tb * 512
                S.dma("sp", ht[:], hT[:, t0:t0 + 512].rearrange("(c p) t -> p c t", p=128),
                      reads=[("hT", tb)], writes=["ht"])
                rmsnorm(ht, "ht", sq, "sq", rstd, "rstd", lambda c: hn[:, c, :], "hn", 8, 7)
                for j in range(4):
                    o_ = ob[oc % 2]
                    ok_ = ("ob", oc % 2)
                    oc += 1
                    for c4 in range(4):
                        p = pc % 6
                        pc += 1
                        for cc in range(4):
                            c = c4 * 4 + cc
                            S.op("pe", lambda c=c, cc=cc, p=p, j=j: nc.tensor.transpose(
                                ps[p][:, cc * 128:(cc + 1) * 128], hn[:, c, j * 128:(j + 1) * 128], ident[:]),
                                reads=["hn"], writes=[pk[p]], inc=(cc == 3))
                        if c4 % 2 == 0:
                            S.op("act", lambda o_=o_, p=p, c4=c4: nc.scalar.copy(o_[:, c4 * 512:(c4 + 1) * 512], ps[p][:]),
                                 reads=[pk[p]], writes=[ok_])
                        else:
                            S.op("dve", lambda o_=o_, p=p, c4=c4: nc.vector.tensor_copy(o_[:, c4 * 512:(c4 + 1) * 512], ps[p][:]),
                                 reads=[pk[p]], writes=[ok_])
                    S.dma("sp", out[t0 + j * 128:t0 + (j + 1) * 128, :], o_[:], reads=[ok_], writes=[("out", 0)])
            S.barrier()
    return nc


def host_consts(T):
    pos = np.arange(T, dtype=np.float32)
    inv_a = (10000.0 ** (-np.arange(0, 128, 2, dtype=np.float32) / 128.0)).astype(np.float32)
    ang_a = (pos[None, :] * inv_a[:, None]).astype(np.float32)
    ca = np.concatenate([np.cos(ang_a), np.cos(ang_a)], 0).astype(np.float32)
    sa = np.concatenate([np.sin(ang_a), np.sin(ang_a)], 0).astype(np.float32)
    inv_r = (10000.0 ** (-np.linspace(0.0, 1.0, 128, dtype=np.float32))).astype(np.float32)
    ang_r = (pos[None, :] * inv_r[:, None]).astype(np.float32)
    cr, sr = np.cos(ang_r).astype(np.float32), np.sin(ang_r).astype(np.float32)
    j = np.arange(128)[:, None]
    i = np.arange(128)[None, :]
    amask = np.concatenate([(j >= i), (j <= i)], 1).astype(np.float32)
    rmask = (i >= j).astype(np.float32)
    log_g = np.log1p(-np.exp2(-5.0 - np.arange(8, dtype=np.float64)))
    jj = np.arange(128, dtype=np.float64)[:, None]
    rconst = np.zeros((128, 24), np.float64)
    rconst[:, 0:8] = np.exp(-log_g[None, :] * (jj + 1.0)) * 0.0625
    rconst[:, 8:16] = np.exp(log_g[None, :] * (127.0 - jj)) * 0.0625
    rconst[:, 16:24] = EPS * np.exp(-2.0 * log_g[None, :] * (jj + 1.0))
    return dict(ca=ca, sa=sa, cr=cr, sr=sr, amask=amask, rmask=rmask,
                rconst=rconst.astype(np.float32), ident=np.eye(128, dtype=np.float32))


def make_in_maps(inputs, T, n_cores=8):
    f = lambda a: np.ascontiguousarray(np.asarray(a, dtype=np.float32))
    hc = host_consts(T)
    gv = np.concatenate([f(inputs["norm_mix"]), f(inputs["norm_ffn"]), f(inputs["norm_final"])[None]], 0)
    gains = np.ascontiguousarray(gv.reshape(9, 16, 128).transpose(2, 0, 1).reshape(128, 144))
    cw = np.ascontiguousarray(f(inputs["conv_w"]).reshape(4, 3, 88, 128).transpose(3, 0, 2, 1).reshape(128, -1))
    cb = np.ascontiguousarray(f(inputs["conv_b"]).reshape(4, 88, 128).transpose(2, 0, 1).reshape(128, -1))
    x = f(inputs["x"])
    B = x.shape[0]
    shared = dict(gains=gains, cw=cw, cb=cb, **hc)
    for k in ("w_in_attn", "w_out_attn", "w_in_ret", "w_out_ret", "w_up", "w_down"):
        shared[k] = f(inputs[k])
    wkeys = ("w_in_attn", "w_out_attn", "w_in_ret", "w_out_ret", "w_up", "w_down")
    zeros = {k: np.zeros_like(shared[k]) for k in wkeys} if n_cores > len(REAL_CORES) else {}
    maps = []
    for c in range(n_cores):
        m = dict(shared)
        if n_cores <= len(REAL_CORES):
            m["x"] = np.ascontiguousarray(x[c % B, :T])
        elif c in REAL_CORES:
            m["x"] = np.ascontiguousarray(x[REAL_CORES.index(c) % B, :T])
        else:
            m["x"] = np.zeros((T, D), np.float32)
            m.update(zeros)
        maps.append(m)
    return maps


_NC_CACHE = {}


def kernel(**inputs):
    T, depth = 8192, 4
    key = (T, depth)
    if key not in _NC_CACHE:
        _NC_CACHE[key] = build(T, depth)
    nc = _NC_CACHE[key]
    maps = make_in_maps(inputs, T)
    res = run_bass_kernel_spmd(nc, maps, core_ids=list(range(8)))
    B = np.asarray(inputs["x"]).shape[0]
    return np.stack([np.asarray(res.results[REAL_CORES[b]]["out"], dtype=np.float32) for b in range(B)], 0)
```
